# Optimizing a Trainium2 kernel written in Bass

```python
import jax, jax.numpy as jnp
from jax import lax
import numpy as np

D_MODEL = 1024
BATCH = 8
SEQ = 2048
DEPTH = 2

HEAD_DIM = 64
N_HEADS_FOX = 6
N_HEADS_MOBA = 6
DILATED_PAIRS = ((128, 1), (512, 4), (2048, 16))
N_SLOTS_DIL = 4
N_HEADS_DIL = N_SLOTS_DIL * len(DILATED_PAIRS)
N_HEADS = N_HEADS_FOX + N_HEADS_MOBA + N_HEADS_DIL
MIX_WIDTH = N_HEADS * HEAD_DIM
N_BRANCH = 3
IN_COLS = 3 * MIX_WIDTH + N_HEADS_FOX + N_BRANCH * D_MODEL
FOX_BLOCK = 128
MOBA_BLOCK = 256
MOBA_TOPK = 3
MOBA_Q_CHUNK = 32
WIN_BLOCK = 128
N_ALIBI = N_HEADS_MOBA + N_HEADS_DIL
DIL_SLOPE_OFFSETS = (0, N_SLOTS_DIL, 2 * N_SLOTS_DIL + N_HEADS_MOBA)
MOBA_SLOPE_OFFSET = 2 * N_SLOTS_DIL
D_FF = 2816
N_EXPERTS = 8
TOP_K = 2
D_FF_EXPERT = 2816
N_DENSE = (DEPTH + 1) // 2
N_MOE = DEPTH // 2
RMS_EPS = 1e-6
NEG_INF = -1e30

kernel_name = 'hybrid_fox_moba_dilated_moe_adaln'


def _rmsnorm(x, g):
    xf = x.astype(jnp.float32)
    y = xf * lax.rsqrt(jnp.mean(xf * xf, axis=-1, keepdims=True) + RMS_EPS)
    return (y * g.astype(jnp.float32)).astype(x.dtype)


def _alibi_slopes(n):
    return jnp.asarray(2.0 ** (-8.0 * np.arange(1, n + 1) / n), dtype=jnp.float32)


def _merge_heads(o):
    B, H, S, dh = o.shape
    return o.transpose(0, 2, 1, 3).reshape(B, S, H * dh)


def _fox_attention(q, k, v, f_logit):
    B, H, S, dh = q.shape
    cum = jnp.cumsum(jax.nn.log_sigmoid(f_logit.astype(jnp.float32)), axis=1).transpose(0, 2, 1)
    nqb = S // FOX_BLOCK
    qb = q.reshape(B, H, nqb, FOX_BLOCK, dh).transpose(2, 0, 1, 3, 4)
    cb = cum.reshape(B, H, nqb, FOX_BLOCK).transpose(2, 0, 1, 3)
    key_pos = jnp.arange(S)
    scale = dh ** -0.5

    def block(args):
        i, q_i, c_i = args
        s = jnp.einsum('bhqd,bhkd->bhqk', q_i, k, preferred_element_type=jnp.float32) * scale
        s = s + c_i[..., :, None] - cum[:, :, None, :]
        qpos = i * FOX_BLOCK + jnp.arange(FOX_BLOCK)
        s = jnp.where(key_pos[None, :] <= qpos[:, None], s, NEG_INF)
        p = jax.nn.softmax(s, axis=-1)
        return jnp.einsum('bhqk,bhkd->bhqd', p.astype(v.dtype), v)

    out = lax.map(block, (jnp.arange(nqb), qb, cb))
    return out.transpose(1, 2, 0, 3, 4).reshape(B, H, S, dh)


def _moba_attention(q, k, v, slopes):
    B, H, S, dh = q.shape
    Sp = -(-S // MOBA_BLOCK) * MOBA_BLOCK
    pad = ((0, 0), (0, 0), (0, Sp - S), (0, 0))
    q, k, v = (jnp.pad(t, pad) for t in (q, k, v))
    nb = Sp // MOBA_BLOCK
    n_sel = min(MOBA_TOPK, nb)
    kb = k.reshape(B, H, nb, MOBA_BLOCK, dh)
    vb = v.reshape(B, H, nb, MOBA_BLOCK, dh)
    k_mean = jnp.mean(kb.astype(jnp.float32), axis=3)
    gate = jnp.einsum('bhsd,bhnd->bhsn', q.astype(jnp.float32), k_mean)
    q_blk = jnp.arange(Sp) // MOBA_BLOCK
    gate = jnp.where(jnp.arange(nb)[None, :] < q_blk[:, None], gate, NEG_INF)
    _, sel = lax.top_k(gate, n_sel)
    sel_ok = sel < q_blk[:, None]
    scale = dh ** -0.5
    b_idx = jnp.arange(B)[:, None, None, None]
    h_idx = jnp.arange(H)[None, :, None, None]
    blk_pos = jnp.arange(MOBA_BLOCK)

    def chunk(i):
        start = i * MOBA_Q_CHUNK
        q_c = lax.dynamic_slice_in_dim(q, start, MOBA_Q_CHUNK, axis=2)
        sel_c = lax.dynamic_slice_in_dim(sel, start, MOBA_Q_CHUNK, axis=2)
        ok_c = lax.dynamic_slice_in_dim(sel_ok, start, MOBA_Q_CHUNK, axis=2)
        qpos = start + jnp.arange(MOBA_Q_CHUNK)
        own = start // MOBA_BLOCK
        k_own = lax.dynamic_slice_in_dim(k, own * MOBA_BLOCK, MOBA_BLOCK, axis=2)
        v_own = lax.dynamic_slice_in_dim(v, own * MOBA_BLOCK, MOBA_BLOCK, axis=2)
        dist_own = (qpos[:, None] - (own * MOBA_BLOCK + blk_pos)[None, :])
        s_own = jnp.einsum('bhcd,bhld->bhcl', q_c, k_own, preferred_element_type=jnp.float32) * scale
        s_own = s_own - slopes[:, None, None] * dist_own.astype(jnp.float32)
        s_own = jnp.where(dist_own >= 0, s_own, NEG_INF)
        k_sel = kb[b_idx, h_idx, sel_c]
        v_sel = vb[b_idx, h_idx, sel_c]
        s_sel = jnp.einsum('bhcd,bhcnld->bhcnl', q_c, k_sel, preferred_element_type=jnp.float32) * scale
        dist_sel = qpos[:, None, None] - (sel_c[..., None] * MOBA_BLOCK + blk_pos)
        s_sel = s_sel - slopes[:, None, None, None] * dist_sel.astype(jnp.float32)
        s_sel = jnp.where(ok_c[..., None], s_sel, NEG_INF)
        s = jnp.concatenate([s_own, s_sel.reshape(B, H, MOBA_Q_CHUNK, n_sel * MOBA_BLOCK)], axis=-1)
        p = jax.nn.softmax(s, axis=-1).astype(v.dtype)
        p_own = p[..., :MOBA_BLOCK]
        p_sel = p[..., MOBA_BLOCK:].reshape(B, H, MOBA_Q_CHUNK, n_sel, MOBA_BLOCK)
        return (jnp.einsum('bhcl,bhld->bhcd', p_own, v_own)
                + jnp.einsum('bhcnl,bhcnld->bhcd', p_sel, v_sel))

    out = lax.map(chunk, jnp.arange(Sp // MOBA_Q_CHUNK))
    return out.transpose(1, 2, 0, 3, 4).reshape(B, H, Sp, dh)[:, :, :S]


def _dilated_group(q, k, v, slopes, window, dil):
    B, H, S, dh = q.shape
    n_back = window // dil
    L = S // dil
    Lp = -(-L // WIN_BLOCK) * WIN_BLOCK
    nblk = Lp // WIN_BLOCK

    def to_blocks(t):
        t = t.reshape(B, H, L, dil, dh).transpose(0, 1, 3, 2, 4)
        t = jnp.pad(t, ((0, 0), (0, 0), (0, 0), (0, Lp - L), (0, 0)))
        return t.reshape(B, H, dil, nblk, WIN_BLOCK, dh)

    def band(t):
        prev = jnp.concatenate([jnp.zeros_like(t[:, :, :, :1]), t[:, :, :, :-1]], axis=3)
        return jnp.concatenate([prev, t], axis=4)

    qb = to_blocks(q)
    kband = band(to_blocks(k))
    vband = band(to_blocks(v))
    s = jnp.einsum('bhrnqd,bhrnkd->bhrnqk', qb, kband, preferred_element_type=jnp.float32) * (dh ** -0.5)
    steps = (jnp.arange(WIN_BLOCK)[:, None] + WIN_BLOCK) - jnp.arange(2 * WIN_BLOCK)[None, :]
    key_sub = (jnp.arange(nblk)[:, None, None] - 1) * WIN_BLOCK + jnp.arange(2 * WIN_BLOCK)[None, None, :]
    valid = (steps >= 0) & (steps <= n_back) & (key_sub >= 0)
    s = s - slopes[:, None, None, None, None] * (steps * dil).astype(jnp.float32)
    s = jnp.where(valid, s, NEG_INF)
    lse = jax.nn.logsumexp(s, axis=-1)
    p = jnp.exp(s - lse[..., None])
    o = jnp.einsum('bhrnqk,bhrnkd->bhrnqd', p.astype(v.dtype), vband)

    def from_blocks(t):
        t = t.reshape((B, H, dil, Lp) + t.shape[5:])[:, :, :, :L]
        t = jnp.moveaxis(t, 2, 3)
        return t.reshape((B, H, S) + t.shape[4:])

    return from_blocks(o), from_blocks(lse)


def _token_mixer(h, w_in, b_fgate, q_gain, k_gain, w_br_fox, w_br_moba, w_br_dil, w_out):
    B, S, _ = h.shape
    proj = h @ w_in
    qkv = proj[..., :3 * MIX_WIDTH].reshape(B, S, 3, N_HEADS, HEAD_DIM)
    f_logit = proj[..., 3 * MIX_WIDTH:3 * MIX_WIDTH + N_HEADS_FOX] + b_fgate
    gates = jax.nn.sigmoid(proj[..., 3 * MIX_WIDTH + N_HEADS_FOX:].astype(jnp.float32))
    gates = gates.reshape(B, S, N_BRANCH, D_MODEL).astype(h.dtype)
    q = _rmsnorm(qkv[:, :, 0], q_gain).transpose(0, 2, 1, 3)
    k = _rmsnorm(qkv[:, :, 1], k_gain).transpose(0, 2, 1, 3)
    v = qkv[:, :, 2].transpose(0, 2, 1, 3)
    slopes = _alibi_slopes(N_ALIBI)

    a1 = N_HEADS_FOX
    b1 = a1 + N_HEADS_MOBA
    o_fox = _fox_attention(q[:, :a1], k[:, :a1], v[:, :a1], f_logit)
    o_moba = _moba_attention(q[:, a1:b1], k[:, a1:b1], v[:, a1:b1],
                             slopes[MOBA_SLOPE_OFFSET:MOBA_SLOPE_OFFSET + N_HEADS_MOBA])
    outs, lses = [], []
    for g, (window, dil) in enumerate(DILATED_PAIRS):
        lo = b1 + g * N_SLOTS_DIL
        so = DIL_SLOPE_OFFSETS[g]
        o_g, l_g = _dilated_group(q[:, lo:lo + N_SLOTS_DIL], k[:, lo:lo + N_SLOTS_DIL],
                                  v[:, lo:lo + N_SLOTS_DIL], slopes[so:so + N_SLOTS_DIL], window, dil)
        outs.append(o_g)
        lses.append(l_g)
    w_dil = jax.nn.softmax(jnp.stack(lses, axis=0), axis=0)
    o_dil = jnp.einsum('gbhs,gbhsd->bhsd', w_dil.astype(v.dtype), jnp.stack(outs, axis=0))

    y = (gates[:, :, 0] * (_merge_heads(o_fox) @ w_br_fox)
         + gates[:, :, 1] * (_merge_heads(o_moba) @ w_br_moba)
         + gates[:, :, 2] * (_merge_heads(o_dil) @ w_br_dil))
    return y @ w_out


def _swiglu(h, w_gate, w_up, w_down):
    return (jax.nn.silu(h @ w_gate) * (h @ w_up)) @ w_down


def _moe_swiglu(h, w_router, b_router, w_gate, w_up, w_down):
    logits = (h @ w_router).astype(jnp.float32) + b_router.astype(jnp.float32)
    top_val, top_idx = lax.top_k(logits, TOP_K)
    top_w = jax.nn.softmax(top_val, axis=-1)
    combine = jnp.sum(jax.nn.one_hot(top_idx, N_EXPERTS, dtype=jnp.float32) * top_w[..., None], axis=-2)
    combine = combine.astype(h.dtype)
    out = jnp.zeros_like(h)
    for e in range(N_EXPERTS):
        out = out + combine[..., e:e + 1] * _swiglu(h, w_gate[e], w_up[e], w_down[e])
    return out


def setup_inputs(seed: int = 0) -> dict:
    key = jax.random.key(seed)
    ks = jax.random.split(key, 22)
    D = D_MODEL

    def nrm(k, shape, scale):
        return jax.random.normal(k, shape, jnp.float32) * scale

    wf = N_HEADS_FOX * HEAD_DIM
    wm = N_HEADS_MOBA * HEAD_DIM
    wd = N_SLOTS_DIL * HEAD_DIM
    return {
        'x': nrm(ks[0], (BATCH, SEQ, D), 1.0),
        'c': nrm(ks[1], (BATCH, D), 1.0),
        'w_ada': nrm(ks[2], (DEPTH, D, 6 * D), 0.5 * D ** -0.5),
        'b_ada': nrm(ks[3], (DEPTH, 6 * D), 0.05),
        'norm_mix': 1.0 + nrm(ks[4], (DEPTH, D), 0.05),
        'norm_ffn': 1.0 + nrm(ks[5], (DEPTH, D), 0.05),
        'w_in': nrm(ks[6], (DEPTH, D, IN_COLS), D ** -0.5),
        'b_fgate': jax.random.uniform(ks[7], (DEPTH, N_HEADS_FOX), jnp.float32, 1.0, 6.0),
        'q_gain': 1.0 + nrm(ks[8], (DEPTH, N_HEADS, HEAD_DIM), 0.05),
        'k_gain': 1.0 + nrm(ks[9], (DEPTH, N_HEADS, HEAD_DIM), 0.05),
        'w_br_fox': nrm(ks[10], (DEPTH, wf, D), wf ** -0.5),
        'w_br_moba': nrm(ks[11], (DEPTH, wm, D), wm ** -0.5),
        'w_br_dil': nrm(ks[12], (DEPTH, wd, D), wd ** -0.5),
        'w_out': nrm(ks[13], (DEPTH, D, D), D ** -0.5),
        'w_ffn_gate': nrm(ks[14], (N_DENSE, D, D_FF), D ** -0.5),
        'w_ffn_up': nrm(ks[15], (N_DENSE, D, D_FF), D ** -0.5),
        'w_ffn_down': nrm(ks[16], (N_DENSE, D_FF, D), D_FF ** -0.5),
        'w_router': nrm(ks[17], (N_MOE, D, N_EXPERTS), D ** -0.5),
        'b_router': nrm(ks[18], (N_MOE, N_EXPERTS), 0.01),
        'w_exp_gate': nrm(ks[19], (N_MOE, N_EXPERTS, D, D_FF_EXPERT), D ** -0.5),
        'w_exp_up': nrm(ks[20], (N_MOE, N_EXPERTS, D, D_FF_EXPERT), D ** -0.5),
        'w_exp_down': nrm(ks[21], (N_MOE, N_EXPERTS, D_FF_EXPERT, D), D_FF_EXPERT ** -0.5),
    }


def reference(x, c, w_ada, b_ada, norm_mix, norm_ffn, w_in, b_fgate, q_gain, k_gain,
              w_br_fox, w_br_moba, w_br_dil, w_out, w_ffn_gate, w_ffn_up, w_ffn_down,
              w_router, b_router, w_exp_gate, w_exp_up, w_exp_down):
    cond = jax.nn.silu(c)
    for l in range(DEPTH):
        mod = (cond @ w_ada[l] + b_ada[l])[:, None, :]
        shift1, scale1, gate1, shift2, scale2, gate2 = jnp.split(mod, 6, axis=-1)
        h = _rmsnorm(x, norm_mix[l]) * (1 + scale1) + shift1
        x = x + gate1 * _token_mixer(h, w_in[l], b_fgate[l], q_gain[l], k_gain[l],
                                     w_br_fox[l], w_br_moba[l], w_br_dil[l], w_out[l])
        h = _rmsnorm(x, norm_ffn[l]) * (1 + scale2) + shift2
        i = l // 2
        if l % 2 == 0:
            y = _swiglu(h, w_ffn_gate[i], w_ffn_up[i], w_ffn_down[i])
        else:
            y = _moe_swiglu(h, w_router[i], b_router[i], w_exp_gate[i], w_exp_up[i], w_exp_down[i])
        x = x + gate2 * y
    return x
```

```python
from concourse.bass_utils import run_bass_kernel_spmd
import numpy as np
import concourse.bass as bass
import concourse.mybir as mybir

F32 = mybir.dt.float32
BF16 = mybir.dt.bfloat16
AF = mybir.ActivationFunctionType
ALU = mybir.AluOpType
AX = mybir.AxisListType

_DTSIZE = {}


def dtsize(dt):
    if dt not in _DTSIZE:
        _DTSIZE[dt] = np.dtype(mybir.dt.np(dt)).itemsize
    return _DTSIZE[dt]


class Rec:
    __slots__ = ("eng", "fn", "deps", "dma", "sig", "idx", "gidx", "dmasem", "dmaval", "vc", "cond", "sv")


class FW:
    ENGS = ("pe", "act", "dve", "pool", "sp")
    CENGS = ("pe", "act", "dve")

    def __init__(self, nc, n_dma_sems=24):
        self.nc = nc
        self.recs = []
        self.eng_recs = {e: [] for e in self.ENGS}
        self.hist = {}
        self.engobj = {"pe": nc.tensor, "act": nc.scalar, "dve": nc.vector, "pool": nc.gpsimd, "sp": nc.sync}
        self.n_dma_sems = n_dma_sems
        self.dma_count = {e: 0 for e in self.ENGS}
        self.dma_last = {}
        self.cur_cond = None
        self.cond_vals = {}

    def region(self, ap):
        t = ap.tensor
        name = t.name
        space = str(ap.space) if hasattr(ap, "space") else ""
        esz = dtsize(ap.dtype)
        apl = list(ap.ap)
        off = ap.offset
        is_dram = "DRAM" in space.upper() or "HBM" in space.upper() or type(t).__name__.startswith("DRam")
        if is_dram:
            lo = off
            hi = off
            for st, cnt in apl:
                if cnt > 1:
                    if st >= 0:
                        hi += st * (cnt - 1)
                    else:
                        lo += st * (cnt - 1)
            return (name, 0, 1, lo * esz, (hi + 1) * esz, False)
        tsz = dtsize(t.dtype)
        pstride = 1
        for s in t.shape[1:]:
            pstride *= s
        if esz != tsz:
            pstride = pstride * tsz // esz
        p0 = off // pstride
        f0 = off % pstride
        pst, pcnt = apl[0]
        if pst == 0:
            pcnt = 1
        p1 = p0 + pcnt
        lo = f0
        hi = f0
        for st, cnt in apl[1:]:
            if cnt > 1:
                if st >= 0:
                    hi += st * (cnt - 1)
                else:
                    lo += st * (cnt - 1)
        b0, b1 = lo * esz, (hi + 1) * esz
        is_psum = type(t).__name__.startswith("PSum")
        if is_psum:
            b0 = (b0 // 2048) * 2048
            b1 = ((b1 + 2047) // 2048) * 2048
            p0, p1 = 0, 128
        return (name, p0, p1, b0, b1, is_psum)

    def op(self, eng, fn, reads=(), writes=(), dma=False):
        r = Rec()
        r.cond = self.cur_cond if eng in self.CENGS else None
        r.sv = None
        r.eng = eng
        r.fn = fn
        r.dma = dma
        r.sig = False
        r.deps = []
        r.idx = len(self.eng_recs[eng])
        r.gidx = len(self.recs)
        r.dmasem = None
        deps = set()
        for ap in reads:
            self._access(r, self.region(ap), False, deps)
        for ap in writes:
            self._access(r, self.region(ap), True, deps)
        if dma:
            slot = self.dma_count[eng] % self.n_dma_sems
            self.dma_count[eng] += 1
            prev = self.dma_last.get((eng, slot))
            if prev is not None:
                deps.add(prev)
            self.dma_last[(eng, slot)] = r
            r.dmasem = slot
        r.deps = sorted(deps, key=lambda d: d.gidx)
        self.recs.append(r)
        self.eng_recs[eng].append(r)
        return r

    def _access(self, r, reg, is_write, deps):
        name, p0, p1, b0, b1, is_psum = reg
        lst = self.hist.setdefault(name, [])
        excl = is_write or is_psum
        keep = []
        for ent in lst:
            ep0, ep1, eb0, eb1, w, rds, wtrue = ent
            if ep1 <= p0 or p1 <= ep0 or eb1 <= b0 or b1 <= eb0:
                keep.append(ent)
                continue
            inside = ep0 >= p0 and ep1 <= p1 and eb0 >= b0 and eb1 <= b1
            if w is not None and w is not r:
                if w.dma or r.dma or w.eng != r.eng:
                    deps.add(w)
                elif wtrue and (not is_write) and r.eng != "pe":
                    deps.add(w)
            if excl:
                for rd in rds:
                    if rd is r:
                        continue
                    if rd.dma or r.dma or rd.eng != r.eng:
                        deps.add(rd)
                if inside:
                    continue
            else:
                if w is None and inside and len(rds) == 1 and (not rds[0].dma) and (not r.dma) and rds[0].eng == r.eng:
                    continue
            keep.append(ent)
        if excl:
            keep.append([p0, p1, b0, b1, r, [], is_write])
        else:
            keep.append([p0, p1, b0, b1, None, [r], False])
        self.hist[name] = keep

    def barrier(self, engs=("pe", "act", "dve")):
        lasts = {e: self.eng_recs[e][-1] for e in engs if self.eng_recs[e]}
        for e in engs:
            eo = self.engobj[e]
            r = self.op(e, (lambda eo=eo: eo.drain()), reads=[], writes=[])
            extra = [lasts[o] for o in engs if o != e and o in lasts]
            r.deps = sorted(set(r.deps) | set(extra), key=lambda d: d.gidx)

    def emit(self):
        nc = self.nc
        ne = len(self.ENGS)
        eidx = {e: i for i, e in enumerate(self.ENGS)}
        grp_of = {}
        groups = []
        for ce in self.CENGS:
            prev = None
            for r in self.eng_recs[ce]:
                if r.cond is None:
                    prev = None
                    continue
                if prev is not None and prev.cond is not None and prev.cond[0] == r.cond[0] and prev.cond[1] <= r.cond[1] \
                        and prev.idx == r.idx - 1:
                    groups[-1].append(r)
                else:
                    groups.append([r])
                grp_of[r.gidx] = len(groups) - 1
                prev = r
        clock = {e: [-1] * ne for e in self.ENGS}
        dma_known = {e: set() for e in self.ENGS}
        final_deps = []
        cur_grp = {e: None for e in self.CENGS}
        saved = {e: None for e in self.CENGS}
        for r in self.recs:
            if r.eng in self.CENGS:
                g = grp_of.get(r.gidx)
                if g != cur_grp[r.eng]:
                    if cur_grp[r.eng] is not None:
                        clock[r.eng] = saved[r.eng][0]
                        dma_known[r.eng] = saved[r.eng][1]
                    if g is not None:
                        saved[r.eng] = (list(clock[r.eng]), set(dma_known[r.eng]))
                    cur_grp[r.eng] = g
            ck = clock[r.eng]
            need = []
            for d in r.deps:
                if d.dma:
                    if d.gidx in dma_known[r.eng]:
                        continue
                    need.append(d)
                else:
                    if ck[eidx[d.eng]] >= d.idx:
                        continue
                    need.append(d)
            best = {}
            nd = []
            for d in need:
                if d.dma:
                    nd.append(d)
                else:
                    if d.eng not in best or best[d.eng].idx < d.idx:
                        best[d.eng] = d
            nd.extend(best.values())
            for d in nd:
                d.sig = True
                if d.dma:
                    dma_known[r.eng].add(d.gidx)
                    dvc = d.vc
                else:
                    dvc = list(d.vc)
                    dvc[eidx[d.eng]] = max(dvc[eidx[d.eng]], d.idx)
                for i in range(ne):
                    if dvc[i] > ck[i]:
                        ck[i] = dvc[i]
            if r.eng in self.CENGS and cur_grp[r.eng] is not None:
                r.vc = list(saved[r.eng][0])
            else:
                r.vc = list(ck)
            final_deps.append(nd)
        cnt = {e: 0 for e in self.ENGS}
        dcnt = {}
        for r in self.recs:
            if r.dma:
                key = (r.eng, r.dmasem)
                dcnt[key] = dcnt.get(key, 0) + 16
                r.dmaval = dcnt[key]
            elif r.sig:
                cnt[r.eng] += 1
                r.sv = cnt[r.eng]
        sems = {}
        for e in self.ENGS:
            sems[e] = nc.alloc_semaphore("sem_" + e)
        dsems = {}
        for e in self.ENGS:
            if self.dma_count[e] > 0:
                dsems[e] = [nc.alloc_semaphore("dsem_%s_%d" % (e, i)) for i in range(self.n_dma_sems)]
        self.nwait = 0

        def emit_one(r, nd):
            eo = self.engobj[r.eng]
            for d in nd:
                if d.dma:
                    eo.wait_ge(dsems[d.eng][d.dmasem], d.dmaval)
                else:
                    eo.wait_ge(sems[d.eng], d.sv)
                self.nwait += 1
            ins = r.fn()
            if r.dma:
                ins.then_inc(dsems[r.eng][r.dmasem], 16)
            elif r.sig:
                ins.then_inc(sems[r.eng], 1)

        def emit_group(recs_):
            eng = recs_[0].eng
            eo = self.engobj[eng]
            key = recs_[0].cond[0]
            val = self.cond_vals[(key, eng)]
            levels = []
            for r in recs_:
                if not levels or levels[-1][0] != r.cond[1]:
                    levels.append((r.cond[1], []))
                levels[-1][1].append(r)

            def rec_level(li):
                if li == len(levels):
                    return
                c, rs = levels[li]
                nsig = sum(1 for lv in levels[li:] for r in lv[1] if r.sig)
                with eo.If(val > c):
                    for r in rs:
                        emit_one(r, final_deps[r.gidx])
                    rec_level(li + 1)
                with eo.Else():
                    if nsig > 0:
                        eo.drain()
                        eo.sem_inc(sems[eng], nsig)
            rec_level(0)

        done = set()
        for r, nd in zip(self.recs, final_deps):
            if r.gidx in done:
                continue
            g = grp_of.get(r.gidx)
            if g is not None:
                emit_group(groups[g])
                for x in groups[g]:
                    done.add(x.gidx)
            else:
                emit_one(r, nd)
        for (e, slot), r in self.dma_last.items():
            self.engobj[e].wait_ge(dsems[e][slot], r.dmaval)
        self.stats = dict(n=len(self.recs), waits=self.nwait, per_eng={e: len(v) for e, v in self.eng_recs.items()},
                          ngroups=len(groups))
        return self.stats

    def dma(self, eng, out, in_, **kw):
        o = self.engobj[eng]
        return self.op(eng, lambda: o.dma_start(out=out, in_=in_, **kw), reads=[in_], writes=[out], dma=True)

    def mm(self, out, lhsT, rhs, start=True, stop=True, **kw):
        t = self.nc.tensor
        return self.op("pe", lambda: t.matmul(out, lhsT, rhs, start=start, stop=stop, **kw), reads=[lhsT, rhs], writes=[out])

    def tr(self, out, in_, ident):
        t = self.nc.tensor
        return self.op("pe", lambda: t.transpose(out, in_, ident), reads=[in_, ident], writes=[out])

    def act(self, out, in_, func, bias=None, scale=None, accum_out=None):
        s = self.nc.scalar
        kw = {}
        rd = [in_]
        if bias is not None:
            kw["bias"] = bias
            if not isinstance(bias, (int, float)):
                rd.append(bias)
        if scale is not None:
            kw["scale"] = scale
            if not isinstance(scale, (int, float)):
                rd.append(scale)
        wr = [out]
        if accum_out is not None:
            kw["accum_out"] = accum_out
            wr.append(accum_out)
        return self.op("act", lambda: s.activation(out=out, in_=in_, func=func, **kw), reads=rd, writes=wr)

    def _veng(self, eng):
        return self.engobj[eng]

    def tt(self, eng, out, in0, in1, op):
        e = self._veng(eng)
        return self.op(eng, lambda: e.tensor_tensor(out=out, in0=in0, in1=in1, op=op), reads=[in0, in1], writes=[out])

    def ts(self, eng, out, in0, s1, s2=None, op0=ALU.mult, op1=None):
        e = self._veng(eng)
        rd = [in0]
        for s in (s1, s2):
            if s is not None and not isinstance(s, (int, float)):
                rd.append(s)
        kw = {}
        if op1 is not None:
            kw["op1"] = op1
        return self.op(eng, lambda: e.tensor_scalar(out=out, in0=in0, scalar1=s1, scalar2=s2, op0=op0, **kw), reads=rd, writes=[out])

    def stt(self, out, in0, scalar, in1, op0, op1, eng="dve"):
        e = self._veng(eng)
        rd = [in0, in1]
        if not isinstance(scalar, (int, float)):
            rd.append(scalar)
        return self.op(eng, lambda: e.scalar_tensor_tensor(out=out, in0=in0, scalar=scalar, in1=in1, op0=op0, op1=op1), reads=rd, writes=[out])

    def copy(self, eng, out, in_):
        if eng == "act":
            s = self.nc.scalar
            return self.op("act", lambda: s.copy(out=out, in_=in_), reads=[in_], writes=[out])
        e = self._veng(eng)
        return self.op(eng, lambda: e.tensor_copy(out=out, in_=in_), reads=[in_], writes=[out])

    def memset(self, eng, out, val):
        e = self._veng(eng)
        return self.op(eng, lambda: e.memset(out, val), reads=[], writes=[out])


def _fw_generic(self, eng, name, reads, writes, *args, **kw):
    e = self.engobj[eng]
    f = getattr(e, name)
    return self.op(eng, lambda: f(*args, **kw), reads=reads, writes=writes)


FW.gen = _fw_generic


import numpy as np
import ml_dtypes

D = 1024
T = 2048
NH = 24
DFF = 2816
NFF = 22
NEXP = 8
IN_COLS = 7686
QOFF, KOFF, VOFF, FOFF, GOFF = 0, 1536, 3072, 4608, 4614
EPS = 1e-6
SLOPES = (2.0 ** (-8.0 * np.arange(1, 19) / 18)).astype(np.float32)
DIL = ((128, 1), (512, 4), (2048, 16))
DIL_SO = (0, 4, 14)
MOBA_SO = 8
NEGM = -30000.0


def split3(v):
    v = np.asarray(v, np.float32)
    hi = v.astype(ml_dtypes.bfloat16)
    r1 = v - hi.astype(np.float32)
    mid = r1.astype(ml_dtypes.bfloat16)
    r2 = r1 - mid.astype(np.float32)
    lo = r2.astype(ml_dtypes.bfloat16)
    return hi, mid, lo


def host_consts():
    c = {}
    c["ident_f"] = np.eye(128, dtype=np.float32)
    c["ident_b"] = np.eye(128, dtype=np.float32).astype(ml_dtypes.bfloat16)
    ob = np.zeros((128, 128), np.float32)
    ob[0:64, 0:64] = 1.0
    c["onesblk"] = ob.astype(ml_dtypes.bfloat16)
    c["ones_b"] = np.ones((128, 128), np.float32).astype(ml_dtypes.bfloat16)
    c["ones_f"] = np.ones((128, 128), np.float32)
    k = np.arange(128)[:, None]
    q = np.arange(128)[None, :]
    c["tri"] = (q >= k).astype(np.float32).astype(ml_dtypes.bfloat16)
    db = np.zeros((128, 12, 256), np.float32)
    for g, (w, d) in enumerate(DIL):
        for j in range(4):
            sl = SLOPES[DIL_SO[g] + j]
            left = np.where(q >= k, -sl * d * (q - k), NEGM)
            right = np.where(k >= q, -sl * d * (128 + q - k), NEGM)
            db[:, g * 4 + j, 0:128] = left
            db[:, g * 4 + j, 128:256] = right
    c["dilbias"] = db.reshape(128, 12 * 256)
    t = np.arange(T, dtype=np.float32)
    aq = np.zeros((6, 6, T), ml_dtypes.bfloat16)
    ak = np.zeros((6, 6, T), ml_dtypes.bfloat16)
    for h in range(6):
        sl = SLOPES[MOBA_SO + h]
        qh = split3(-8.0 * sl * t)
        kh = split3(8.0 * sl * t)
        for i in range(3):
            aq[h, i] = qh[i]
            aq[h, 3 + i] = 1.0
            ak[h, i] = 1.0
            ak[h, 3 + i] = kh[i]
    c["moba_aq"] = aq
    c["moba_ak"] = ak
    bi = np.zeros((8, T), np.float32)
    for b in range(8):
        bi[b, b * 256:(b + 1) * 256] = 1.0
    c["blkind"] = bi.astype(ml_dtypes.bfloat16)
    pos = (np.arange(16)[None, :, None] * 128 + np.arange(128)[:, None, None])
    own = pos // 256
    B = np.arange(8)[None, None, :]
    c["mg_sb1"] = np.where(B < own, 0.0, -1e30).astype(np.float32).reshape(128, 128)
    c["mg_v1"] = (B < own).astype(np.float32).reshape(128, 128)
    c["mg_e"] = (B == own).astype(np.float32).reshape(128, 128)
    c["fox_ones"] = np.ones((3, T), np.float32).astype(ml_dtypes.bfloat16)
    c["ustrict"] = np.triu(np.ones((128, 128), np.float32), 1).astype(ml_dtypes.bfloat16)
    c["ebase"] = np.tile(np.arange(8, dtype=np.float32) * 2048.0, (128, 16))
    return c


CONST_DT = dict(ident_f=F32, ident_b=BF16, onesblk=BF16, ones_b=BF16, ones_f=F32, tri=BF16, dilbias=F32,
                moba_aq=BF16, moba_ak=BF16, blkind=BF16, mg_sb1=F32, mg_v1=F32, mg_e=F32, fox_ones=BF16, ustrict=BF16, ebase=F32)


def host_layout(inp, b):
    m = {}
    m["x"] = np.ascontiguousarray(inp["x"][b])
    m["cT"] = np.ascontiguousarray(inp["c"][b].reshape(8, 128).T)
    m["b_adaT"] = np.ascontiguousarray(inp["b_ada"].reshape(2, 48, 128).transpose(2, 0, 1))
    m["nmixT"] = np.ascontiguousarray(inp["norm_mix"].reshape(2, 8, 128).transpose(2, 0, 1))
    m["nffnT"] = np.ascontiguousarray(inp["norm_ffn"].reshape(2, 8, 128).transpose(2, 0, 1))
    m["qgT"] = np.ascontiguousarray(inp["q_gain"].transpose(2, 0, 1))
    m["kgT"] = np.ascontiguousarray(inp["k_gain"].transpose(2, 0, 1))
    m["bfg"] = np.ascontiguousarray(inp["b_fgate"].T)
    m["b_router"] = np.ascontiguousarray(inp["b_router"])
    for k in ("w_ada", "w_in", "w_br_fox", "w_br_moba", "w_br_dil", "w_out", "w_ffn_gate", "w_ffn_up",
              "w_ffn_down", "w_router", "w_exp_gate", "w_exp_up", "w_exp_down"):
        m[k] = inp[k]
    return m


IN_SHAPES = dict(
    x=[T, D], cT=[128, 8], b_adaT=[128, 2, 48], nmixT=[128, 2, 8], nffnT=[128, 2, 8], qgT=[64, 2, 24], kgT=[64, 2, 24],
    bfg=[6, 2], b_router=[1, 8], w_ada=[2, D, 6 * D], w_in=[2, D, IN_COLS], w_br_fox=[2, 384, D], w_br_moba=[2, 384, D],
    w_br_dil=[2, 256, D], w_out=[2, D, D], w_ffn_gate=[1, D, DFF], w_ffn_up=[1, D, DFF], w_ffn_down=[1, DFF, D],
    w_router=[1, D, 8], w_exp_gate=[1, 8, D, DFF], w_exp_up=[1, 8, D, DFF], w_exp_down=[1, 8, DFF, D])


def prod(xs):
    r = 1
    for x in xs:
        r *= x
    return r


A_OT = 0
A_Y = 32768
A_VA = 32768
A_VB = 32768 + 12544
A_QK = 57856
A_X = 65536
A_WQK = 74240
A_WV = 82432
A_ACCN = 88576
A_ODD = 96768
A_PT = 100864
A_Q2 = 103936
A_RQ = 105984
A_RD = 110080
A_BC = 112128
A_MG = 114176
A_KM = 118272
A_FST = 65536
A_Z = 131072
A_DILM = 131072
A_Z2 = 137216
A_END = 151552
A_ACT = 0
A_WGU = 45056
A_WD = 53248
A_SG = 58880
A_CBC = A_Z2
A_RT = A_Z2 + 4096


class K:
    def __init__(self, debug=None, nlayers=2, stop_after=None, heads=None, sparse=True):
        self.debug = debug or []
        self.nlayers = nlayers
        self.stop_after = stop_after
        self.heads = heads
        nc = bass.Bass("TRN2", target_bir_lowering=False)
        self.nc = nc
        self.fw = FW(nc)
        self.inp = {}
        for k, shp in IN_SHAPES.items():
            self.inp[k] = nc.dram_tensor(k, shp, F32, kind="ExternalInput").ap()
        self.cst = {}
        hc = host_consts()
        self.hc = hc
        for k, v in hc.items():
            self.cst[k] = nc.dram_tensor("c_" + k, list(v.shape), CONST_DT[k], kind="ExternalInput").ap()
        self.out = nc.dram_tensor("out", [T, D], F32, kind="ExternalOutput").ap()
        self.xs = nc.dram_tensor("xs_scr", [128, 8 * T], F32, kind="Internal").ap()
        self.fsc = nc.dram_tensor("f_scr", [2, 6, 3 * T], BF16, kind="Internal").ap()
        self.HS = nc.dram_tensor("hs_scr", [NEXP * T, 1026], BF16, kind="Internal").ap()
        self.YS = nc.dram_tensor("ys_scr", [NEXP * T, D], BF16, kind="Internal").ap()
        self.XTOK = nc.dram_tensor("xtok_scr", [T, D], F32, kind="Internal").ap()
        self.sparse = sparse
        self.dbg = {}
        self._rr = 0
        self._cnt = 0
        self.build()

    def sb(self, name, shape, dt):
        return self.nc.alloc_sbuf_tensor("s_" + name, shape, dt)

    def carve(self, off, shape, dt):
        n = prod(shape[1:])
        nb = n * dtsize(dt)
        assert off % 4 == 0 and off + nb <= A_END, (off, nb)
        v = self.AR[0:shape[0], off // 2:(off + nb) // 2]
        if dt != BF16:
            v = v.bitcast(dt)
        if len(shape) == 3:
            v = v.rearrange("p (a b) -> p a b", a=shape[1])
        elif len(shape) == 4:
            v = v.rearrange("p (a b c) -> p a b c", a=shape[1], b=shape[2])
        return v

    def dbg_out(self, name, shape, dt=F32):
        t = self.nc.dram_tensor("dbg_" + name, shape, dt, kind="ExternalOutput").ap()
        self.dbg[name] = t
        return t

    def evac_eng(self):
        self._rr += 1
        return "act" if self._rr % 2 else "dve"

    def wview(self, w2d, c0, ncols):
        return w2d.rearrange("(kc p) n -> p kc n", p=128)[:, :, c0:c0 + ncols]

    def build(self):
        nc, fw = self.nc, self.fw
        C = {}
        for k in ("ident_f", "ident_b", "onesblk", "ones_b", "ones_f", "tri", "mg_sb1", "mg_v1", "mg_e", "ustrict", "ebase"):
            v = self.hc[k]
            C[k] = self.sb("k_" + k, list(v.shape), CONST_DT[k])
            fw.dma("sp", C[k][:], self.cst[k][:, :])
        self.C = C
        self.cT = self.sb("cT", [128, 8], F32)
        fw.dma("sp", self.cT[:], self.inp["cT"][:, :])
        self.b_adaT = self.sb("b_adaT", [128, 2, 48], F32)
        fw.dma("sp", self.b_adaT[:], self.inp["b_adaT"][:, :, :])
        self.nmixT = self.sb("nmixT", [128, 2, 8], F32)
        fw.dma("sp", self.nmixT[:], self.inp["nmixT"][:, :, :])
        self.nffnT = self.sb("nffnT", [128, 2, 8], F32)
        fw.dma("sp", self.nffnT[:], self.inp["nffnT"][:, :, :])
        self.qg = self.sb("qg", [64, 2, 24], F32)
        fw.dma("sp", self.qg[:], self.inp["qgT"][:, :, :])
        self.kg = self.sb("kg", [64, 2, 24], F32)
        fw.dma("sp", self.kg[:], self.inp["kgT"][:, :, :])
        self.bfg = self.sb("bfg", [6, 2], F32)
        fw.dma("sp", self.bfg[:], self.inp["bfg"][:, :])
        self.negb = self.sb("negb", [6, 2], F32)
        fw.ts("dve", self.negb[:], self.bfg[:], -1.0, None, op0=ALU.mult)
        self.AR = self.sb("arena", [128, A_END // 2], BF16)
        self.hT = self.sb("hT", [128, 8, T], BF16)
        self.xT = self.carve(A_X, [128, 8, T], F32)
        self.psb = [nc.alloc_psum_tensor("psb%d" % i, [128, 512], F32) for i in range(8)]
        self.cond = self.sb("cond", [128, 8], BF16)
        self.modT = self.sb("modT", [128, 2, 48], F32)
        self.a1 = self.sb("a1", [128, 2, 8], F32)
        self.a2 = self.sb("a2", [128, 2, 8], F32)
        self.sqb = [self.sb("sqb%d" % i, [128, 512], BF16) for i in range(3)]
        self.rstd = [self.sb("rstd%d" % i, [128, 512], F32) for i in range(2)]
        self.ntmp = [self.sb("ntmp%d" % i, [128, 512], F32) for i in range(3)]
        self.OT = [self.carve(A_OT + i * 4096, [128, T], BF16) for i in range(8)]
        self.VA = self.carve(A_VA, [128, 16, 6, 65], BF16)
        self.VB = self.carve(A_VB, [128, 16, 6, 65], BF16)
        self.Qb = [self.carve(A_QK + (2 * s) * 4096, [128, T], BF16) for s in range(2)]
        self.Kb = [self.carve(A_QK + (2 * s + 1) * 4096, [128, T], BF16) for s in range(2)]
        self.wqk = [self.carve(A_WQK + s * 4096, [128, 8, 256], BF16) for s in range(2)]
        self.wv = self.carve(A_WV, [128, 8, 384], BF16)
        self.accN = self.carve(A_ACCN, [128, T], F32)
        self.oddt = self.carve(A_ODD, [128, T], BF16)
        self.ptb = [self.carve(A_PT + i * 1024, [128, 512], BF16) for i in range(3)]
        self.q2b = [self.carve(A_Q2 + i * 1024, [128, 512], BF16) for i in range(2)]
        self.rqb = [self.carve(A_RQ + i * 2048, [128, 512], F32) for i in range(2)]
        self.rd = self.carve(A_RD, [128, 512], F32)
        self.bcs = self.carve(A_BC, [128, 512], F32)
        self.dilM = self.carve(A_DILM, [128, 12, 256], BF16)
        self.rbt4 = [self.carve(119296 + i * 1024, [128, 512], BF16) for i in range(4)]
        self.rbt = self.rbt4[0:2]
        self.rlt = [self.carve(119296 + 4096 + i * 2048, [128, 512], F32) for i in range(2)]

        self.stage_load_x()
        self.stage_adaln()
        for l in range(self.nlayers):
            self.stage_rmsnorm(l, 0)
            if self.stop_after == "norm1":
                break
            fw.dma("sp", self.xs[:, :], self.xT.rearrange("p a b -> p (a b)"))
            if l == 0:
                self.stage_dilmask()
            self.stage_attention(l)
            if self.stop_after == "attn":
                break
            fw.dma("sp", self.xT.rearrange("p a b -> p (a b)"), self.xs[:, :])
            self.stage_outproj(l)
            if self.stop_after == "outproj":
                break
            self.stage_rmsnorm(l, 1, rt_ps=(self.psb[7] if l % 2 == 1 else None))
            if l % 2 == 0:
                self.ffn_expert(l, self.inp["w_ffn_gate"][0], self.inp["w_ffn_up"][0], self.inp["w_ffn_down"][0], None)
                if self.sparse and self.stop_after is None and l == 0:
                    zt = self.carve(A_VB, [128, 4104], BF16)
                    fw.memset("pool", zt, 0.0)
                    hsv = self.HS.rearrange("(p r) c -> p (r c)", p=128)
                    for i in range(32):
                        fw.dma("sp", hsv[:, i * 4104:(i + 1) * 4104], zt)
            elif self.sparse and l == self.nlayers - 1 and self.stop_after is None:
                self.stage_moe_sparse(l)
                self.final_done = True
            else:
                self.stage_moe(l)
            if self.stop_after == "ffn":
                break
        if self.stop_after is None and not getattr(self, "final_done", False):
            self.stage_output()
        for nm in self.debug:
            if nm == "hT":
                o = self.dbg_out("hT", [128, 8, T], BF16)
                fw.dma("sp", o[:, :, :], self.hT[:])
            elif nm == "xT":
                o = self.dbg_out("xT", [128, 8, T], F32)
                fw.dma("sp", o[:, :, :], self.xT)
            elif nm == "OT":
                o = self.dbg_out("OT", [8, 128, T], BF16)
                for i in range(8):
                    fw.dma("sp", o[i], self.OT[i])
            elif nm == "modT":
                o = self.dbg_out("modT", [128, 2, 48], F32)
                fw.dma("sp", o[:, :, :], self.modT[:])
        self.stats = fw.emit()

    def stage_load_x(self):
        fw, C = self.fw, self.C
        xin = [self.carve(i * 4096, [128, D], F32) for i in range(4)]
        for g in range(4):
            for i in range(4):
                tt = g * 4 + i
                fw.dma("sp", xin[i], self.inp["x"][tt * 128:(tt + 1) * 128, :])
            for dc in range(8):
                pb = self.psb[dc % 4]
                for i in range(4):
                    fw.tr(pb[:, i * 128:(i + 1) * 128], xin[i][:, dc * 128:(dc + 1) * 128], C["ident_f"][:])
                fw.copy(self.evac_eng(), self.xT[:, dc, g * 512:(g + 1) * 512], pb[:, :])

    def stage_adaln(self):
        fw, C = self.fw, self.C
        fw.act(self.cond[:], self.cT[:], AF.Silu)
        wbuf = [self.carve(i * 8192, [128, 8, 512], BF16) for i in range(2)]
        n = 0
        for l in range(self.nlayers):
            pm = self.psb[4 + l]
            wv = self.inp["w_ada"][l].rearrange("(kc p) n -> p kc n", p=128)
            for half in range(12):
                wb = wbuf[n % 2]
                n += 1
                fw.dma("pool", wb, wv[:, :, half * 512:(half + 1) * 512])
                for c4 in range(4):
                    j = half * 4 + c4
                    for kc in range(8):
                        fw.mm(pm[:, j:j + 1], wb[:, kc, c4 * 128:(c4 + 1) * 128], self.cond[:, kc:kc + 1],
                              start=(kc == 0), stop=(kc == 7))
            fw.tt("dve", self.modT[:, l, :], pm[:, 0:48], self.b_adaT[:, l, :], ALU.add)
            fw.stt(self.a1[:, l, :], self.modT[:, l, 8:16], 1.0, self.nmixT[:, l, :], ALU.add, ALU.mult)
            fw.stt(self.a2[:, l, :], self.modT[:, l, 32:40], 1.0, self.nffnT[:, l, :], ALU.add, ALU.mult)

    def stage_rmsnorm(self, l, which, rt_ps=None):
        fw, C = self.fw, self.C
        a = self.a1 if which == 0 else self.a2
        shoff = 0 if which == 0 else 24
        for tt in range(4):
            cs = slice(tt * 512, (tt + 1) * 512)
            pss = self.psb[tt % 2]
            for dc in range(8):
                sq = self.sqb[dc % 3]
                fw.act(sq[:], self.xT[:, dc, cs], AF.Square)
                fw.mm(pss[:, :], C["ones_b"][:], sq[:], start=(dc == 0), stop=(dc == 7))
            rs = self.rstd[tt % 2]
            fw.act(rs[:], pss[:, :], AF.Ln, bias=EPS, scale=1.0 / D)
            fw.act(rs[:], rs[:], AF.Exp, scale=-0.5)
            if rt_ps is not None:
                for i in range(4):
                    fw.mm(rt_ps[:, tt * 4 + i:tt * 4 + i + 1], rs[0:1, i * 128:(i + 1) * 128], C["ones_f"][0:1, 0:1])
            for dc in range(8):
                nt = self.ntmp[dc % 3]
                fw.stt(nt[:], self.xT[:, dc, cs], a[:, l, dc:dc + 1], rs[:], ALU.mult, ALU.mult)
                fw.act(self.hT[:, dc, cs], nt[:], AF.Identity, bias=self.modT[:, l, shoff + dc:shoff + dc + 1])

    def stage_dilmask(self):
        fw = self.fw
        tmp = self.carve(A_X, [128, 3072], F32)
        fw.dma("sp", tmp, self.cst["dilbias"][:, :])
        fw.act(self.dilM.rearrange("p a b -> p (a b)"), tmp, AF.Exp)

    def fox_prep(self, l):
        fw = self.fw
        wf = self.carve(A_Z2, [128, 8, 6], BF16)
        fw.dma("pool", wf, self.wview(self.inp["w_in"][l], FOFF, 6))
        fl = self.carve(A_FST, [6, T], F32)
        G = self.carve(A_FST + 8192, [6, T], F32)
        r1 = self.carve(A_FST + 16384, [6, T], F32)
        kp = self.carve(A_FST + 24576, [6, 3, T], BF16)
        qp = self.carve(A_FST + 36864, [6, 3, T], BF16)
        for tt in range(4):
            cs = slice(tt * 512, (tt + 1) * 512)
            ps = self.psb[tt % 2]
            for kc in range(8):
                fw.mm(ps[0:6, :], wf[:, kc, :], self.hT[:, kc, cs], start=(kc == 0), stop=(kc == 7))
            fw.act(fl[:, cs], ps[0:6, :], AF.Exp, scale=-1.0, bias=self.negb[0:6, l:l + 1])
        fw.act(fl, fl, AF.Ln, bias=1.0)
        fw.memset("dve", r1, 1.0)
        fw.gen("dve", "tensor_tensor_scan", [fl, r1], [G], out=G, data0=r1, data1=fl, initial=0.0,
               op0=ALU.mult, op1=ALU.add)
        fw.ts("dve", kp[:, 0, :], G, 8.0, None, op0=ALU.mult)
        fw.stt(r1, G, 8.0, kp[:, 0, :], ALU.mult, ALU.subtract)
        fw.copy("dve", kp[:, 1, :], r1)
        fw.tt("dve", r1, r1, kp[:, 1, :], ALU.subtract)
        fw.copy("dve", kp[:, 2, :], r1)
        kpf = kp.rearrange("p a b -> p (a b)")
        qpf = qp.rearrange("p a b -> p (a b)")
        fw.ts("dve", qpf, kpf, -1.0, None, op0=ALU.mult)
        fw.dma("sp", self.fsc[0], qpf)
        fw.dma("sp", self.fsc[1], kpf)

    def proj_qk(self, l, h, s, d):
        fw, C = self.fw, self.C
        w = self.wqk[s]
        Wl = self.inp["w_in"][l]
        fw.dma("pool", w[:, :, 0:128], self.wview(Wl, QOFF + h * 64, 128))
        fw.dma("pool", w[:, :, 128:256], self.wview(Wl, KOFF + h * 64, 128))
        fw.memset("pool", self.Qb[s][64:128, :], 0.0)
        fw.memset("pool", self.Kb[s][64:128, :], 0.0)
        for which, dst, gain in ((0, self.Qb[s], self.qg), (1, self.Kb[s], self.kg)):
            for tt in range(4):
                cs = slice(tt * 512, (tt + 1) * 512)
                ps = self.psb[tt % 2]
                for kc in range(8):
                    fw.mm(ps[:, :], w[:, kc, which * 128:(which + 1) * 128], self.hT[:, kc, cs],
                          start=(kc == 0), stop=(kc == 7))
                q2 = self.q2b[tt % 2]
                fw.act(q2, ps[:, :], AF.Square)
                p2 = self.psb[2]
                fw.mm(p2[:, :], C["onesblk"][:], q2, start=True, stop=True)
                rq = self.rqb[tt % 2]
                fw.act(rq[0:64, :], p2[0:64, :], AF.Ln, bias=EPS, scale=1.0 / 64)
                fw.act(rq[0:64, :], rq[0:64, :], AF.Exp, scale=-0.5)
                if d == 1:
                    o, i0, i1 = dst[0:64, cs], ps[0:64, :], rq[0:64, :]
                else:
                    n0, n1 = tt * 512 // d, (tt + 1) * 512 // d
                    o = dst[0:64, :].rearrange("p (r n) -> p n r", r=d)[:, n0:n1, :]
                    i0 = ps[0:64, :].rearrange("p (n r) -> p n r", r=d)
                    i1 = rq[0:64, :].rearrange("p (n r) -> p n r", r=d)
                fw.stt(o, i0, gain[:, l, h:h + 1], i1, ALU.mult, ALU.mult)

    def load_wv(self, l, grp):
        self.fw.dma("pool", self.wv, self.wview(self.inp["w_in"][l], VOFF + grp * 384, 384))

    def proj_v(self, nh, d, Vbuf, slot0, wcol0):
        fw = self.fw
        L = T // d
        nblk = L // 128
        for pb in range(16):
            r, nb = pb // nblk, pb % nblk
            ps = self.psb[pb % 2]
            if d == 1:
                tok = slice(pb * 128, (pb + 1) * 128)
            else:
                st0 = nb * 128 * d + r
                tok = slice(st0, st0 + 127 * d + 1, d)
            for kc in range(8):
                fw.mm(ps[:, 0:nh * 64], self.hT[:, kc, tok], self.wv[:, kc, wcol0:wcol0 + nh * 64],
                      start=(kc == 0), stop=(kc == 7))
            fw.copy(self.evac_eng(), Vbuf[:, pb, slot0:slot0 + nh, 0:64],
                    ps[:, 0:nh * 64].rearrange("p (h e) -> p h e", h=nh))

    def recip_row(self, den, k, eng):
        fw = self.fw
        rb = self.rbt[k % 2] if eng == "dve" else self.rbt4[k % 4]
        if eng == "dve":
            nc = self.nc
            o_ = rb[64:65, :]

            def f(o_=o_, den=den):
                with nc.allow_low_precision("bf16 reciprocal row feeds a bf16 broadcast matmul"):
                    return nc.vector.reciprocal(out=o_, in_=den)
            fw.op("dve", f, reads=[den], writes=[o_])
        else:
            lt = self.rlt[k % 2]
            fw.act(lt[64:65, :], den, AF.Ln)
            fw.act(rb[64:65, :], lt[64:65, :], AF.Exp, scale=-1.0)
        return rb

    def bcast_mul(self, src, rb, dest):
        fw, C = self.fw, self.C
        bp = self.psb[2]
        fw.mm(bp[:, :], C["ones_b"][64:65, :], rb[64:65, :], start=True, stop=True)
        fw.tt("dve", dest, src, bp[0:64, :], ALU.mult)

    def attn_causal(self, s, Vbuf, vslot, dest, hook=None, pending=None):
        fw, C = self.fw, self.C
        Qh, Kh = self.Qb[s], self.Kb[s]
        otS = (self.rd, self.bcs)
        for j in range(4):
            ot = self.psb[6 + j % 2]
            nkb = 4 * j + 4

            def S(kb):
                c0 = max(0, kb - 4 * j) * 128
                st = self.psb[3 + kb % 3]
                fw.mm(st[:, c0:512], Kh[:, kb * 128:(kb + 1) * 128], Qh[:, j * 512 + c0:(j + 1) * 512],
                      start=True, stop=True)
            S(0)
            if nkb > 1:
                S(1)
            for kb in range(nkb):
                if kb + 2 < nkb:
                    S(kb + 2)
                c0 = max(0, kb - 4 * j) * 128
                st = self.psb[3 + kb % 3]
                pt = self.ptb[kb % 3]
                fw.act(pt[:, c0:512], st[:, c0:512], AF.Exp, scale=0.125)
                if kb >= 4 * j:
                    fw.tt("pool", pt[:, c0:c0 + 128], pt[:, c0:c0 + 128], C["tri"][:], ALU.mult)
                fw.mm(ot[0:65, c0:512], Vbuf[:, kb, vslot, 0:65], pt[:, c0:512], start=(kb == 0), stop=(kb == nkb - 1))
                if kb == min(5, nkb - 1) and pending is not None:
                    pending()
                    pending = None
                if hook is not None and j == 2 and kb == 5:
                    hook()
                    hook = None
            if pending is not None:
                pending()
                pending = None

            o = otS[j % 2]
            fw.copy("act", o[0:65, :], ot[0:65, :])
            rb = self.recip_row(o[64:65, :], j, "dve")

            def fin(j=j, o=o, rb=rb):
                self.bcast_mul(o[0:64, :], rb, dest[:, j * 512:(j + 1) * 512])
            pending = fin
        return pending

    def fox_aug(self, h, s):
        fw = self.fw
        Qh, Kh = self.Qb[s], self.Kb[s]
        fw.dma("sp", Qh[64:67, :], self.fsc[0, h].rearrange("(a t) -> a t", a=3))
        fw.dma("sp", Qh[67:70, :], self.cst["fox_ones"][:, :])
        fw.dma("sp", Kh[64:67, :], self.cst["fox_ones"][:, :])
        fw.dma("sp", Kh[67:70, :], self.fsc[1, h].rearrange("(a t) -> a t", a=3))

    def moba_prep(self, h6, s):
        fw, C = self.fw, self.C
        Qh, Kh = self.Qb[s], self.Kb[s]
        km = self.carve(A_KM, [128, 8], F32)
        kmb = self.carve(A_KM + 64, [128, 8], BF16)
        fw.gen("dve", "tensor_reduce", [Kh[0:64, :]], [km[0:64, :]], out=km[0:64, :],
               in_=Kh[0:64, :].rearrange("p (b n) -> p b n", b=8), axis=AX.X, op=ALU.add)
        fw.ts("dve", kmb[0:64, :], km[0:64, :], 1.0 / 256, None, op0=ALU.mult)
        gp = self.psb[2]
        for qb in range(16):
            fw.mm(gp[:, qb * 8:(qb + 1) * 8], Qh[0:64, qb * 128:(qb + 1) * 128], kmb[0:64, :], start=True, stop=True)
        gm = self.carve(A_MG, [128, 128], F32)
        m8 = self.carve(A_MG + 512, [128, 128], F32)
        sel = self.carve(A_MG + 1024, [128, 128], F32)
        mbb = self.carve(A_MG + 1536, [128, 128], BF16)
        fw.tt("dve", gm, gp[:, 0:128], C["mg_sb1"][:], ALU.add)
        for qb in range(16):
            sl = slice(qb * 8, (qb + 1) * 8)
            fw.gen("dve", "max", [gm[:, sl]], [m8[:, sl]], out=m8[:, sl], in_=gm[:, sl])
        gm3 = gm.rearrange("p (a b) -> p a b", b=8)
        m83 = m8.rearrange("p (a b) -> p a b", b=8)
        sel3 = sel.rearrange("p (a b) -> p a b", b=8)
        fw.tt("dve", sel3, gm3, m83[:, :, 2:3].to_broadcast([128, 16, 8]), ALU.is_ge)
        fw.tt("dve", sel, sel, C["mg_v1"][:], ALU.mult)
        fw.tt("dve", sel, sel, C["mg_e"][:], ALU.add)
        fw.ts("dve", mbb, sel, 240000.0, -240000.0, op0=ALU.mult, op1=ALU.add)
        for tt in range(4):
            pp = self.psb[tt % 2]
            for i in range(4):
                qb = tt * 4 + i
                fw.mm(pp[64:72, i * 128:(i + 1) * 128], mbb[:, qb * 8:(qb + 1) * 8], C["ident_b"][:], start=True, stop=True)
            fw.copy(self.evac_eng(), Qh[64:72, tt * 512:(tt + 1) * 512], pp[64:72, :])
        fw.dma("sp", Qh[72:78, :], self.cst["moba_aq"][h6])
        fw.dma("sp", Kh[64:72, :], self.cst["blkind"][:, :])
        fw.dma("sp", Kh[72:78, :], self.cst["moba_ak"][h6])

    def attn_dil(self, g, j, s, Vbuf, vslot, first, hook=None, pending=None):
        fw = self.fw
        d = DIL[g][1]
        L = T // d
        nblk = L // 128
        Qh, Kh = self.Qb[s], self.Kb[s]
        M = self.dilM[:, g * 4 + j, :]
        accN = self.accN

        def S(pbq):
            nb = pbq % nblk
            st = self.psb[3 + pbq % 3]
            qs = slice(pbq * 128, (pbq + 1) * 128)
            fw.mm(st[:, 0:128], Kh[:, qs], Qh[:, qs], start=True, stop=True)
            if nb > 0:
                fw.mm(st[:, 128:256], Kh[:, (pbq - 1) * 128:pbq * 128], Qh[:, qs], start=True, stop=True)
        S(0)
        S(1)
        for c in range(4):
            ot = self.psb[6 + c % 2]
            for i in range(4):
                pbq = c * 4 + i
                nb = pbq % nblk
                if pbq + 2 < 16:
                    S(pbq + 2)
                st = self.psb[3 + pbq % 3]
                pt = self.ptb[pbq % 3]
                w = 256 if nb > 0 else 128
                fw.act(pt[:, 0:w], st[:, 0:w], AF.Exp, scale=0.125)
                fw.tt("pool", pt[:, 0:w], pt[:, 0:w], M[:, 0:w], ALU.mult)
                oc = ot[0:65, i * 128:(i + 1) * 128]
                fw.mm(oc, Vbuf[:, pbq, vslot, 0:65], pt[:, 0:128], start=True, stop=(nb == 0))
                if nb > 0:
                    fw.mm(oc, Vbuf[:, pbq - 1, vslot, 0:65], pt[:, 128:256], start=False, stop=True)
                if pending is not None and pbq == 2:
                    pending()
                    pending = None
            if d == 1:
                dst, src = accN[0:65, c * 512:(c + 1) * 512], ot[0:65, :]
            elif d == 4:
                dst, src = accN[0:65, c:c + 4 * 511 + 1:4], ot[0:65, :]
            else:
                dst = accN[0:65, :].rearrange("p (n r) -> p r n", r=16)[:, 4 * c:4 * c + 4, :]
                src = ot[0:65, :].rearrange("p (r n) -> p r n", r=4)
            if first:
                fw.copy("dve", dst, src)
            else:
                fw.tt("dve", dst, src, dst, ALU.add)
            if hook is not None and c == 1:
                hook()
                hook = None

    def stage_attention(self, l):
        fw = self.fw
        heads = self.heads or ("fox", "moba", "dil")
        fw.memset("pool", self.VA[:, :, :, 64:65], 1.0)
        fw.memset("pool", self.VB[:, :, :, 64:65], 1.0)
        jobs = []
        if "fox" in heads:
            self.fox_prep(l)
            for h in range(6):
                s = h % 2

                def prep(h=h, s=s):
                    if h == 0:
                        self.load_wv(l, 0)
                        self.proj_v(6, 1, self.VA, 0, 0)
                    self.proj_qk(l, h, s, 1)
                    self.fox_aug(h, s)

                def attn(hook, pending, h=h, s=s):
                    dest = self.OT[h // 2][0:64, :] if h % 2 == 0 else self.oddt[0:64, :]
                    p = self.attn_causal(s, self.VA, h, dest, hook=hook, pending=pending)
                    if h % 2 == 1:
                        def fin2(p=p, h=h):
                            p()
                            fw.dma("sp", self.OT[h // 2][64:128, :], self.oddt[0:64, :])
                        return fin2
                    return p
                jobs.append((prep, attn))
        if "moba" in heads:
            for h6 in range(6):
                h = 6 + h6
                s = h % 2

                def prep(h=h, h6=h6, s=s):
                    if h6 == 0:
                        self.load_wv(l, 1)
                        self.proj_v(6, 1, self.VB, 0, 0)
                    self.proj_qk(l, h, s, 1)
                    self.moba_prep(h6, s)

                def attn(hook, pending, h6=h6, s=s):
                    dest = self.OT[3 + h6 // 2][0:64, :] if h6 % 2 == 0 else self.oddt[0:64, :]
                    p = self.attn_causal(s, self.VB, h6, dest, hook=hook, pending=pending)
                    if h6 % 2 == 1:
                        def fin2(p=p, h6=h6):
                            p()
                            fw.dma("sp", self.OT[3 + h6 // 2][64:128, :], self.oddt[0:64, :])
                        return fin2
                    return p
                jobs.append((prep, attn))
        if "dil" in heads:
            n = 0
            for j in range(4):
                for g in range(3):
                    h = 12 + g * 4 + j
                    Vbuf, vslot = (self.VA, h - 12) if h < 18 else (self.VB, h - 18)
                    s = n % 2
                    first_dil = (n == 0)
                    n += 1

                    def prep(h=h, s=s, g=g, first_dil=first_dil):
                        if first_dil:
                            self.load_wv(l, 2)
                            self.proj_v(4, 1, self.VA, 0, 0)
                            self.proj_v(2, 4, self.VA, 4, 256)
                        if h == 20:
                            self.load_wv(l, 3)
                            self.proj_v(2, 4, self.VB, 0, 0)
                            self.proj_v(4, 16, self.VB, 2, 128)
                        self.proj_qk(l, h, s, DIL[g][1])

                    def attn(hook, pending, g=g, j=j, s=s, Vbuf=Vbuf, vslot=vslot):
                        self.attn_dil(g, j, s, Vbuf, vslot, g == 0, hook=hook, pending=pending)
                        if g == 2:
                            dest = self.OT[6 + j // 2][0:64, :] if j % 2 == 0 else self.oddt[0:64, :]
                            rbs = []
                            for tt in range(4):
                                cs = slice(tt * 512, (tt + 1) * 512)
                                rbs.append(self.recip_row(self.accN[64:65, cs], tt, "act"))

                            def fin(j=j, dest=dest):
                                for tt in range(4):
                                    cs = slice(tt * 512, (tt + 1) * 512)
                                    rb = self.rbt4[tt]
                                    self.bcast_mul(self.accN[0:64, cs], rb, dest[:, cs])
                                if j % 2 == 1:
                                    fw.dma("sp", self.OT[6 + j // 2][64:128, :], self.oddt[0:64, :])
                            return fin
                        return None
                    jobs.append((prep, attn))
        pending = None
        if jobs:
            jobs[0][0]()
        for i, (prep, attn) in enumerate(jobs):
            hook = jobs[i + 1][0] if i + 1 < len(jobs) else None
            pending = attn(hook, pending)
        if pending is not None:
            pending()

    def stage_outproj(self, l):
        fw = self.fw
        yT = [self.carve(A_Y + i * 4096, [128, T], BF16) for i in range(8)]
        wbr = [self.carve(A_Z2 + i * 2048, [128, 8, 128], BF16) for i in range(2)]
        wg = [self.carve(A_Z2 + 4096 + i * 2048, [128, 8, 128], BF16) for i in range(4)]
        Wl = self.inp["w_in"][l]
        ng = 0
        for fc in range(8):
            fs = slice(fc * 128, (fc + 1) * 128)
            wb = wbr[fc % 2]
            fw.dma("pool", wb[:, 0:3, :], self.inp["w_br_fox"][l].rearrange("(kc p) n -> p kc n", p=128)[:, :, fs])
            fw.dma("pool", wb[:, 3:6, :], self.inp["w_br_moba"][l].rearrange("(kc p) n -> p kc n", p=128)[:, :, fs])
            fw.dma("pool", wb[:, 6:8, :], self.inp["w_br_dil"][l].rearrange("(kc p) n -> p kc n", p=128)[:, :, fs])
            wgs = []
            for br in range(3):
                w = wg[ng % 4]
                ng += 1
                fw.dma("pool", w, self.wview(Wl, GOFF + br * 1024 + fc * 128, 128))
                wgs.append(w)
            for tt in range(4):
                cs = slice(tt * 512, (tt + 1) * 512)
                nt0, nt1 = self.ntmp[0], self.ntmp[1 + tt % 2]
                for br, (k0, nk) in enumerate(((0, 3), (3, 3), (6, 2))):
                    pz = self.psb[br]
                    for kc in range(nk):
                        fw.mm(pz[:, :], wb[:, k0 + kc, :], self.OT[k0 + kc][:, cs], start=(kc == 0), stop=(kc == nk - 1))
                    pg = self.psb[3 + br]
                    for kc in range(8):
                        fw.mm(pg[:, :], wgs[br][:, kc, :], self.hT[:, kc, cs], start=(kc == 0), stop=(kc == 7))
                    sg = self.sqb[br]
                    fw.act(sg[:], pg[:, :], AF.Sigmoid)
                    if br == 0:
                        fw.tt("dve", nt0[:], pz[:, :], sg[:], ALU.mult)
                    else:
                        fw.tt("dve", nt1[:], pz[:, :], sg[:], ALU.mult)
                        if br == 1:
                            fw.tt("pool", nt0[:], nt0[:], nt1[:], ALU.add)
                        else:
                            fw.tt("pool", yT[fc][:, cs], nt0[:], nt1[:], ALU.add)
        for fc in range(8):
            w = wg[ng % 4]
            ng += 1
            fw.dma("pool", w, self.wview(self.inp["w_out"][l], fc * 128, 128))
            for tt in range(4):
                cs = slice(tt * 512, (tt + 1) * 512)
                ps = self.psb[6 + tt % 2]
                for kc in range(8):
                    fw.mm(ps[:, :], w[:, kc, :], yT[kc][:, cs], start=(kc == 0), stop=(kc == 7))
                fw.stt(self.xT[:, fc, cs], ps[:, :], self.modT[:, l, 16 + fc:17 + fc], self.xT[:, fc, cs], ALU.mult, ALU.add)

    def ffn_expert(self, l, wg_ap, wu_ap, wd_ap, combbc):
        fw = self.fw
        actT = self.carve(A_ACT, [128, 11, T], BF16)
        wgu = [self.carve(A_WGU + i * 2048, [128, 8, 128], BF16) for i in range(4)]
        wdt = [self.carve(A_WD + i * 2816, [128, 11, 128], BF16) for i in range(2)]
        sgt = [self.carve(A_SG + i * 1024, [128, 512], BF16) for i in range(3)]
        if not hasattr(self, "_ffc"):
            self._ffc = 0
            self._wdc = 0
        for half in range(2):
            for fi in range(11):
                ffc = half * 11 + fi
                wgt = wgu[(2 * self._ffc) % 4]
                wut = wgu[(2 * self._ffc + 1) % 4]
                self._ffc += 1
                fw.dma("pool", wgt, self.wview(wg_ap, ffc * 128, 128))
                fw.dma("pool", wut, self.wview(wu_ap, ffc * 128, 128))
                for tt in range(4):
                    cs = slice(tt * 512, (tt + 1) * 512)
                    pg = self.psb[(2 * tt) % 4]
                    pu = self.psb[(2 * tt + 1) % 4]
                    for kc in range(8):
                        fw.mm(pg[:, :], wgt[:, kc, :], self.hT[:, kc, cs], start=(kc == 0), stop=(kc == 7))
                    for kc in range(8):
                        fw.mm(pu[:, :], wut[:, kc, :], self.hT[:, kc, cs], start=(kc == 0), stop=(kc == 7))
                    sg = sgt[tt % 3]
                    fw.act(sg, pg[:, :], AF.Silu)
                    if combbc is not None:
                        fw.tt("pool", sg, sg, combbc[:, cs], ALU.mult)
                    fw.tt("dve", actT[:, fi, cs], pu[:, :], sg, ALU.mult)
            for fc in range(8):
                wd = wdt[self._wdc % 2]
                self._wdc += 1
                fw.dma("pool", wd, wd_ap.rearrange("(f p) n -> p f n", p=128)[:, half * 11:(half + 1) * 11, fc * 128:(fc + 1) * 128])
                for tt in range(4):
                    cs = slice(tt * 512, (tt + 1) * 512)
                    ps = self.psb[4 + tt % 2]
                    for fi in range(11):
                        fw.mm(ps[:, :], wd[:, fi, :], actT[:, fi, cs], start=(fi == 0), stop=(fi == 10))
                    fw.stt(self.xT[:, fc, cs], ps[:, :], self.modT[:, l, 40 + fc:41 + fc], self.xT[:, fc, cs], ALU.mult, ALU.add)

    def stage_moe(self, l):
        fw, C = self.fw, self.C
        R0 = A_Z2 + 8192
        wr = self.carve(R0, [128, 8, 8], F32)
        wr2 = self.carve(R0 + 256, [128, 8, 8], F32)
        brt = self.carve(R0 + 512, [128, 8], F32)
        crow = self.carve(R0 + 544, [128, 8], F32)
        cb = self.carve(R0 + 576, [128, 8], F32)
        rt = self.carve(R0 + 608, [128, 16], F32)
        lg = self.carve(R0 + 1024, [128, 16, 8], F32)
        m8 = self.carve(R0 + 1536, [128, 16, 8], F32)
        eq = self.carve(R0 + 2048, [128, 16, 8], F32)
        comb = self.carve(R0 + 2560, [128, 16, 8], F32)
        w1 = self.carve(R0 + 3072, [128, 16], F32)
        w2 = self.carve(R0 + 3136, [128, 16], F32)
        e21 = self.carve(R0 + 3200, [128, 16], F32)
        fw.dma("sp", wr, self.inp["w_router"][0].rearrange("(kc p) e -> p kc e", p=128))
        fw.dma("sp", brt[0:1, :], self.inp["b_router"][0:1, :])
        for kc in range(8):
            fw.ts("dve", wr2[:, kc, :], wr[:, kc, :], self.a2[:, l, kc:kc + 1], None, op0=ALU.mult)
        pc = self.psb[2]
        for kc in range(8):
            fw.mm(pc[0:1, 0:8], self.modT[:, l, 24 + kc:25 + kc], wr[:, kc, :], start=(kc == 0), stop=(kc == 7))
        fw.tt("dve", crow[0:1, :], pc[0:1, 0:8], brt[0:1, :], ALU.add)
        pcb = self.psb[3]
        fw.mm(pcb[:, 0:8], C["ones_f"][0:1, :], crow[0:1, :], start=True, stop=True)
        fw.copy("dve", cb, pcb[:, 0:8])
        pl = self.psb[6]
        for t16 in range(16):
            for kc in range(8):
                fw.mm(pl[:, t16 * 8:(t16 + 1) * 8], self.xT[:, kc, t16 * 128:(t16 + 1) * 128], wr2[:, kc, :],
                      start=(kc == 0), stop=(kc == 7))
        fw.copy("dve", rt, self.psb[7][:, 0:16])
        fw.tt("dve", lg, pl[:, 0:128].rearrange("p (a b) -> p a b", b=8), rt.unsqueeze(2).to_broadcast([128, 16, 8]), ALU.mult)
        fw.tt("dve", lg, lg, cb.unsqueeze(1).to_broadcast([128, 16, 8]), ALU.add)
        for t16 in range(16):
            fw.gen("dve", "max", [lg[:, t16, :]], [m8[:, t16, :]], out=m8[:, t16, :], in_=lg[:, t16, :])
        fw.tt("dve", e21, m8[:, :, 1], m8[:, :, 0], ALU.subtract)
        fw.act(e21, e21, AF.Exp)
        fw.ts("dve", w1, e21, 1.0, None, op0=ALU.add)
        fw.gen("dve", "reciprocal", [w1], [w1], out=w1, in_=w1)
        fw.tt("dve", w2, e21, w1, ALU.mult)
        fw.tt("dve", eq, lg, m8[:, :, 0:1].to_broadcast([128, 16, 8]), ALU.is_equal)
        fw.tt("dve", comb, eq, w1.unsqueeze(2).to_broadcast([128, 16, 8]), ALU.mult)
        fw.tt("dve", eq, lg, m8[:, :, 1:2].to_broadcast([128, 16, 8]), ALU.is_equal)
        fw.tt("dve", eq, eq, w2.unsqueeze(2).to_broadcast([128, 16, 8]), ALU.mult)
        fw.tt("dve", comb, comb, eq, ALU.add)
        if "comb" in self.debug:
            o = self.dbg_out("comb", [128, 16, 8], F32)
            fw.dma("sp", o[:, :, :], comb)
        cbc = [self.carve(A_Z2 + i * 4096, [128, T], BF16) for i in range(2)]
        for e in range(NEXP):
            cc = cbc[e % 2]
            for tt in range(4):
                nt = self.ntmp[tt % 3]
                for i in range(4):
                    fw.ts("dve", nt[:, i * 128:(i + 1) * 128], C["ident_f"][:], comb[:, tt * 4 + i, e:e + 1], None, op0=ALU.mult)
                pb = self.psb[6 + tt % 2]
                fw.mm(pb[:, :], C["ones_f"][:], nt[:], start=True, stop=True)
                fw.copy("act", cc[:, tt * 512:(tt + 1) * 512], pb[:, :])
            self.ffn_expert(l, self.inp["w_exp_gate"][0, e], self.inp["w_exp_up"][0, e], self.inp["w_exp_down"][0, e], cc)

    def stage_output(self):
        fw, C = self.fw, self.C
        xo = [self.carve(i * 4096, [128, D], F32) for i in range(2)]
        for t16 in range(16):
            xb = xo[t16 % 2]
            for half in range(2):
                pb = self.psb[(t16 * 2 + half) % 4]
                for i in range(4):
                    dc = half * 4 + i
                    fw.tr(pb[:, i * 128:(i + 1) * 128], self.xT[:, dc, t16 * 128:(t16 + 1) * 128], C["ident_f"][:])
                fw.copy(self.evac_eng(), xb[:, half * 512:(half + 1) * 512], pb[:, :])
            fw.dma("sp", self.out[t16 * 128:(t16 + 1) * 128, :], xb)

    def stage_moe_sparse(self, l):
        fw, C, nc = self.fw, self.C, self.nc
        I32 = mybir.dt.int32
        R0 = A_Z2
        wr = self.carve(R0, [128, 8, 8], F32)
        wr2 = self.carve(R0 + 256, [128, 8, 8], F32)
        brt = self.carve(R0 + 512, [128, 8], F32)
        crow = self.carve(R0 + 544, [128, 8], F32)
        cb = self.carve(R0 + 576, [128, 8], F32)
        rt = self.carve(R0 + 608, [128, 16], F32)
        e21 = self.carve(R0 + 672, [128, 16], F32)
        lg = self.carve(R0 + 1024, [128, 16, 8], F32)
        m8 = self.carve(R0 + 1536, [128, 16, 8], F32)
        eq1 = self.carve(R0 + 2048, [128, 16, 8], F32)
        eq2 = self.carve(R0 + 2560, [128, 16, 8], F32)
        tot = self.carve(R0 + 3072, [128, 16, 8], F32)
        pre = self.carve(R0 + 3584, [128, 16, 8], F32)
        pos = self.carve(R0 + 4096, [128, 16, 8], F32)
        tmp = self.carve(R0 + 4608, [128, 16, 8], F32)
        maskb = self.carve(R0 + 5120, [128, 128], BF16)
        gf = self.carve(R0 + 5376, [128, 16], F32)
        cntf = self.carve(R0 + 5440, [128, 8], F32)
        w1 = self.sb("moe_w1", [128, 16], F32)[:, :]
        w2 = self.sb("moe_w2", [128, 16], F32)[:, :]
        gi = [self.sb("moe_gi%d" % k, [128, 16], I32)[:, :] for k in range(2)]
        cnti = self.sb("moe_cnt", [1, 8], I32)[:, :]
        fw.dma("sp", wr, self.inp["w_router"][0].rearrange("(kc p) e -> p kc e", p=128))
        fw.dma("sp", brt[0:1, :], self.inp["b_router"][0:1, :])
        for kc in range(8):
            fw.ts("dve", wr2[:, kc, :], wr[:, kc, :], self.a2[:, l, kc:kc + 1], None, op0=ALU.mult)
        pc = self.psb[2]
        for kc in range(8):
            fw.mm(pc[0:1, 0:8], self.modT[:, l, 24 + kc:25 + kc], wr[:, kc, :], start=(kc == 0), stop=(kc == 7))
        fw.tt("dve", crow[0:1, :], pc[0:1, 0:8], brt[0:1, :], ALU.add)
        pcb = self.psb[3]
        fw.mm(pcb[:, 0:8], C["ones_f"][0:1, :], crow[0:1, :], start=True, stop=True)
        fw.copy("dve", cb, pcb[:, 0:8])
        pl = self.psb[6]
        for t16 in range(16):
            for kc in range(8):
                fw.mm(pl[:, t16 * 8:(t16 + 1) * 8], self.xT[:, kc, t16 * 128:(t16 + 1) * 128], wr2[:, kc, :],
                      start=(kc == 0), stop=(kc == 7))
        fw.copy("dve", rt, self.psb[7][:, 0:16])
        bc3 = [128, 16, 8]
        fw.tt("dve", lg, pl[:, 0:128].rearrange("p (a b) -> p a b", b=8), rt.unsqueeze(2).to_broadcast(bc3), ALU.mult)
        fw.tt("dve", lg, lg, cb.unsqueeze(1).to_broadcast(bc3), ALU.add)
        for t16 in range(16):
            fw.gen("dve", "max", [lg[:, t16, :]], [m8[:, t16, :]], out=m8[:, t16, :], in_=lg[:, t16, :])
        fw.tt("dve", e21, m8[:, :, 1], m8[:, :, 0], ALU.subtract)
        fw.act(e21, e21, AF.Exp)
        fw.ts("dve", w1, e21, 1.0, None, op0=ALU.add)
        fw.gen("dve", "reciprocal", [w1], [w1], out=w1, in_=w1)
        fw.tt("dve", w2, e21, w1, ALU.mult)
        fw.tt("dve", eq1, lg, m8[:, :, 0:1].to_broadcast(bc3), ALU.is_equal)
        fw.tt("dve", eq2, lg, m8[:, :, 1:2].to_broadcast(bc3), ALU.is_equal)
        fl = lambda a: a.rearrange("p a b -> p (a b)")
        fw.tt("dve", maskb, fl(eq1), fl(eq2), ALU.add)
        ptot, pwi = self.psb[4], self.psb[5]
        fw.mm(ptot[:, 0:128], C["ones_b"][:], maskb, start=True, stop=True)
        for t16 in range(16):
            fw.mm(pwi[:, t16 * 8:(t16 + 1) * 8], C["ustrict"][:], maskb[:, t16 * 8:(t16 + 1) * 8], start=True, stop=True)
        fw.copy("dve", fl(tot), ptot[:, 0:128])
        fw.memset("dve", pre[:, 0, :], 0.0)
        for t16 in range(1, 16):
            fw.tt("dve", pre[:, t16, :], pre[:, t16 - 1, :], tot[:, t16 - 1, :], ALU.add)
        fw.tt("dve", cntf, pre[:, 15, :], tot[:, 15, :], ALU.add)
        fw.copy("dve", cnti[0:1, :], cntf[0:1, :])
        fw.tt("dve", fl(pos), pwi[:, 0:128], fl(pre), ALU.add)
        fw.tt("dve", fl(pos), fl(pos), C["ebase"][:], ALU.add)
        for k, eq in enumerate((eq1, eq2)):
            fw.tt("dve", tmp, pos, eq, ALU.mult)
            fw.gen("dve", "tensor_reduce", [tmp], [gf], out=gf, in_=tmp, axis=AX.X, op=ALU.add)
            fw.copy("dve", gi[k], gf)
        if "moeidx" in self.debug:
            o = self.dbg_out("gi0", [128, 16], I32)
            fw.dma("sp", o[:, :], gi[0][:])
            o = self.dbg_out("gi1", [128, 16], I32)
            fw.dma("sp", o[:, :], gi[1][:])
            o = self.dbg_out("cnt", [1, 8], I32)
            fw.dma("sp", o[:, :], cnti[:])
        rows = [[self.carve(k * 2056 + i * 4112, [128, 1028], BF16) for k in range(2)] for i in range(2)]
        xrow = [self.carve(16448 + i * 4096, [128, D], F32) for i in range(2)]
        wsrc = (w1, w2)
        for t16 in range(16):
            ts_ = slice(t16 * 128, (t16 + 1) * 128)
            ph = self.psb[t16 % 2].bitcast(BF16)
            for kc in range(8):
                fw.tr(ph[:, kc * 128:(kc + 1) * 128], self.hT[:, kc, ts_], C["ident_b"][:])
            for k in range(2):
                Rk = rows[t16 % 2][k]
                fw.copy("act" if k == 0 else "dve", Rk[:, 0:1024], ph[:, :])
                fw.copy("dve", Rk[:, 1024:1026].bitcast(F32), wsrc[k][:, t16:t16 + 1])
                g = nc.gpsimd
                idx = gi[k][:, t16:t16 + 1]
                src = Rk[:, 0:1026]
                fw.op("pool", (lambda src=src, idx=idx: g.indirect_dma_start(
                    out=self.HS[:, :], out_offset=bass.IndirectOffsetOnAxis(ap=idx, axis=0), in_=src, in_offset=None)),
                    reads=[src, idx], writes=[self.HS[:, :]], dma=True)
            xr = xrow[t16 % 2]
            for half in range(2):
                pb = self.psb[2 + half]
                for i in range(4):
                    dc = half * 4 + i
                    fw.tr(pb[:, i * 128:(i + 1) * 128], self.xT[:, dc, ts_], C["ident_f"][:])
                fw.copy("act" if half == 0 else "dve", xr[:, half * 512:(half + 1) * 512], pb[:, :])
            fw.dma("sp", self.XTOK[ts_, :], xr)
        E_WD, E_WGU, E_HTE, E_ACT, E_ACTT, E_G, E_YSL, E_SG, E_WS = 0, 22528, 56320, 89088, 134144, 139776, 143888, 147984, 149392
        wd = self.carve(E_WD, [128, 11, D], BF16)
        wgu = [self.carve(E_WGU + i * 5632, [128, 8, 352], BF16) for i in range(6)]
        hTe = self.carve(E_HTE, [128, 16, 1024], BF16)
        act_sl = self.carve(E_ACT, [128, 16, 1408], BF16)
        actT = [self.carve(E_ACTT + i * 2816, [128, 1408], BF16) for i in range(2)]
        G = [self.carve(E_G + i * 2056, [128, 1028], BF16) for i in range(2)]
        ysl = [self.carve(E_YSL + i * 2048, [128, D], BF16) for i in range(2)]
        sgt = [self.carve(E_SG + i * 704, [128, 352], BF16) for i in range(2)]
        ws = self.carve(E_WS, [128, 16], F32)
        y0 = self.hT.rearrange("p a b -> p (a b)").rearrange("p (c f) -> p c f", c=16)
        self._nw = 0
        for e in range(NEXP):
            key = "moe_e%d" % e

            for ce in fw.CENGS:
                def ld(key=key, e=e, ce=ce):
                    eo = fw.engobj[ce]
                    reg = eo.alloc_register("r_%s_%s" % (key, ce))
                    ins = eo.reg_load(reg, cnti[0:1, e:e + 1])
                    fw.cond_vals[(key, ce)] = eo.snap(reg)
                    return ins
                fw.op(ce, ld, reads=[cnti[0:1, e:e + 1]], writes=[])
            Wg, Wu, Wd = self.inp["w_exp_gate"][0, e], self.inp["w_exp_up"][0, e], self.inp["w_exp_down"][0, e]
            for c in range(16):
                Gc = G[c % 2]
                fw.dma("sp", Gc[:, 0:1026], self.HS[e * T + c * 128:e * T + (c + 1) * 128, :])
                fw.cur_cond = (key, c * 128)
                fw.copy("dve", ws[:, c:c + 1], Gc[:, 1024:1026].bitcast(F32))
                ph = self.psb[c % 2].bitcast(BF16)
                for kc in range(8):
                    fw.tr(ph[:, kc * 128:(kc + 1) * 128], Gc[:, kc * 128:(kc + 1) * 128], C["ident_b"][:])
                fw.copy("act", hTe[:, c, :], ph[:, :])
                fw.cur_cond = None
            fw.barrier()
            for half in range(2):
                tiles = {}

                def issue_cg(cg, half=half, tiles=tiles, Wg=Wg, Wu=Wu):
                    col0 = half * 1408 + cg * 352
                    wgt, wut = wgu[(2 * self._nw) % 6], wgu[(2 * self._nw + 1) % 6]
                    self._nw += 1
                    fw.dma("pool", wgt, self.wview(Wg, col0, 352))
                    fw.dma("pool", wut, self.wview(Wu, col0, 352))
                    tiles[cg] = (wgt, wut)
                issue_cg(0)
                issue_cg(1)
                issue_cg(2)
                fw.dma("pool", wd, Wd.rearrange("(f p) n -> p f n", p=128)[:, half * 11:(half + 1) * 11, :])
                for cg in range(4):
                    wgt, wut = tiles[cg]
                    for c in range(16):
                        pg, pu = self.psb[2 + 2 * (c % 2)], self.psb[3 + 2 * (c % 2)]
                        fw.cur_cond = (key, c * 128)
                        for kc in range(8):
                            fw.mm(pg[:, 0:352], hTe[:, c, kc * 128:(kc + 1) * 128], wgt[:, kc, :], start=(kc == 0), stop=(kc == 7))
                        for kc in range(8):
                            fw.mm(pu[:, 0:352], hTe[:, c, kc * 128:(kc + 1) * 128], wut[:, kc, :], start=(kc == 0), stop=(kc == 7))
                        sg = sgt[c % 2]
                        fw.act(sg, pg[:, 0:352], AF.Silu)
                        fw.stt(act_sl[:, c, cg * 352:(cg + 1) * 352], pu[:, 0:352], ws[:, c:c + 1], sg, ALU.mult, ALU.mult)
                        fw.cur_cond = None
                    fw.barrier()
                    if cg + 3 < 4:
                        issue_cg(cg + 3)
                for c in range(16):
                    at = actT[c % 2]
                    pa, pb2 = self.psb[0].bitcast(BF16), self.psb[1].bitcast(BF16)
                    fw.cur_cond = (key, c * 128)
                    for f in range(11):
                        dstp = pa[:, f * 128:(f + 1) * 128] if f < 8 else pb2[:, (f - 8) * 128:(f - 7) * 128]
                        fw.tr(dstp, act_sl[:, c, f * 128:(f + 1) * 128], C["ident_b"][:])
                    fw.copy("act", at[:, 0:1024], pa[:, :])
                    fw.copy("dve", at[:, 1024:1408], pb2[:, 0:384])
                    py = (self.psb[6], self.psb[7])
                    for fo in range(2):
                        for f in range(11):
                            fw.mm(py[fo][:, :], at[:, f * 128:(f + 1) * 128], wd[:, f, fo * 512:(fo + 1) * 512],
                                  start=(f == 0), stop=(f == 10))
                    if half == 0:
                        fw.copy("act", y0[:, c, 0:512], py[0][:, :])
                        fw.copy("dve", y0[:, c, 512:1024], py[1][:, :])
                    else:
                        yb = ysl[c % 2]
                        fw.tt("dve", yb[:, 0:512], py[0][:, :], y0[:, c, 0:512], ALU.add)
                        fw.tt("dve", yb[:, 512:1024], py[1][:, :], y0[:, c, 512:1024], ALU.add)
                    fw.cur_cond = None
                    if half == 1:
                        fw.dma("sp", self.YS[e * T + c * 128:e * T + (c + 1) * 128, :], ysl[c % 2])
                fw.barrier()
        g2bc = self.carve(0, [128, D], F32)
        dg = self.carve(4096, [128, D], F32)
        for dc in range(8):
            fw.ts("dve", dg[:, dc * 128:(dc + 1) * 128], C["ident_f"][:], self.modT[:, l, 40 + dc:41 + dc], None, op0=ALU.mult)
        for half in range(2):
            pb = self.psb[2 + half]
            fw.mm(pb[:, :], C["ones_f"][:], dg[:, half * 512:(half + 1) * 512], start=True, stop=True)
            fw.copy("act", g2bc[:, half * 512:(half + 1) * 512], pb[:, :])
        ya = [[self.carve(8192 + (2 * i + k) * 2048, [128, D], BF16) for k in range(2)] for i in range(2)]
        ysum = [self.carve(16384 + i * 4096, [128, D], F32) for i in range(2)]
        xo = [self.carve(24576 + i * 4096, [128, D], F32) for i in range(2)]
        for t16 in range(16):
            ts_ = slice(t16 * 128, (t16 + 1) * 128)
            g = nc.gpsimd
            for k in range(2):
                dst = ya[t16 % 2][k]
                idx = gi[k][:, t16:t16 + 1]
                fw.op("pool", (lambda dst=dst, idx=idx: g.indirect_dma_start(
                    out=dst, out_offset=None, in_=self.YS[:, :], in_offset=bass.IndirectOffsetOnAxis(ap=idx, axis=0))),
                    reads=[self.YS[:, :], idx], writes=[dst], dma=True)
            xb = xo[t16 % 2]
            fw.dma("sp", xb, self.XTOK[ts_, :])
            y1, y2 = ya[t16 % 2]
            ysm = ysum[t16 % 2]
            fw.tt("dve", ysm, y1, y2, ALU.add)
            fw.tt("pool", ysm, ysm, g2bc, ALU.mult)
            fw.tt("dve", xb, xb, ysm, ALU.add)
            fw.dma("sp", self.out[ts_, :], xb)


_CACHE = {}


def kernel(**inputs):
    inputs = {k: np.asarray(v) for k, v in inputs.items()}
    k = K()
    in_maps = []
    for b in range(8):
        m = host_layout(inputs, b)
        for kk, v in k.hc.items():
            m["c_" + kk] = v
        in_maps.append(m)
    res = run_bass_kernel_spmd(k.nc, in_maps, core_ids=list(range(8)))
    out = np.stack([np.asarray(res.results[b]["out"]) for b in range(8)], axis=0)
    return out.astype(np.float32)
```

```python
from concourse.bass_utils import run_bass_kernel_spmd
import numpy as np
import concourse.bass as bass
import concourse.mybir as mybir

F32 = mybir.dt.float32
BF16 = mybir.dt.bfloat16
AF = mybir.ActivationFunctionType
ALU = mybir.AluOpType
AX = mybir.AxisListType

_DTSIZE = {}


def dtsize(dt):
    if dt not in _DTSIZE:
        _DTSIZE[dt] = np.dtype(mybir.dt.np(dt)).itemsize
    return _DTSIZE[dt]


class Rec:
    __slots__ = ("eng", "fn", "deps", "dma", "sig", "idx", "gidx", "dmasem", "dmaval", "vc", "cond", "sv")


class FW:
    ENGS = ("pe", "act", "dve", "pool", "sp")
    CENGS = ("pe", "act", "dve")

    def __init__(self, nc, n_dma_sems=24):
        self.nc = nc
        self.recs = []
        self.eng_recs = {e: [] for e in self.ENGS}
        self.hist = {}
        self.engobj = {"pe": nc.tensor, "act": nc.scalar, "dve": nc.vector, "pool": nc.gpsimd, "sp": nc.sync}
        self.n_dma_sems = n_dma_sems
        self.dma_count = {e: 0 for e in self.ENGS}
        self.dma_last = {}
        self.cur_cond = None
        self.cond_vals = {}

    def region(self, ap):
        t = ap.tensor
        name = t.name
        space = str(ap.space) if hasattr(ap, "space") else ""
        esz = dtsize(ap.dtype)
        apl = list(ap.ap)
        off = ap.offset
        is_dram = "DRAM" in space.upper() or "HBM" in space.upper() or type(t).__name__.startswith("DRam")
        if is_dram:
            lo = off
            hi = off
            for st, cnt in apl:
                if cnt > 1:
                    if st >= 0:
                        hi += st * (cnt - 1)
                    else:
                        lo += st * (cnt - 1)
            return (name, 0, 1, lo * esz, (hi + 1) * esz, False)
        tsz = dtsize(t.dtype)
        pstride = 1
        for s in t.shape[1:]:
            pstride *= s
        if esz != tsz:
            pstride = pstride * tsz // esz
        p0 = off // pstride
        f0 = off % pstride
        pst, pcnt = apl[0]
        if pst == 0:
            pcnt = 1
        p1 = p0 + pcnt
        lo = f0
        hi = f0
        for st, cnt in apl[1:]:
            if cnt > 1:
                if st >= 0:
                    hi += st * (cnt - 1)
                else:
                    lo += st * (cnt - 1)
        b0, b1 = lo * esz, (hi + 1) * esz
        is_psum = type(t).__name__.startswith("PSum")
        if is_psum:
            b0 = (b0 // 2048) * 2048
            b1 = ((b1 + 2047) // 2048) * 2048
            p0, p1 = 0, 128
        return (name, p0, p1, b0, b1, is_psum)

    def op(self, eng, fn, reads=(), writes=(), dma=False):
        r = Rec()
        r.cond = self.cur_cond if eng in self.CENGS else None
        r.sv = None
        r.eng = eng
        r.fn = fn
        r.dma = dma
        r.sig = False
        r.deps = []
        r.idx = len(self.eng_recs[eng])
        r.gidx = len(self.recs)
        r.dmasem = None
        deps = set()
        for ap in reads:
            self._access(r, self.region(ap), False, deps)
        for ap in writes:
            self._access(r, self.region(ap), True, deps)
        if dma:
            slot = self.dma_count[eng] % self.n_dma_sems
            self.dma_count[eng] += 1
            prev = self.dma_last.get((eng, slot))
            if prev is not None:
                deps.add(prev)
            self.dma_last[(eng, slot)] = r
            r.dmasem = slot
        r.deps = sorted(deps, key=lambda d: d.gidx)
        self.recs.append(r)
        self.eng_recs[eng].append(r)
        return r

    def _access(self, r, reg, is_write, deps):
        name, p0, p1, b0, b1, is_psum = reg
        lst = self.hist.setdefault(name, [])
        excl = is_write or is_psum
        keep = []
        for ent in lst:
            ep0, ep1, eb0, eb1, w, rds, wtrue = ent
            if ep1 <= p0 or p1 <= ep0 or eb1 <= b0 or b1 <= eb0:
                keep.append(ent)
                continue
            inside = ep0 >= p0 and ep1 <= p1 and eb0 >= b0 and eb1 <= b1
            if w is not None and w is not r:
                if w.dma or r.dma or w.eng != r.eng:
                    deps.add(w)
                elif wtrue and (not is_write) and r.eng != "pe":
                    deps.add(w)
            if excl:
                for rd in rds:
                    if rd is r:
                        continue
                    if rd.dma or r.dma or rd.eng != r.eng:
                        deps.add(rd)
                if inside:
                    continue
            else:
                if w is None and inside and len(rds) == 1 and (not rds[0].dma) and (not r.dma) and rds[0].eng == r.eng:
                    continue
            keep.append(ent)
        if excl:
            keep.append([p0, p1, b0, b1, r, [], is_write])
        else:
            keep.append([p0, p1, b0, b1, None, [r], False])
        self.hist[name] = keep

    def barrier(self, engs=("pe", "act", "dve")):
        lasts = {e: self.eng_recs[e][-1] for e in engs if self.eng_recs[e]}
        for e in engs:
            eo = self.engobj[e]
            r = self.op(e, (lambda eo=eo: eo.drain()), reads=[], writes=[])
            extra = [lasts[o] for o in engs if o != e and o in lasts]
            r.deps = sorted(set(r.deps) | set(extra), key=lambda d: d.gidx)

    def emit(self):
        nc = self.nc
        ne = len(self.ENGS)
        eidx = {e: i for i, e in enumerate(self.ENGS)}
        grp_of = {}
        groups = []
        for ce in self.CENGS:
            prev = None
            for r in self.eng_recs[ce]:
                if r.cond is None:
                    prev = None
                    continue
                if prev is not None and prev.cond is not None and prev.cond[0] == r.cond[0] and prev.cond[1] <= r.cond[1] \
                        and prev.idx == r.idx - 1:
                    groups[-1].append(r)
                else:
                    groups.append([r])
                grp_of[r.gidx] = len(groups) - 1
                prev = r
        clock = {e: [-1] * ne for e in self.ENGS}
        dma_known = {e: set() for e in self.ENGS}
        final_deps = []
        cur_grp = {e: None for e in self.CENGS}
        saved = {e: None for e in self.CENGS}
        for r in self.recs:
            if r.eng in self.CENGS:
                g = grp_of.get(r.gidx)
                if g != cur_grp[r.eng]:
                    if cur_grp[r.eng] is not None:
                        clock[r.eng] = saved[r.eng][0]
                        dma_known[r.eng] = saved[r.eng][1]
                    if g is not None:
                        saved[r.eng] = (list(clock[r.eng]), set(dma_known[r.eng]))
                    cur_grp[r.eng] = g
            ck = clock[r.eng]
            need = []
            for d in r.deps:
                if d.dma:
                    if d.gidx in dma_known[r.eng]:
                        continue
                    need.append(d)
                else:
                    if ck[eidx[d.eng]] >= d.idx:
                        continue
                    need.append(d)
            best = {}
            nd = []
            for d in need:
                if d.dma:
                    nd.append(d)
                else:
                    if d.eng not in best or best[d.eng].idx < d.idx:
                        best[d.eng] = d
            nd.extend(best.values())
            for d in nd:
                d.sig = True
                if d.dma:
                    dma_known[r.eng].add(d.gidx)
                    dvc = d.vc
                else:
                    dvc = list(d.vc)
                    dvc[eidx[d.eng]] = max(dvc[eidx[d.eng]], d.idx)
                for i in range(ne):
                    if dvc[i] > ck[i]:
                        ck[i] = dvc[i]
            if r.eng in self.CENGS and cur_grp[r.eng] is not None:
                r.vc = list(saved[r.eng][0])
            else:
                r.vc = list(ck)
            final_deps.append(nd)
        cnt = {e: 0 for e in self.ENGS}
        dcnt = {}
        for r in self.recs:
            if r.dma:
                key = (r.eng, r.dmasem)
                dcnt[key] = dcnt.get(key, 0) + 16
                r.dmaval = dcnt[key]
            elif r.sig:
                cnt[r.eng] += 1
                r.sv = cnt[r.eng]
        sems = {}
        for e in self.ENGS:
            sems[e] = nc.alloc_semaphore("sem_" + e)
        dsems = {}
        for e in self.ENGS:
            if self.dma_count[e] > 0:
                dsems[e] = [nc.alloc_semaphore("dsem_%s_%d" % (e, i)) for i in range(self.n_dma_sems)]
        self.nwait = 0

        def emit_one(r, nd):
            eo = self.engobj[r.eng]
            for d in nd:
                if d.dma:
                    eo.wait_ge(dsems[d.eng][d.dmasem], d.dmaval)
                else:
                    eo.wait_ge(sems[d.eng], d.sv)
                self.nwait += 1
            ins = r.fn()
            if r.dma:
                ins.then_inc(dsems[r.eng][r.dmasem], 16)
            elif r.sig:
                ins.then_inc(sems[r.eng], 1)

        def emit_group(recs_):
            eng = recs_[0].eng
            eo = self.engobj[eng]
            key = recs_[0].cond[0]
            val = self.cond_vals[(key, eng)]
            levels = []
            for r in recs_:
                if not levels or levels[-1][0] != r.cond[1]:
                    levels.append((r.cond[1], []))
                levels[-1][1].append(r)

            def rec_level(li):
                if li == len(levels):
                    return
                c, rs = levels[li]
                nsig = sum(1 for lv in levels[li:] for r in lv[1] if r.sig)
                with eo.If(val > c):
                    for r in rs:
                        emit_one(r, final_deps[r.gidx])
                    rec_level(li + 1)
                with eo.Else():
                    if nsig > 0:
                        eo.drain()
                        eo.sem_inc(sems[eng], nsig)
            rec_level(0)

        done = set()
        for r, nd in zip(self.recs, final_deps):
            if r.gidx in done:
                continue
            g = grp_of.get(r.gidx)
            if g is not None:
                emit_group(groups[g])
                for x in groups[g]:
                    done.add(x.gidx)
            else:
                emit_one(r, nd)
        for (e, slot), r in self.dma_last.items():
            self.engobj[e].wait_ge(dsems[e][slot], r.dmaval)
        self.stats = dict(n=len(self.recs), waits=self.nwait, per_eng={e: len(v) for e, v in self.eng_recs.items()},
                          ngroups=len(groups))
        return self.stats

    def dma(self, eng, out, in_, **kw):
        o = self.engobj[eng]
        return self.op(eng, lambda: o.dma_start(out=out, in_=in_, **kw), reads=[in_], writes=[out], dma=True)

    def mm(self, out, lhsT, rhs, start=True, stop=True, **kw):
        t = self.nc.tensor
        return self.op("pe", lambda: t.matmul(out, lhsT, rhs, start=start, stop=stop, **kw), reads=[lhsT, rhs], writes=[out])

    def tr(self, out, in_, ident):
        t = self.nc.tensor
        return self.op("pe", lambda: t.transpose(out, in_, ident), reads=[in_, ident], writes=[out])

    def act(self, out, in_, func, bias=None, scale=None, accum_out=None):
        s = self.nc.scalar
        kw = {}
        rd = [in_]
        if bias is not None:
            kw["bias"] = bias
            if not isinstance(bias, (int, float)):
                rd.append(bias)
        if scale is not None:
            kw["scale"] = scale
            if not isinstance(scale, (int, float)):
                rd.append(scale)
        wr = [out]
        if accum_out is not None:
            kw["accum_out"] = accum_out
            wr.append(accum_out)
        return self.op("act", lambda: s.activation(out=out, in_=in_, func=func, **kw), reads=rd, writes=wr)

    def _veng(self, eng):
        return self.engobj[eng]

    def tt(self, eng, out, in0, in1, op):
        e = self._veng(eng)
        return self.op(eng, lambda: e.tensor_tensor(out=out, in0=in0, in1=in1, op=op), reads=[in0, in1], writes=[out])

    def ts(self, eng, out, in0, s1, s2=None, op0=ALU.mult, op1=None):
        e = self._veng(eng)
        rd = [in0]
        for s in (s1, s2):
            if s is not None and not isinstance(s, (int, float)):
                rd.append(s)
        kw = {}
        if op1 is not None:
            kw["op1"] = op1
        return self.op(eng, lambda: e.tensor_scalar(out=out, in0=in0, scalar1=s1, scalar2=s2, op0=op0, **kw), reads=rd, writes=[out])

    def stt(self, out, in0, scalar, in1, op0, op1, eng="dve"):
        e = self._veng(eng)
        rd = [in0, in1]
        if not isinstance(scalar, (int, float)):
            rd.append(scalar)
        return self.op(eng, lambda: e.scalar_tensor_tensor(out=out, in0=in0, scalar=scalar, in1=in1, op0=op0, op1=op1), reads=rd, writes=[out])

    def copy(self, eng, out, in_):
        if eng == "act":
            s = self.nc.scalar
            return self.op("act", lambda: s.copy(out=out, in_=in_), reads=[in_], writes=[out])
        e = self._veng(eng)
        return self.op(eng, lambda: e.tensor_copy(out=out, in_=in_), reads=[in_], writes=[out])

    def memset(self, eng, out, val):
        e = self._veng(eng)
        return self.op(eng, lambda: e.memset(out, val), reads=[], writes=[out])


def _fw_generic(self, eng, name, reads, writes, *args, **kw):
    e = self.engobj[eng]
    f = getattr(e, name)
    return self.op(eng, lambda: f(*args, **kw), reads=reads, writes=writes)


FW.gen = _fw_generic


import numpy as np
import ml_dtypes

D = 1024
T = 2048
NH = 24
DFF = 2816
NFF = 22
NEXP = 8
IN_COLS = 7686
QOFF, KOFF, VOFF, FOFF, GOFF = 0, 1536, 3072, 4608, 4614
EPS = 1e-6
SLOPES = (2.0 ** (-8.0 * np.arange(1, 19) / 18)).astype(np.float32)
DIL = ((128, 1), (512, 4), (2048, 16))
DIL_SO = (0, 4, 14)
MOBA_SO = 8
NEGM = -30000.0


def split3(v):
    v = np.asarray(v, np.float32)
    hi = v.astype(ml_dtypes.bfloat16)
    r1 = v - hi.astype(np.float32)
    mid = r1.astype(ml_dtypes.bfloat16)
    r2 = r1 - mid.astype(np.float32)
    lo = r2.astype(ml_dtypes.bfloat16)
    return hi, mid, lo


def host_consts():
    c = {}
    c["ident_f"] = np.eye(128, dtype=np.float32)
    c["ident_b"] = np.eye(128, dtype=np.float32).astype(ml_dtypes.bfloat16)
    ob = np.zeros((128, 128), np.float32)
    ob[0:64, 0:64] = 1.0
    c["onesblk"] = ob.astype(ml_dtypes.bfloat16)
    c["ones_b"] = np.ones((128, 128), np.float32).astype(ml_dtypes.bfloat16)
    c["ones_f"] = np.ones((128, 128), np.float32)
    k = np.arange(128)[:, None]
    q = np.arange(128)[None, :]
    c["tri"] = (q >= k).astype(np.float32).astype(ml_dtypes.bfloat16)
    db = np.zeros((128, 12, 256), np.float32)
    for g, (w, d) in enumerate(DIL):
        for j in range(4):
            sl = SLOPES[DIL_SO[g] + j]
            left = np.where(q >= k, -sl * d * (q - k), NEGM)
            right = np.where(k >= q, -sl * d * (128 + q - k), NEGM)
            db[:, g * 4 + j, 0:128] = left
            db[:, g * 4 + j, 128:256] = right
    c["dilbias"] = db.reshape(128, 12 * 256)
    t = np.arange(T, dtype=np.float32)
    aq = np.zeros((6, 6, T), ml_dtypes.bfloat16)
    ak = np.zeros((6, 6, T), ml_dtypes.bfloat16)
    for h in range(6):
        sl = SLOPES[MOBA_SO + h]
        qh = split3(-8.0 * sl * t)
        kh = split3(8.0 * sl * t)
        for i in range(3):
            aq[h, i] = qh[i]
            aq[h, 3 + i] = 1.0
            ak[h, i] = 1.0
            ak[h, 3 + i] = kh[i]
    c["moba_aq"] = aq
    c["moba_ak"] = ak
    bi = np.zeros((8, T), np.float32)
    for b in range(8):
        bi[b, b * 256:(b + 1) * 256] = 1.0
    c["blkind"] = bi.astype(ml_dtypes.bfloat16)
    pos = (np.arange(16)[None, :, None] * 128 + np.arange(128)[:, None, None])
    own = pos // 256
    B = np.arange(8)[None, None, :]
    c["mg_sb1"] = np.where(B < own, 0.0, -1e30).astype(np.float32).reshape(128, 128)
    c["mg_v1"] = (B < own).astype(np.float32).reshape(128, 128)
    c["mg_e"] = (B == own).astype(np.float32).reshape(128, 128)
    c["fox_ones"] = np.ones((3, T), np.float32).astype(ml_dtypes.bfloat16)
    c["ustrict"] = np.triu(np.ones((128, 128), np.float32), 1).astype(ml_dtypes.bfloat16)
    c["ebase"] = np.tile(np.arange(8, dtype=np.float32) * 2048.0, (128, 16))
    return c


CONST_DT = dict(ident_f=F32, ident_b=BF16, onesblk=BF16, ones_b=BF16, ones_f=F32, tri=BF16, dilbias=F32,
                moba_aq=BF16, moba_ak=BF16, blkind=BF16, mg_sb1=F32, mg_v1=F32, mg_e=F32, fox_ones=BF16, ustrict=BF16, ebase=F32)


def host_layout(inp, b):
    m = {}
    m["x"] = np.ascontiguousarray(inp["x"][b])
    m["cT"] = np.ascontiguousarray(inp["c"][b].reshape(8, 128).T)
    m["b_adaT"] = np.ascontiguousarray(inp["b_ada"].reshape(2, 48, 128).transpose(2, 0, 1))
    m["nmixT"] = np.ascontiguousarray(inp["norm_mix"].reshape(2, 8, 128).transpose(2, 0, 1))
    m["nffnT"] = np.ascontiguousarray(inp["norm_ffn"].reshape(2, 8, 128).transpose(2, 0, 1))
    m["qgT"] = np.ascontiguousarray(inp["q_gain"].transpose(2, 0, 1))
    m["kgT"] = np.ascontiguousarray(inp["k_gain"].transpose(2, 0, 1))
    m["bfg"] = np.ascontiguousarray(inp["b_fgate"].T)
    m["b_router"] = np.ascontiguousarray(inp["b_router"])
    for k in ("w_ada", "w_in", "w_br_fox", "w_br_moba", "w_br_dil", "w_out", "w_ffn_gate", "w_ffn_up",
              "w_ffn_down", "w_router", "w_exp_gate", "w_exp_up", "w_exp_down"):
        m[k] = inp[k]
    return m


IN_SHAPES = dict(
    x=[T, D], cT=[128, 8], b_adaT=[128, 2, 48], nmixT=[128, 2, 8], nffnT=[128, 2, 8], qgT=[64, 2, 24], kgT=[64, 2, 24],
    bfg=[6, 2], b_router=[1, 8], w_ada=[2, D, 6 * D], w_in=[2, D, IN_COLS], w_br_fox=[2, 384, D], w_br_moba=[2, 384, D],
    w_br_dil=[2, 256, D], w_out=[2, D, D], w_ffn_gate=[1, D, DFF], w_ffn_up=[1, D, DFF], w_ffn_down=[1, DFF, D],
    w_router=[1, D, 8], w_exp_gate=[1, 8, D, DFF], w_exp_up=[1, 8, D, DFF], w_exp_down=[1, 8, DFF, D])


def prod(xs):
    r = 1
    for x in xs:
        r *= x
    return r


A_OT = 0
A_Y = 32768
A_VA = 32768
A_VB = 32768 + 12544
A_QK = 57856
A_X = 65536
A_WQK = 74240
A_WV = 82432
A_ACCN = 88576
A_ODD = 96768
A_PT = 100864
A_Q2 = 103936
A_RQ = 105984
A_RD = 110080
A_BC = 112128
A_MG = 114176
A_KM = 118272
A_FST = 65536
A_Z = 131072
A_DILM = 131072
A_Z2 = 137216
A_END = 151552
A_ACT = 0
A_WGU = 45056
A_WD = 53248
A_SG = 58880
A_CBC = A_Z2
A_RT = A_Z2 + 4096


class K:
    def __init__(self, debug=None, nlayers=2, stop_after=None, heads=None, sparse=True):
        self.debug = debug or []
        self.nlayers = nlayers
        self.stop_after = stop_after
        self.heads = heads
        nc = bass.Bass("TRN2", target_bir_lowering=False)
        self.nc = nc
        self.fw = FW(nc)
        self.inp = {}
        for k, shp in IN_SHAPES.items():
            self.inp[k] = nc.dram_tensor(k, shp, F32, kind="ExternalInput").ap()
        self.cst = {}
        hc = host_consts()
        self.hc = hc
        for k, v in hc.items():
            self.cst[k] = nc.dram_tensor("c_" + k, list(v.shape), CONST_DT[k], kind="ExternalInput").ap()
        self.out = nc.dram_tensor("out", [T, D], F32, kind="ExternalOutput").ap()
        self.xs = nc.dram_tensor("xs_scr", [128, 8 * T], F32, kind="Internal").ap()
        self.fsc = nc.dram_tensor("f_scr", [2, 6, 3 * T], BF16, kind="Internal").ap()
        self.HS = nc.dram_tensor("hs_scr", [NEXP * T, 1026], BF16, kind="Internal").ap()
        self.YS = nc.dram_tensor("ys_scr", [NEXP * T, D], BF16, kind="Internal").ap()
        self.XTOK = nc.dram_tensor("xtok_scr", [T, D], F32, kind="Internal").ap()
        self.sparse = sparse
        self.dbg = {}
        self._rr = 0
        self._cnt = 0
        self.build()

    def sb(self, name, shape, dt):
        return self.nc.alloc_sbuf_tensor("s_" + name, shape, dt)

    def carve(self, off, shape, dt):
        n = prod(shape[1:])
        nb = n * dtsize(dt)
        assert off % 4 == 0 and off + nb <= A_END, (off, nb)
        v = self.AR[0:shape[0], off // 2:(off + nb) // 2]
        if dt != BF16:
            v = v.bitcast(dt)
        if len(shape) == 3:
            v = v.rearrange("p (a b) -> p a b", a=shape[1])
        elif len(shape) == 4:
            v = v.rearrange("p (a b c) -> p a b c", a=shape[1], b=shape[2])
        return v

    def dbg_out(self, name, shape, dt=F32):
        t = self.nc.dram_tensor("dbg_" + name, shape, dt, kind="ExternalOutput").ap()
        self.dbg[name] = t
        return t

    def evac_eng(self):
        self._rr += 1
        return "act" if self._rr % 2 else "dve"

    def wview(self, w2d, c0, ncols):
        return w2d.rearrange("(kc p) n -> p kc n", p=128)[:, :, c0:c0 + ncols]

    def build(self):
        nc, fw = self.nc, self.fw
        C = {}
        for k in ("ident_f", "ident_b", "onesblk", "ones_b", "ones_f", "tri", "mg_sb1", "mg_v1", "mg_e", "ustrict", "ebase"):
            v = self.hc[k]
            C[k] = self.sb("k_" + k, list(v.shape), CONST_DT[k])
            fw.dma("sp", C[k][:], self.cst[k][:, :])
        self.C = C
        self.cT = self.sb("cT", [128, 8], F32)
        fw.dma("sp", self.cT[:], self.inp["cT"][:, :])
        self.b_adaT = self.sb("b_adaT", [128, 2, 48], F32)
        fw.dma("sp", self.b_adaT[:], self.inp["b_adaT"][:, :, :])
        self.nmixT = self.sb("nmixT", [128, 2, 8], F32)
        fw.dma("sp", self.nmixT[:], self.inp["nmixT"][:, :, :])
        self.nffnT = self.sb("nffnT", [128, 2, 8], F32)
        fw.dma("sp", self.nffnT[:], self.inp["nffnT"][:, :, :])
        self.qg = self.sb("qg", [64, 2, 24], F32)
        fw.dma("sp", self.qg[:], self.inp["qgT"][:, :, :])
        self.kg = self.sb("kg", [64, 2, 24], F32)
        fw.dma("sp", self.kg[:], self.inp["kgT"][:, :, :])
        self.bfg = self.sb("bfg", [6, 2], F32)
        fw.dma("sp", self.bfg[:], self.inp["bfg"][:, :])
        self.negb = self.sb("negb", [6, 2], F32)
        fw.ts("dve", self.negb[:], self.bfg[:], -1.0, None, op0=ALU.mult)
        self.AR = self.sb("arena", [128, A_END // 2], BF16)
        self.hT = self.sb("hT", [128, 8, T], BF16)
        self.xT = self.carve(A_X, [128, 8, T], F32)
        self.psb = [nc.alloc_psum_tensor("psb%d" % i, [128, 512], F32) for i in range(8)]
        self.cond = self.sb("cond", [128, 8], BF16)
        self.modT = self.sb("modT", [128, 2, 48], F32)
        self.a1 = self.sb("a1", [128, 2, 8], F32)
        self.a2 = self.sb("a2", [128, 2, 8], F32)
        self.sqb = [self.sb("sqb%d" % i, [128, 512], BF16) for i in range(3)]
        self.rstd = [self.sb("rstd%d" % i, [128, 512], F32) for i in range(2)]
        self.ntmp = [self.sb("ntmp%d" % i, [128, 512], F32) for i in range(3)]
        self.OT = [self.carve(A_OT + i * 4096, [128, T], BF16) for i in range(8)]
        self.VA = self.carve(A_VA, [128, 16, 6, 65], BF16)
        self.VB = self.carve(A_VB, [128, 16, 6, 65], BF16)
        self.Qb = [self.carve(A_QK + (2 * s) * 4096, [128, T], BF16) for s in range(2)]
        self.Kb = [self.carve(A_QK + (2 * s + 1) * 4096, [128, T], BF16) for s in range(2)]
        self.wqk = [self.carve(A_WQK + s * 4096, [128, 8, 256], BF16) for s in range(2)]
        self.wv = self.carve(A_WV, [128, 8, 384], BF16)
        self.accN = self.carve(A_ACCN, [128, T], F32)
        self.oddt = self.carve(A_ODD, [128, T], BF16)
        self.ptb = [self.carve(A_PT + i * 1024, [128, 512], BF16) for i in range(3)]
        self.q2b = [self.carve(A_Q2 + i * 1024, [128, 512], BF16) for i in range(2)]
        self.rqb = [self.carve(A_RQ + i * 2048, [128, 512], F32) for i in range(2)]
        self.rd = self.carve(A_RD, [128, 512], F32)
        self.bcs = self.carve(A_BC, [128, 512], F32)
        self.dilM = self.carve(A_DILM, [128, 12, 256], BF16)
        self.rbt4 = [self.carve(119296 + i * 1024, [128, 512], BF16) for i in range(4)]
        self.rbt = self.rbt4[0:2]
        self.rlt = [self.carve(119296 + 4096 + i * 2048, [128, 512], F32) for i in range(2)]

        self.stage_load_x()
        self.stage_adaln()
        for l in range(self.nlayers):
            self.stage_rmsnorm(l, 0)
            if self.stop_after == "norm1":
                break
            fw.dma("sp", self.xs[:, :], self.xT.rearrange("p a b -> p (a b)"))
            if l == 0:
                self.stage_dilmask()
            self.stage_attention(l)
            if self.stop_after == "attn":
                break
            fw.dma("sp", self.xT.rearrange("p a b -> p (a b)"), self.xs[:, :])
            self.stage_outproj(l)
            if self.stop_after == "outproj":
                break
            self.stage_rmsnorm(l, 1, rt_ps=(self.psb[7] if l % 2 == 1 else None))
            if l % 2 == 0:
                self.ffn_expert(l, self.inp["w_ffn_gate"][0], self.inp["w_ffn_up"][0], self.inp["w_ffn_down"][0], None)
                if self.sparse and self.stop_after is None and l == 0:
                    zt = self.carve(A_VB, [128, 4104], BF16)
                    fw.memset("pool", zt, 0.0)
                    hsv = self.HS.rearrange("(p r) c -> p (r c)", p=128)
                    for i in range(32):
                        fw.dma("sp", hsv[:, i * 4104:(i + 1) * 4104], zt)
            elif self.sparse and l == self.nlayers - 1 and self.stop_after is None:
                self.stage_moe_sparse(l)
                self.final_done = True
            else:
                self.stage_moe(l)
            if self.stop_after == "ffn":
                break
        if self.stop_after is None and not getattr(self, "final_done", False):
            self.stage_output()
        for nm in self.debug:
            if nm == "hT":
                o = self.dbg_out("hT", [128, 8, T], BF16)
                fw.dma("sp", o[:, :, :], self.hT[:])
            elif nm == "xT":
                o = self.dbg_out("xT", [128, 8, T], F32)
                fw.dma("sp", o[:, :, :], self.xT)
            elif nm == "OT":
                o = self.dbg_out("OT", [8, 128, T], BF16)
                for i in range(8):
                    fw.dma("sp", o[i], self.OT[i])
            elif nm == "modT":
                o = self.dbg_out("modT", [128, 2, 48], F32)
                fw.dma("sp", o[:, :, :], self.modT[:])
        self.stats = fw.emit()

    def stage_load_x(self):
        fw, C = self.fw, self.C
        xin = [self.carve(i * 4096, [128, D], F32) for i in range(4)]
        for g in range(4):
            for i in range(4):
                tt = g * 4 + i
                fw.dma("sp", xin[i], self.inp["x"][tt * 128:(tt + 1) * 128, :])
            for dc in range(8):
                pb = self.psb[dc % 4]
                for i in range(4):
                    fw.tr(pb[:, i * 128:(i + 1) * 128], xin[i][:, dc * 128:(dc + 1) * 128], C["ident_f"][:])
                fw.copy(self.evac_eng(), self.xT[:, dc, g * 512:(g + 1) * 512], pb[:, :])

    def stage_adaln(self):
        fw, C = self.fw, self.C
        fw.act(self.cond[:], self.cT[:], AF.Silu)
        wbuf = [self.carve(i * 8192, [128, 8, 512], BF16) for i in range(2)]
        n = 0
        for l in range(self.nlayers):
            pm = self.psb[4 + l]
            wv = self.inp["w_ada"][l].rearrange("(kc p) n -> p kc n", p=128)
            for half in range(12):
                wb = wbuf[n % 2]
                n += 1
                fw.dma("pool", wb, wv[:, :, half * 512:(half + 1) * 512])
                for c4 in range(4):
                    j = half * 4 + c4
                    for kc in range(8):
                        fw.mm(pm[:, j:j + 1], wb[:, kc, c4 * 128:(c4 + 1) * 128], self.cond[:, kc:kc + 1],
                              start=(kc == 0), stop=(kc == 7))
            fw.tt("dve", self.modT[:, l, :], pm[:, 0:48], self.b_adaT[:, l, :], ALU.add)
            fw.stt(self.a1[:, l, :], self.modT[:, l, 8:16], 1.0, self.nmixT[:, l, :], ALU.add, ALU.mult)
            fw.stt(self.a2[:, l, :], self.modT[:, l, 32:40], 1.0, self.nffnT[:, l, :], ALU.add, ALU.mult)

    def stage_rmsnorm(self, l, which, rt_ps=None):
        fw, C = self.fw, self.C
        a = self.a1 if which == 0 else self.a2
        shoff = 0 if which == 0 else 24
        for tt in range(4):
            cs = slice(tt * 512, (tt + 1) * 512)
            pss = self.psb[tt % 2]
            for dc in range(8):
                sq = self.sqb[dc % 3]
                fw.act(sq[:], self.xT[:, dc, cs], AF.Square)
                fw.mm(pss[:, :], C["ones_b"][:], sq[:], start=(dc == 0), stop=(dc == 7))
            rs = self.rstd[tt % 2]
            fw.act(rs[:], pss[:, :], AF.Ln, bias=EPS, scale=1.0 / D)
            fw.act(rs[:], rs[:], AF.Exp, scale=-0.5)
            if rt_ps is not None:
                for i in range(4):
                    fw.mm(rt_ps[:, tt * 4 + i:tt * 4 + i + 1], rs[0:1, i * 128:(i + 1) * 128], C["ones_f"][0:1, 0:1])
            for dc in range(8):
                nt = self.ntmp[dc % 3]
                fw.stt(nt[:], self.xT[:, dc, cs], a[:, l, dc:dc + 1], rs[:], ALU.mult, ALU.mult)
                fw.act(self.hT[:, dc, cs], nt[:], AF.Identity, bias=self.modT[:, l, shoff + dc:shoff + dc + 1])

    def stage_dilmask(self):
        fw = self.fw
        tmp = self.carve(A_X, [128, 3072], F32)
        fw.dma("sp", tmp, self.cst["dilbias"][:, :])
        fw.act(self.dilM.rearrange("p a b -> p (a b)"), tmp, AF.Exp)

    def fox_prep(self, l):
        fw = self.fw
        wf = self.carve(A_Z2, [128, 8, 6], BF16)
        fw.dma("pool", wf, self.wview(self.inp["w_in"][l], FOFF, 6))
        fl = self.carve(A_FST, [6, T], F32)
        G = self.carve(A_FST + 8192, [6, T], F32)
        r1 = self.carve(A_FST + 16384, [6, T], F32)
        kp = self.carve(A_FST + 24576, [6, 3, T], BF16)
        qp = self.carve(A_FST + 36864, [6, 3, T], BF16)
        for tt in range(4):
            cs = slice(tt * 512, (tt + 1) * 512)
            ps = self.psb[tt % 2]
            for kc in range(8):
                fw.mm(ps[0:6, :], wf[:, kc, :], self.hT[:, kc, cs], start=(kc == 0), stop=(kc == 7))
            fw.act(fl[:, cs], ps[0:6, :], AF.Exp, scale=-1.0, bias=self.negb[0:6, l:l + 1])
        fw.act(fl, fl, AF.Ln, bias=1.0)
        fw.memset("dve", r1, 1.0)
        fw.gen("dve", "tensor_tensor_scan", [fl, r1], [G], out=G, data0=r1, data1=fl, initial=0.0,
               op0=ALU.mult, op1=ALU.add)
        fw.ts("dve", kp[:, 0, :], G, 8.0, None, op0=ALU.mult)
        fw.stt(r1, G, 8.0, kp[:, 0, :], ALU.mult, ALU.subtract)
        fw.copy("dve", kp[:, 1, :], r1)
        fw.tt("dve", r1, r1, kp[:, 1, :], ALU.subtract)
        fw.copy("dve", kp[:, 2, :], r1)
        kpf = kp.rearrange("p a b -> p (a b)")
        qpf = qp.rearrange("p a b -> p (a b)")
        fw.ts("dve", qpf, kpf, -1.0, None, op0=ALU.mult)
        fw.dma("sp", self.fsc[0], qpf)
        fw.dma("sp", self.fsc[1], kpf)

    def proj_qk(self, l, h, s, d):
        fw, C = self.fw, self.C
        w = self.wqk[s]
        Wl = self.inp["w_in"][l]
        fw.dma("pool", w[:, :, 0:128], self.wview(Wl, QOFF + h * 64, 128))
        fw.dma("pool", w[:, :, 128:256], self.wview(Wl, KOFF + h * 64, 128))
        fw.memset("pool", self.Qb[s][64:128, :], 0.0)
        fw.memset("pool", self.Kb[s][64:128, :], 0.0)
        for which, dst, gain in ((0, self.Qb[s], self.qg), (1, self.Kb[s], self.kg)):
            for tt in range(4):
                cs = slice(tt * 512, (tt + 1) * 512)
                ps = self.psb[tt % 2]
                for kc in range(8):
                    fw.mm(ps[:, :], w[:, kc, which * 128:(which + 1) * 128], self.hT[:, kc, cs],
                          start=(kc == 0), stop=(kc == 7))
                q2 = self.q2b[tt % 2]
                fw.act(q2, ps[:, :], AF.Square)
                p2 = self.psb[2]
                fw.mm(p2[:, :], C["onesblk"][:], q2, start=True, stop=True)
                rq = self.rqb[tt % 2]
                fw.act(rq[0:64, :], p2[0:64, :], AF.Ln, bias=EPS, scale=1.0 / 64)
                fw.act(rq[0:64, :], rq[0:64, :], AF.Exp, scale=-0.5)
                if d == 1:
                    o, i0, i1 = dst[0:64, cs], ps[0:64, :], rq[0:64, :]
                else:
                    n0, n1 = tt * 512 // d, (tt + 1) * 512 // d
                    o = dst[0:64, :].rearrange("p (r n) -> p n r", r=d)[:, n0:n1, :]
                    i0 = ps[0:64, :].rearrange("p (n r) -> p n r", r=d)
                    i1 = rq[0:64, :].rearrange("p (n r) -> p n r", r=d)
                fw.stt(o, i0, gain[:, l, h:h + 1], i1, ALU.mult, ALU.mult)

    def load_wv(self, l, grp):
        self.fw.dma("pool", self.wv, self.wview(self.inp["w_in"][l], VOFF + grp * 384, 384))

    def proj_v(self, nh, d, Vbuf, slot0, wcol0):
        fw = self.fw
        L = T // d
        nblk = L // 128
        for pb in range(16):
            r, nb = pb // nblk, pb % nblk
            ps = self.psb[pb % 2]
            if d == 1:
                tok = slice(pb * 128, (pb + 1) * 128)
            else:
                st0 = nb * 128 * d + r
                tok = slice(st0, st0 + 127 * d + 1, d)
            for kc in range(8):
                fw.mm(ps[:, 0:nh * 64], self.hT[:, kc, tok], self.wv[:, kc, wcol0:wcol0 + nh * 64],
                      start=(kc == 0), stop=(kc == 7))
            fw.copy(self.evac_eng(), Vbuf[:, pb, slot0:slot0 + nh, 0:64],
                    ps[:, 0:nh * 64].rearrange("p (h e) -> p h e", h=nh))

    def recip_row(self, den, k, eng):
        fw = self.fw
        rb = self.rbt[k % 2] if eng == "dve" else self.rbt4[k % 4]
        if eng == "dve":
            nc = self.nc
            o_ = rb[64:65, :]

            def f(o_=o_, den=den):
                with nc.allow_low_precision("bf16 reciprocal row feeds a bf16 broadcast matmul"):
                    return nc.vector.reciprocal(out=o_, in_=den)
            fw.op("dve", f, reads=[den], writes=[o_])
        else:
            lt = self.rlt[k % 2]
            fw.act(lt[64:65, :], den, AF.Ln)
            fw.act(rb[64:65, :], lt[64:65, :], AF.Exp, scale=-1.0)
        return rb

    def bcast_mul(self, src, rb, dest):
        fw, C = self.fw, self.C
        bp = self.psb[2]
        fw.mm(bp[:, :], C["ones_b"][64:65, :], rb[64:65, :], start=True, stop=True)
        fw.tt("dve", dest, src, bp[0:64, :], ALU.mult)

    def attn_causal(self, s, Vbuf, vslot, dest, hook=None, pending=None):
        fw, C = self.fw, self.C
        Qh, Kh = self.Qb[s], self.Kb[s]
        otS = (self.rd, self.bcs)
        for j in range(4):
            ot = self.psb[6 + j % 2]
            nkb = 4 * j + 4

            def S(kb):
                c0 = max(0, kb - 4 * j) * 128
                st = self.psb[3 + kb % 3]
                fw.mm(st[:, c0:512], Kh[:, kb * 128:(kb + 1) * 128], Qh[:, j * 512 + c0:(j + 1) * 512],
                      start=True, stop=True)
            S(0)
            if nkb > 1:
                S(1)
            for kb in range(nkb):
                if kb + 2 < nkb:
                    S(kb + 2)
                c0 = max(0, kb - 4 * j) * 128
                st = self.psb[3 + kb % 3]
                pt = self.ptb[kb % 3]
                fw.act(pt[:, c0:512], st[:, c0:512], AF.Exp, scale=0.125)
                if kb >= 4 * j:
                    fw.tt("pool", pt[:, c0:c0 + 128], pt[:, c0:c0 + 128], C["tri"][:], ALU.mult)
                fw.mm(ot[0:65, c0:512], Vbuf[:, kb, vslot, 0:65], pt[:, c0:512], start=(kb == 0), stop=(kb == nkb - 1))
                if kb == min(5, nkb - 1) and pending is not None:
                    pending()
                    pending = None
                if hook is not None and j == 2 and kb == 5:
                    hook()
                    hook = None
            if pending is not None:
                pending()
                pending = None

            o = otS[j % 2]
            fw.copy("act", o[0:65, :], ot[0:65, :])
            rb = self.recip_row(o[64:65, :], j, "dve")

            def fin(j=j, o=o, rb=rb):
                self.bcast_mul(o[0:64, :], rb, dest[:, j * 512:(j + 1) * 512])
            pending = fin
        return pending

    def fox_aug(self, h, s):
        fw = self.fw
        Qh, Kh = self.Qb[s], self.Kb[s]
        fw.dma("sp", Qh[64:67, :], self.fsc[0, h].rearrange("(a t) -> a t", a=3))
        fw.dma("sp", Qh[67:70, :], self.cst["fox_ones"][:, :])
        fw.dma("sp", Kh[64:67, :], self.cst["fox_ones"][:, :])
        fw.dma("sp", Kh[67:70, :], self.fsc[1, h].rearrange("(a t) -> a t", a=3))

    def moba_prep(self, h6, s):
        fw, C = self.fw, self.C
        Qh, Kh = self.Qb[s], self.Kb[s]
        km = self.carve(A_KM, [128, 8], F32)
        kmb = self.carve(A_KM + 64, [128, 8], BF16)
        fw.gen("dve", "tensor_reduce", [Kh[0:64, :]], [km[0:64, :]], out=km[0:64, :],
               in_=Kh[0:64, :].rearrange("p (b n) -> p b n", b=8), axis=AX.X, op=ALU.add)
        fw.ts("dve", kmb[0:64, :], km[0:64, :], 1.0 / 256, None, op0=ALU.mult)
        gp = self.psb[2]
        for qb in range(16):
            fw.mm(gp[:, qb * 8:(qb + 1) * 8], Qh[0:64, qb * 128:(qb + 1) * 128], kmb[0:64, :], start=True, stop=True)
        gm = self.carve(A_MG, [128, 128], F32)
        m8 = self.carve(A_MG + 512, [128, 128], F32)
        sel = self.carve(A_MG + 1024, [128, 128], F32)
        mbb = self.carve(A_MG + 1536, [128, 128], BF16)
        fw.tt("dve", gm, gp[:, 0:128], C["mg_sb1"][:], ALU.add)
        for qb in range(16):
            sl = slice(qb * 8, (qb + 1) * 8)
            fw.gen("dve", "max", [gm[:, sl]], [m8[:, sl]], out=m8[:, sl], in_=gm[:, sl])
        gm3 = gm.rearrange("p (a b) -> p a b", b=8)
        m83 = m8.rearrange("p (a b) -> p a b", b=8)
        sel3 = sel.rearrange("p (a b) -> p a b", b=8)
        fw.tt("dve", sel3, gm3, m83[:, :, 2:3].to_broadcast([128, 16, 8]), ALU.is_ge)
        fw.tt("dve", sel, sel, C["mg_v1"][:], ALU.mult)
        fw.tt("dve", sel, sel, C["mg_e"][:], ALU.add)
        fw.ts("dve", mbb, sel, 240000.0, -240000.0, op0=ALU.mult, op1=ALU.add)
        for tt in range(4):
            pp = self.psb[tt % 2]
            for i in range(4):
                qb = tt * 4 + i
                fw.mm(pp[64:72, i * 128:(i + 1) * 128], mbb[:, qb * 8:(qb + 1) * 8], C["ident_b"][:], start=True, stop=True)
            fw.copy(self.evac_eng(), Qh[64:72, tt * 512:(tt + 1) * 512], pp[64:72, :])
        fw.dma("sp", Qh[72:78, :], self.cst["moba_aq"][h6])
        fw.dma("sp", Kh[64:72, :], self.cst["blkind"][:, :])
        fw.dma("sp", Kh[72:78, :], self.cst["moba_ak"][h6])

    def attn_dil(self, g, j, s, Vbuf, vslot, first, hook=None, pending=None):
        fw = self.fw
        d = DIL[g][1]
        L = T // d
        nblk = L // 128
        Qh, Kh = self.Qb[s], self.Kb[s]
        M = self.dilM[:, g * 4 + j, :]
        accN = self.accN

        def S(pbq):
            nb = pbq % nblk
            st = self.psb[3 + pbq % 3]
            qs = slice(pbq * 128, (pbq + 1) * 128)
            fw.mm(st[:, 0:128], Kh[:, qs], Qh[:, qs], start=True, stop=True)
            if nb > 0:
                fw.mm(st[:, 128:256], Kh[:, (pbq - 1) * 128:pbq * 128], Qh[:, qs], start=True, stop=True)
        S(0)
        S(1)
        for c in range(4):
            ot = self.psb[6 + c % 2]
            for i in range(4):
                pbq = c * 4 + i
                nb = pbq % nblk
                if pbq + 2 < 16:
                    S(pbq + 2)
                st = self.psb[3 + pbq % 3]
                pt = self.ptb[pbq % 3]
                w = 256 if nb > 0 else 128
                fw.act(pt[:, 0:w], st[:, 0:w], AF.Exp, scale=0.125)
                fw.tt("pool", pt[:, 0:w], pt[:, 0:w], M[:, 0:w], ALU.mult)
                oc = ot[0:65, i * 128:(i + 1) * 128]
                fw.mm(oc, Vbuf[:, pbq, vslot, 0:65], pt[:, 0:128], start=True, stop=(nb == 0))
                if nb > 0:
                    fw.mm(oc, Vbuf[:, pbq - 1, vslot, 0:65], pt[:, 128:256], start=False, stop=True)
                if pending is not None and pbq == 2:
                    pending()
                    pending = None
            if d == 1:
                dst, src = accN[0:65, c * 512:(c + 1) * 512], ot[0:65, :]
            elif d == 4:
                dst, src = accN[0:65, c:c + 4 * 511 + 1:4], ot[0:65, :]
            else:
                dst = accN[0:65, :].rearrange("p (n r) -> p r n", r=16)[:, 4 * c:4 * c + 4, :]
                src = ot[0:65, :].rearrange("p (r n) -> p r n", r=4)
            if first:
                fw.copy("dve", dst, src)
            else:
                fw.tt("dve", dst, src, dst, ALU.add)
            if hook is not None and c == 1:
                hook()
                hook = None

    def stage_attention(self, l):
        fw = self.fw
        heads = self.heads or ("fox", "moba", "dil")
        fw.memset("pool", self.VA[:, :, :, 64:65], 1.0)
        fw.memset("pool", self.VB[:, :, :, 64:65], 1.0)
        jobs = []
        if "fox" in heads:
            self.fox_prep(l)
            for h in range(6):
                s = h % 2

                def prep(h=h, s=s):
                    if h == 0:
                        self.load_wv(l, 0)
                        self.proj_v(6, 1, self.VA, 0, 0)
                    self.proj_qk(l, h, s, 1)
                    self.fox_aug(h, s)

                def attn(hook, pending, h=h, s=s):
                    dest = self.OT[h // 2][0:64, :] if h % 2 == 0 else self.oddt[0:64, :]
                    p = self.attn_causal(s, self.VA, h, dest, hook=hook, pending=pending)
                    if h % 2 == 1:
                        def fin2(p=p, h=h):
                            p()
                            fw.dma("sp", self.OT[h // 2][64:128, :], self.oddt[0:64, :])
                        return fin2
                    return p
                jobs.append((prep, attn))
        if "moba" in heads:
            for h6 in range(6):
                h = 6 + h6
                s = h % 2

                def prep(h=h, h6=h6, s=s):
                    if h6 == 0:
                        self.load_wv(l, 1)
                        self.proj_v(6, 1, self.VB, 0, 0)
                    self.proj_qk(l, h, s, 1)
                    self.moba_prep(h6, s)

                def attn(hook, pending, h6=h6, s=s):
                    dest = self.OT[3 + h6 // 2][0:64, :] if h6 % 2 == 0 else self.oddt[0:64, :]
                    p = self.attn_causal(s, self.VB, h6, dest, hook=hook, pending=pending)
                    if h6 % 2 == 1:
                        def fin2(p=p, h6=h6):
                            p()
                            fw.dma("sp", self.OT[3 + h6 // 2][64:128, :], self.oddt[0:64, :])
                        return fin2
                    return p
                jobs.append((prep, attn))
        if "dil" in heads:
            n = 0
            for j in range(4):
                for g in range(3):
                    h = 12 + g * 4 + j
                    Vbuf, vslot = (self.VA, h - 12) if h < 18 else (self.VB, h - 18)
                    s = n % 2
                    first_dil = (n == 0)
                    n += 1

                    def prep(h=h, s=s, g=g, first_dil=first_dil):
                        if first_dil:
                            self.load_wv(l, 2)
                            self.proj_v(4, 1, self.VA, 0, 0)
                            self.proj_v(2, 4, self.VA, 4, 256)
                        if h == 20:
                            self.load_wv(l, 3)
                            self.proj_v(2, 4, self.VB, 0, 0)
                            self.proj_v(4, 16, self.VB, 2, 128)
                        self.proj_qk(l, h, s, DIL[g][1])

                    def attn(hook, pending, g=g, j=j, s=s, Vbuf=Vbuf, vslot=vslot):
                        self.attn_dil(g, j, s, Vbuf, vslot, g == 0, hook=hook, pending=pending)
                        if g == 2:
                            dest = self.OT[6 + j // 2][0:64, :] if j % 2 == 0 else self.oddt[0:64, :]
                            rbs = []
                            for tt in range(4):
                                cs = slice(tt * 512, (tt + 1) * 512)
                                rbs.append(self.recip_row(self.accN[64:65, cs], tt, "act"))

                            def fin(j=j, dest=dest):
                                for tt in range(4):
                                    cs = slice(tt * 512, (tt + 1) * 512)
                                    rb = self.rbt4[tt]
                                    self.bcast_mul(self.accN[0:64, cs], rb, dest[:, cs])
                                if j % 2 == 1:
                                    fw.dma("sp", self.OT[6 + j // 2][64:128, :], self.oddt[0:64, :])
                            return fin
                        return None
                    jobs.append((prep, attn))
        pending = None
        if jobs:
            jobs[0][0]()
        for i, (prep, attn) in enumerate(jobs):
            hook = jobs[i + 1][0] if i + 1 < len(jobs) else None
            pending = attn(hook, pending)
        if pending is not None:
            pending()

    def stage_outproj(self, l):
        fw = self.fw
        yT = [self.carve(A_Y + i * 4096, [128, T], BF16) for i in range(8)]
        wbr = [self.carve(A_Z2 + i * 2048, [128, 8, 128], BF16) for i in range(2)]
        wg = [self.carve(A_Z2 + 4096 + i * 2048, [128, 8, 128], BF16) for i in range(4)]
        Wl = self.inp["w_in"][l]
        ng = 0
        for fc in range(8):
            fs = slice(fc * 128, (fc + 1) * 128)
            wb = wbr[fc % 2]
            fw.dma("pool", wb[:, 0:3, :], self.inp["w_br_fox"][l].rearrange("(kc p) n -> p kc n", p=128)[:, :, fs])
            fw.dma("pool", wb[:, 3:6, :], self.inp["w_br_moba"][l].rearrange("(kc p) n -> p kc n", p=128)[:, :, fs])
            fw.dma("pool", wb[:, 6:8, :], self.inp["w_br_dil"][l].rearrange("(kc p) n -> p kc n", p=128)[:, :, fs])
            wgs = []
            for br in range(3):
                w = wg[ng % 4]
                ng += 1
                fw.dma("pool", w, self.wview(Wl, GOFF + br * 1024 + fc * 128, 128))
                wgs.append(w)
            for tt in range(4):
                cs = slice(tt * 512, (tt + 1) * 512)
                nt0, nt1 = self.ntmp[0], self.ntmp[1 + tt % 2]
                for br, (k0, nk) in enumerate(((0, 3), (3, 3), (6, 2))):
                    pz = self.psb[br]
                    for kc in range(nk):
                        fw.mm(pz[:, :], wb[:, k0 + kc, :], self.OT[k0 + kc][:, cs], start=(kc == 0), stop=(kc == nk - 1))
                    pg = self.psb[3 + br]
                    for kc in range(8):
                        fw.mm(pg[:, :], wgs[br][:, kc, :], self.hT[:, kc, cs], start=(kc == 0), stop=(kc == 7))
                    sg = self.sqb[br]
                    fw.act(sg[:], pg[:, :], AF.Sigmoid)
                    if br == 0:
                        fw.tt("dve", nt0[:], pz[:, :], sg[:], ALU.mult)
                    else:
                        fw.tt("dve", nt1[:], pz[:, :], sg[:], ALU.mult)
                        if br == 1:
                            fw.tt("pool", nt0[:], nt0[:], nt1[:], ALU.add)
                        else:
                            fw.tt("pool", yT[fc][:, cs], nt0[:], nt1[:], ALU.add)
        for fc in range(8):
            w = wg[ng % 4]
            ng += 1
            fw.dma("pool", w, self.wview(self.inp["w_out"][l], fc * 128, 128))
            for tt in range(4):
                cs = slice(tt * 512, (tt + 1) * 512)
                ps = self.psb[6 + tt % 2]
                for kc in range(8):
                    fw.mm(ps[:, :], w[:, kc, :], yT[kc][:, cs], start=(kc == 0), stop=(kc == 7))
                fw.stt(self.xT[:, fc, cs], ps[:, :], self.modT[:, l, 16 + fc:17 + fc], self.xT[:, fc, cs], ALU.mult, ALU.add)

    def ffn_expert(self, l, wg_ap, wu_ap, wd_ap, combbc):
        fw = self.fw
        actT = self.carve(A_ACT, [128, 11, T], BF16)
        wgu = [self.carve(A_WGU + i * 2048, [128, 8, 128], BF16) for i in range(4)]
        wdt = [self.carve(A_WD + i * 2816, [128, 11, 128], BF16) for i in range(2)]
        sgt = [self.carve(A_SG + i * 1024, [128, 512], BF16) for i in range(3)]
        if not hasattr(self, "_ffc"):
            self._ffc = 0
            self._wdc = 0
        for half in range(2):
            for fi in range(11):
                ffc = half * 11 + fi
                wgt = wgu[(2 * self._ffc) % 4]
                wut = wgu[(2 * self._ffc + 1) % 4]
                self._ffc += 1
                fw.dma("pool", wgt, self.wview(wg_ap, ffc * 128, 128))
                fw.dma("pool", wut, self.wview(wu_ap, ffc * 128, 128))
                for tt in range(4):
                    cs = slice(tt * 512, (tt + 1) * 512)
                    pg = self.psb[(2 * tt) % 4]
                    pu = self.psb[(2 * tt + 1) % 4]
                    for kc in range(8):
                        fw.mm(pg[:, :], wgt[:, kc, :], self.hT[:, kc, cs], start=(kc == 0), stop=(kc == 7))
                    for kc in range(8):
                        fw.mm(pu[:, :], wut[:, kc, :], self.hT[:, kc, cs], start=(kc == 0), stop=(kc == 7))
                    sg = sgt[tt % 3]
                    fw.act(sg, pg[:, :], AF.Silu)
                    if combbc is not None:
                        fw.tt("pool", sg, sg, combbc[:, cs], ALU.mult)
                    fw.tt("dve", actT[:, fi, cs], pu[:, :], sg, ALU.mult)
            for fc in range(8):
                wd = wdt[self._wdc % 2]
                self._wdc += 1
                fw.dma("pool", wd, wd_ap.rearrange("(f p) n -> p f n", p=128)[:, half * 11:(half + 1) * 11, fc * 128:(fc + 1) * 128])
                for tt in range(4):
                    cs = slice(tt * 512, (tt + 1) * 512)
                    ps = self.psb[4 + tt % 2]
                    for fi in range(11):
                        fw.mm(ps[:, :], wd[:, fi, :], actT[:, fi, cs], start=(fi == 0), stop=(fi == 10))
                    fw.stt(self.xT[:, fc, cs], ps[:, :], self.modT[:, l, 40 + fc:41 + fc], self.xT[:, fc, cs], ALU.mult, ALU.add)

    def stage_moe(self, l):
        fw, C = self.fw, self.C
        R0 = A_Z2 + 8192
        wr = self.carve(R0, [128, 8, 8], F32)
        wr2 = self.carve(R0 + 256, [128, 8, 8], F32)
        brt = self.carve(R0 + 512, [128, 8], F32)
        crow = self.carve(R0 + 544, [128, 8], F32)
        cb = self.carve(R0 + 576, [128, 8], F32)
        rt = self.carve(R0 + 608, [128, 16], F32)
        lg = self.carve(R0 + 1024, [128, 16, 8], F32)
        m8 = self.carve(R0 + 1536, [128, 16, 8], F32)
        eq = self.carve(R0 + 2048, [128, 16, 8], F32)
        comb = self.carve(R0 + 2560, [128, 16, 8], F32)
        w1 = self.carve(R0 + 3072, [128, 16], F32)
        w2 = self.carve(R0 + 3136, [128, 16], F32)
        e21 = self.carve(R0 + 3200, [128, 16], F32)
        fw.dma("sp", wr, self.inp["w_router"][0].rearrange("(kc p) e -> p kc e", p=128))
        fw.dma("sp", brt[0:1, :], self.inp["b_router"][0:1, :])
        for kc in range(8):
            fw.ts("dve", wr2[:, kc, :], wr[:, kc, :], self.a2[:, l, kc:kc + 1], None, op0=ALU.mult)
        pc = self.psb[2]
        for kc in range(8):
            fw.mm(pc[0:1, 0:8], self.modT[:, l, 24 + kc:25 + kc], wr[:, kc, :], start=(kc == 0), stop=(kc == 7))
        fw.tt("dve", crow[0:1, :], pc[0:1, 0:8], brt[0:1, :], ALU.add)
        pcb = self.psb[3]
        fw.mm(pcb[:, 0:8], C["ones_f"][0:1, :], crow[0:1, :], start=True, stop=True)
        fw.copy("dve", cb, pcb[:, 0:8])
        pl = self.psb[6]
        for t16 in range(16):
            for kc in range(8):
                fw.mm(pl[:, t16 * 8:(t16 + 1) * 8], self.xT[:, kc, t16 * 128:(t16 + 1) * 128], wr2[:, kc, :],
                      start=(kc == 0), stop=(kc == 7))
        fw.copy("dve", rt, self.psb[7][:, 0:16])
        fw.tt("dve", lg, pl[:, 0:128].rearrange("p (a b) -> p a b", b=8), rt.unsqueeze(2).to_broadcast([128, 16, 8]), ALU.mult)
        fw.tt("dve", lg, lg, cb.unsqueeze(1).to_broadcast([128, 16, 8]), ALU.add)
        for t16 in range(16):
            fw.gen("dve", "max", [lg[:, t16, :]], [m8[:, t16, :]], out=m8[:, t16, :], in_=lg[:, t16, :])
        fw.tt("dve", e21, m8[:, :, 1], m8[:, :, 0], ALU.subtract)
        fw.act(e21, e21, AF.Exp)
        fw.ts("dve", w1, e21, 1.0, None, op0=ALU.add)
        fw.gen("dve", "reciprocal", [w1], [w1], out=w1, in_=w1)
        fw.tt("dve", w2, e21, w1, ALU.mult)
        fw.tt("dve", eq, lg, m8[:, :, 0:1].to_broadcast([128, 16, 8]), ALU.is_equal)
        fw.tt("dve", comb, eq, w1.unsqueeze(2).to_broadcast([128, 16, 8]), ALU.mult)
        fw.tt("dve", eq, lg, m8[:, :, 1:2].to_broadcast([128, 16, 8]), ALU.is_equal)
        fw.tt("dve", eq, eq, w2.unsqueeze(2).to_broadcast([128, 16, 8]), ALU.mult)
        fw.tt("dve", comb, comb, eq, ALU.add)
        if "comb" in self.debug:
            o = self.dbg_out("comb", [128, 16, 8], F32)
            fw.dma("sp", o[:, :, :], comb)
        cbc = [self.carve(A_Z2 + i * 4096, [128, T], BF16) for i in range(2)]
        for e in range(NEXP):
            cc = cbc[e % 2]
            for tt in range(4):
                nt = self.ntmp[tt % 3]
                for i in range(4):
                    fw.ts("dve", nt[:, i * 128:(i + 1) * 128], C["ident_f"][:], comb[:, tt * 4 + i, e:e + 1], None, op0=ALU.mult)
                pb = self.psb[6 + tt % 2]
                fw.mm(pb[:, :], C["ones_f"][:], nt[:], start=True, stop=True)
                fw.copy("act", cc[:, tt * 512:(tt + 1) * 512], pb[:, :])
            self.ffn_expert(l, self.inp["w_exp_gate"][0, e], self.inp["w_exp_up"][0, e], self.inp["w_exp_down"][0, e], cc)

    def stage_output(self):
        fw, C = self.fw, self.C
        xo = [self.carve(i * 4096, [128, D], F32) for i in range(2)]
        for t16 in range(16):
            xb = xo[t16 % 2]
            for half in range(2):
                pb = self.psb[(t16 * 2 + half) % 4]
                for i in range(4):
                    dc = half * 4 + i
                    fw.tr(pb[:, i * 128:(i + 1) * 128], self.xT[:, dc, t16 * 128:(t16 + 1) * 128], C["ident_f"][:])
                fw.copy(self.evac_eng(), xb[:, half * 512:(half + 1) * 512], pb[:, :])
            fw.dma("sp", self.out[t16 * 128:(t16 + 1) * 128, :], xb)

    def stage_moe_sparse(self, l):
        fw, C, nc = self.fw, self.C, self.nc
        I32 = mybir.dt.int32
        R0 = A_Z2
        wr = self.carve(R0, [128, 8, 8], F32)
        wr2 = self.carve(R0 + 256, [128, 8, 8], F32)
        brt = self.carve(R0 + 512, [128, 8], F32)
        crow = self.carve(R0 + 544, [128, 8], F32)
        cb = self.carve(R0 + 576, [128, 8], F32)
        rt = self.carve(R0 + 608, [128, 16], F32)
        e21 = self.carve(R0 + 672, [128, 16], F32)
        lg = self.carve(R0 + 1024, [128, 16, 8], F32)
        m8 = self.carve(R0 + 1536, [128, 16, 8], F32)
        eq1 = self.carve(R0 + 2048, [128, 16, 8], F32)
        eq2 = self.carve(R0 + 2560, [128, 16, 8], F32)
        tot = self.carve(R0 + 3072, [128, 16, 8], F32)
        pre = self.carve(R0 + 3584, [128, 16, 8], F32)
        pos = self.carve(R0 + 4096, [128, 16, 8], F32)
        tmp = self.carve(R0 + 4608, [128, 16, 8], F32)
        maskb = self.carve(R0 + 5120, [128, 128], BF16)
        gf = self.carve(R0 + 5376, [128, 16], F32)
        cntf = self.carve(R0 + 5440, [128, 8], F32)
        w1 = self.sb("moe_w1", [128, 16], F32)[:, :]
        w2 = self.sb("moe_w2", [128, 16], F32)[:, :]
        gi = [self.sb("moe_gi%d" % k, [128, 16], I32)[:, :] for k in range(2)]
        cnti = self.sb("moe_cnt", [1, 8], I32)[:, :]
        fw.dma("sp", wr, self.inp["w_router"][0].rearrange("(kc p) e -> p kc e", p=128))
        fw.dma("sp", brt[0:1, :], self.inp["b_router"][0:1, :])
        for kc in range(8):
            fw.ts("dve", wr2[:, kc, :], wr[:, kc, :], self.a2[:, l, kc:kc + 1], None, op0=ALU.mult)
        pc = self.psb[2]
        for kc in range(8):
            fw.mm(pc[0:1, 0:8], self.modT[:, l, 24 + kc:25 + kc], wr[:, kc, :], start=(kc == 0), stop=(kc == 7))
        fw.tt("dve", crow[0:1, :], pc[0:1, 0:8], brt[0:1, :], ALU.add)
        pcb = self.psb[3]
        fw.mm(pcb[:, 0:8], C["ones_f"][0:1, :], crow[0:1, :], start=True, stop=True)
        fw.copy("dve", cb, pcb[:, 0:8])
        pl = self.psb[6]
        for t16 in range(16):
            for kc in range(8):
                fw.mm(pl[:, t16 * 8:(t16 + 1) * 8], self.xT[:, kc, t16 * 128:(t16 + 1) * 128], wr2[:, kc, :],
                      start=(kc == 0), stop=(kc == 7))
        fw.copy("dve", rt, self.psb[7][:, 0:16])
        bc3 = [128, 16, 8]
        fw.tt("dve", lg, pl[:, 0:128].rearrange("p (a b) -> p a b", b=8), rt.unsqueeze(2).to_broadcast(bc3), ALU.mult)
        fw.tt("dve", lg, lg, cb.unsqueeze(1).to_broadcast(bc3), ALU.add)
        for t16 in range(16):
            fw.gen("dve", "max", [lg[:, t16, :]], [m8[:, t16, :]], out=m8[:, t16, :], in_=lg[:, t16, :])
        fw.tt("dve", e21, m8[:, :, 1], m8[:, :, 0], ALU.subtract)
        fw.act(e21, e21, AF.Exp)
        fw.ts("dve", w1, e21, 1.0, None, op0=ALU.add)
        fw.gen("dve", "reciprocal", [w1], [w1], out=w1, in_=w1)
        fw.tt("dve", w2, e21, w1, ALU.mult)
        fw.tt("dve", eq1, lg, m8[:, :, 0:1].to_broadcast(bc3), ALU.is_equal)
        fw.tt("dve", eq2, lg, m8[:, :, 1:2].to_broadcast(bc3), ALU.is_equal)
        fl = lambda a: a.rearrange("p a b -> p (a b)")
        fw.tt("dve", maskb, fl(eq1), fl(eq2), ALU.add)
        ptot, pwi = self.psb[4], self.psb[5]
        fw.mm(ptot[:, 0:128], C["ones_b"][:], maskb, start=True, stop=True)
        for t16 in range(16):
            fw.mm(pwi[:, t16 * 8:(t16 + 1) * 8], C["ustrict"][:], maskb[:, t16 * 8:(t16 + 1) * 8], start=True, stop=True)
        fw.copy("dve", fl(tot), ptot[:, 0:128])
        fw.memset("dve", pre[:, 0, :], 0.0)
        for t16 in range(1, 16):
            fw.tt("dve", pre[:, t16, :], pre[:, t16 - 1, :], tot[:, t16 - 1, :], ALU.add)
        fw.tt("dve", cntf, pre[:, 15, :], tot[:, 15, :], ALU.add)
        fw.copy("dve", cnti[0:1, :], cntf[0:1, :])
        fw.tt("dve", fl(pos), pwi[:, 0:128], fl(pre), ALU.add)
        fw.tt("dve", fl(pos), fl(pos), C["ebase"][:], ALU.add)
        for k, eq in enumerate((eq1, eq2)):
            fw.tt("dve", tmp, pos, eq, ALU.mult)
            fw.gen("dve", "tensor_reduce", [tmp], [gf], out=gf, in_=tmp, axis=AX.X, op=ALU.add)
            fw.copy("dve", gi[k], gf)
        if "moeidx" in self.debug:
            o = self.dbg_out("gi0", [128, 16], I32)
            fw.dma("sp", o[:, :], gi[0][:])
            o = self.dbg_out("gi1", [128, 16], I32)
            fw.dma("sp", o[:, :], gi[1][:])
            o = self.dbg_out("cnt", [1, 8], I32)
            fw.dma("sp", o[:, :], cnti[:])
        rows = [[self.carve(k * 2056 + i * 4112, [128, 1028], BF16) for k in range(2)] for i in range(2)]
        xrow = [self.carve(16448 + i * 4096, [128, D], F32) for i in range(2)]
        wsrc = (w1, w2)
        for t16 in range(16):
            ts_ = slice(t16 * 128, (t16 + 1) * 128)
            ph = self.psb[t16 % 2].bitcast(BF16)
            for kc in range(8):
                fw.tr(ph[:, kc * 128:(kc + 1) * 128], self.hT[:, kc, ts_], C["ident_b"][:])
            for k in range(2):
                Rk = rows[t16 % 2][k]
                fw.copy("act" if k == 0 else "dve", Rk[:, 0:1024], ph[:, :])
                fw.copy("dve", Rk[:, 1024:1026].bitcast(F32), wsrc[k][:, t16:t16 + 1])
                g = nc.gpsimd
                idx = gi[k][:, t16:t16 + 1]
                src = Rk[:, 0:1026]
                fw.op("pool", (lambda src=src, idx=idx: g.indirect_dma_start(
                    out=self.HS[:, :], out_offset=bass.IndirectOffsetOnAxis(ap=idx, axis=0), in_=src, in_offset=None)),
                    reads=[src, idx], writes=[self.HS[:, :]], dma=True)
            xr = xrow[t16 % 2]
            for half in range(2):
                pb = self.psb[2 + half]
                for i in range(4):
                    dc = half * 4 + i
                    fw.tr(pb[:, i * 128:(i + 1) * 128], self.xT[:, dc, ts_], C["ident_f"][:])
                fw.copy("act" if half == 0 else "dve", xr[:, half * 512:(half + 1) * 512], pb[:, :])
            fw.dma("sp", self.XTOK[ts_, :], xr)
        E_WD, E_WGU, E_HTE, E_ACT, E_ACTT, E_G, E_YSL, E_SG, E_WS = 0, 22528, 56320, 89088, 134144, 139776, 143888, 147984, 149392
        wd = self.carve(E_WD, [128, 11, D], BF16)
        wgu = [self.carve(E_WGU + i * 5632, [128, 8, 352], BF16) for i in range(6)]
        hTe = self.carve(E_HTE, [128, 16, 1024], BF16)
        act_sl = self.carve(E_ACT, [128, 16, 1408], BF16)
        actT = [self.carve(E_ACTT + i * 2816, [128, 1408], BF16) for i in range(2)]
        G = [self.carve(E_G + i * 2056, [128, 1028], BF16) for i in range(2)]
        ysl = [self.carve(E_YSL + i * 2048, [128, D], BF16) for i in range(2)]
        sgt = [self.carve(E_SG + i * 704, [128, 352], BF16) for i in range(2)]
        ws = self.carve(E_WS, [128, 16], F32)
        y0 = self.hT.rearrange("p a b -> p (a b)").rearrange("p (c f) -> p c f", c=16)
        self._nw = 0
        for e in range(NEXP):
            key = "moe_e%d" % e

            for ce in fw.CENGS:
                def ld(key=key, e=e, ce=ce):
                    eo = fw.engobj[ce]
                    reg = eo.alloc_register("r_%s_%s" % (key, ce))
                    ins = eo.reg_load(reg, cnti[0:1, e:e + 1])
                    fw.cond_vals[(key, ce)] = eo.snap(reg)
                    return ins
                fw.op(ce, ld, reads=[cnti[0:1, e:e + 1]], writes=[])
            Wg, Wu, Wd = self.inp["w_exp_gate"][0, e], self.inp["w_exp_up"][0, e], self.inp["w_exp_down"][0, e]
            for c in range(16):
                Gc = G[c % 2]
                fw.dma("sp", Gc[:, 0:1026], self.HS[e * T + c * 128:e * T + (c + 1) * 128, :])
                fw.cur_cond = (key, c * 128)
                fw.copy("dve", ws[:, c:c + 1], Gc[:, 1024:1026].bitcast(F32))
                ph = self.psb[c % 2].bitcast(BF16)
                for kc in range(8):
                    fw.tr(ph[:, kc * 128:(kc + 1) * 128], Gc[:, kc * 128:(kc + 1) * 128], C["ident_b"][:])
                fw.copy("act", hTe[:, c, :], ph[:, :])
                fw.cur_cond = None
            fw.barrier()
            for half in range(2):
                tiles = {}

                def issue_cg(cg, half=half, tiles=tiles, Wg=Wg, Wu=Wu):
                    col0 = half * 1408 + cg * 352
                    wgt, wut = wgu[(2 * self._nw) % 6], wgu[(2 * self._nw + 1) % 6]
                    self._nw += 1
                    fw.dma("pool", wgt, self.wview(Wg, col0, 352))
                    fw.dma("pool", wut, self.wview(Wu, col0, 352))
                    tiles[cg] = (wgt, wut)
                issue_cg(0)
                issue_cg(1)
                issue_cg(2)
                fw.dma("pool", wd, Wd.rearrange("(f p) n -> p f n", p=128)[:, half * 11:(half + 1) * 11, :])
                for cg in range(4):
                    wgt, wut = tiles[cg]
                    for c in range(16):
                        pg, pu = self.psb[2 + 2 * (c % 2)], self.psb[3 + 2 * (c % 2)]
                        fw.cur_cond = (key, c * 128)
                        for kc in range(8):
                            fw.mm(pg[:, 0:352], hTe[:, c, kc * 128:(kc + 1) * 128], wgt[:, kc, :], start=(kc == 0), stop=(kc == 7))
                        for kc in range(8):
                            fw.mm(pu[:, 0:352], hTe[:, c, kc * 128:(kc + 1) * 128], wut[:, kc, :], start=(kc == 0), stop=(kc == 7))
                        sg = sgt[c % 2]
                        fw.act(sg, pg[:, 0:352], AF.Silu)
                        fw.stt(act_sl[:, c, cg * 352:(cg + 1) * 352], pu[:, 0:352], ws[:, c:c + 1], sg, ALU.mult, ALU.mult)
                        fw.cur_cond = None
                    if cg == 3:
                        fw.barrier()
                    if cg + 3 < 4:
                        issue_cg(cg + 3)
                for c in range(16):
                    at = actT[c % 2]
                    pa, pb2 = self.psb[0].bitcast(BF16), self.psb[1].bitcast(BF16)
                    fw.cur_cond = (key, c * 128)
                    for f in range(11):
                        dstp = pa[:, f * 128:(f + 1) * 128] if f < 8 else pb2[:, (f - 8) * 128:(f - 7) * 128]
                        fw.tr(dstp, act_sl[:, c, f * 128:(f + 1) * 128], C["ident_b"][:])
                    fw.copy("act", at[:, 0:1024], pa[:, :])
                    fw.copy("dve", at[:, 1024:1408], pb2[:, 0:384])
                    py = (self.psb[6], self.psb[7])
                    for fo in range(2):
                        for f in range(11):
                            fw.mm(py[fo][:, :], at[:, f * 128:(f + 1) * 128], wd[:, f, fo * 512:(fo + 1) * 512],
                                  start=(f == 0), stop=(f == 10))
                    if half == 0:
                        fw.copy("act", y0[:, c, 0:512], py[0][:, :])
                        fw.copy("dve", y0[:, c, 512:1024], py[1][:, :])
                    else:
                        yb = ysl[c % 2]
                        fw.tt("dve", yb[:, 0:512], py[0][:, :], y0[:, c, 0:512], ALU.add)
                        fw.tt("dve", yb[:, 512:1024], py[1][:, :], y0[:, c, 512:1024], ALU.add)
                    fw.cur_cond = None
                    if half == 1:
                        fw.dma("sp", self.YS[e * T + c * 128:e * T + (c + 1) * 128, :], ysl[c % 2])
                fw.barrier()
        g2bc = self.carve(0, [128, D], F32)
        dg = self.carve(4096, [128, D], F32)
        for dc in range(8):
            fw.ts("dve", dg[:, dc * 128:(dc + 1) * 128], C["ident_f"][:], self.modT[:, l, 40 + dc:41 + dc], None, op0=ALU.mult)
        for half in range(2):
            pb = self.psb[2 + half]
            fw.mm(pb[:, :], C["ones_f"][:], dg[:, half * 512:(half + 1) * 512], start=True, stop=True)
            fw.copy("act", g2bc[:, half * 512:(half + 1) * 512], pb[:, :])
        ya = [[self.carve(8192 + (2 * i + k) * 2048, [128, D], BF16) for k in range(2)] for i in range(2)]
        ysum = [self.carve(16384 + i * 4096, [128, D], F32) for i in range(2)]
        xo = [self.carve(24576 + i * 4096, [128, D], F32) for i in range(2)]
        for t16 in range(16):
            ts_ = slice(t16 * 128, (t16 + 1) * 128)
            g = nc.gpsimd
            for k in range(2):
                dst = ya[t16 % 2][k]
                idx = gi[k][:, t16:t16 + 1]
                fw.op("pool", (lambda dst=dst, idx=idx: g.indirect_dma_start(
                    out=dst, out_offset=None, in_=self.YS[:, :], in_offset=bass.IndirectOffsetOnAxis(ap=idx, axis=0))),
                    reads=[self.YS[:, :], idx], writes=[dst], dma=True)
            xb = xo[t16 % 2]
            fw.dma("sp", xb, self.XTOK[ts_, :])
            y1, y2 = ya[t16 % 2]
            ysm = ysum[t16 % 2]
            fw.tt("dve", ysm, y1, y2, ALU.add)
            fw.tt("pool", ysm, ysm, g2bc, ALU.mult)
            fw.tt("dve", xb, xb, ysm, ALU.add)
            fw.dma("sp", self.out[ts_, :], xb)


_CACHE = {}


def kernel(**inputs):
    inputs = {k: np.asarray(v) for k, v in inputs.items()}
    k = K()
    in_maps = []
    for b in range(8):
        m = host_layout(inputs, b)
        for kk, v in k.hc.items():
            m["c_" + kk] = v
        in_maps.append(m)
    res = run_bass_kernel_spmd(k.nc, in_maps, core_ids=list(range(8)))
    out = np.stack([np.asarray(res.results[b]["out"]) for b in range(8)], axis=0)
    return out.astype(np.float32)
```

```python
from concourse.bass_utils import run_bass_kernel_spmd
import numpy as np
import concourse.bass as bass
import concourse.mybir as mybir

F32 = mybir.dt.float32
BF16 = mybir.dt.bfloat16
AF = mybir.ActivationFunctionType
ALU = mybir.AluOpType
AX = mybir.AxisListType

_DTSIZE = {}


def dtsize(dt):
    if dt not in _DTSIZE:
        _DTSIZE[dt] = np.dtype(mybir.dt.np(dt)).itemsize
    return _DTSIZE[dt]


class Rec:
    __slots__ = ("eng", "fn", "deps", "dma", "sig", "idx", "gidx", "dmasem", "dmaval", "vc", "cond", "sv")


class FW:
    ENGS = ("pe", "act", "dve", "pool", "sp")
    CENGS = ("pe", "act", "dve")

    def __init__(self, nc, n_dma_sems=24):
        self.nc = nc
        self.recs = []
        self.eng_recs = {e: [] for e in self.ENGS}
        self.hist = {}
        self.engobj = {"pe": nc.tensor, "act": nc.scalar, "dve": nc.vector, "pool": nc.gpsimd, "sp": nc.sync}
        self.n_dma_sems = n_dma_sems
        self.dma_count = {e: 0 for e in self.ENGS}
        self.dma_last = {}
        self.cur_cond = None
        self.cond_vals = {}

    def region(self, ap):
        t = ap.tensor
        name = t.name
        space = str(ap.space) if hasattr(ap, "space") else ""
        esz = dtsize(ap.dtype)
        apl = list(ap.ap)
        off = ap.offset
        is_dram = "DRAM" in space.upper() or "HBM" in space.upper() or type(t).__name__.startswith("DRam")
        if is_dram:
            lo = off
            hi = off
            for st, cnt in apl:
                if cnt > 1:
                    if st >= 0:
                        hi += st * (cnt - 1)
                    else:
                        lo += st * (cnt - 1)
            return (name, 0, 1, lo * esz, (hi + 1) * esz, False)
        tsz = dtsize(t.dtype)
        pstride = 1
        for s in t.shape[1:]:
            pstride *= s
        if esz != tsz:
            pstride = pstride * tsz // esz
        p0 = off // pstride
        f0 = off % pstride
        pst, pcnt = apl[0]
        if pst == 0:
            pcnt = 1
        p1 = p0 + pcnt
        lo = f0
        hi = f0
        for st, cnt in apl[1:]:
            if cnt > 1:
                if st >= 0:
                    hi += st * (cnt - 1)
                else:
                    lo += st * (cnt - 1)
        b0, b1 = lo * esz, (hi + 1) * esz
        is_psum = type(t).__name__.startswith("PSum")
        if is_psum:
            b0 = (b0 // 2048) * 2048
            b1 = ((b1 + 2047) // 2048) * 2048
            p0, p1 = 0, 128
        return (name, p0, p1, b0, b1, is_psum)

    def op(self, eng, fn, reads=(), writes=(), dma=False):
        r = Rec()
        r.cond = self.cur_cond if eng in self.CENGS else None
        r.sv = None
        r.eng = eng
        r.fn = fn
        r.dma = dma
        r.sig = False
        r.deps = []
        r.idx = len(self.eng_recs[eng])
        r.gidx = len(self.recs)
        r.dmasem = None
        deps = set()
        for ap in reads:
            self._access(r, self.region(ap), False, deps)
        for ap in writes:
            self._access(r, self.region(ap), True, deps)
        if dma:
            slot = self.dma_count[eng] % self.n_dma_sems
            self.dma_count[eng] += 1
            prev = self.dma_last.get((eng, slot))
            if prev is not None:
                deps.add(prev)
            self.dma_last[(eng, slot)] = r
            r.dmasem = slot
        r.deps = sorted(deps, key=lambda d: d.gidx)
        self.recs.append(r)
        self.eng_recs[eng].append(r)
        return r

    def _access(self, r, reg, is_write, deps):
        name, p0, p1, b0, b1, is_psum = reg
        lst = self.hist.setdefault(name, [])
        excl = is_write or is_psum
        keep = []
        for ent in lst:
            ep0, ep1, eb0, eb1, w, rds, wtrue = ent
            if ep1 <= p0 or p1 <= ep0 or eb1 <= b0 or b1 <= eb0:
                keep.append(ent)
                continue
            inside = ep0 >= p0 and ep1 <= p1 and eb0 >= b0 and eb1 <= b1
            if w is not None and w is not r:
                if w.dma or r.dma or w.eng != r.eng:
                    deps.add(w)
                elif wtrue and (not is_write) and r.eng != "pe":
                    deps.add(w)
            if excl:
                for rd in rds:
                    if rd is r:
                        continue
                    if rd.dma or r.dma or rd.eng != r.eng:
                        deps.add(rd)
                if inside:
                    continue
            else:
                if w is None and inside and len(rds) == 1 and (not rds[0].dma) and (not r.dma) and rds[0].eng == r.eng:
                    continue
            keep.append(ent)
        if excl:
            keep.append([p0, p1, b0, b1, r, [], is_write])
        else:
            keep.append([p0, p1, b0, b1, None, [r], False])
        self.hist[name] = keep

    def barrier(self, engs=("pe", "act", "dve")):
        lasts = {e: self.eng_recs[e][-1] for e in engs if self.eng_recs[e]}
        for e in engs:
            eo = self.engobj[e]
            r = self.op(e, (lambda eo=eo: eo.drain()), reads=[], writes=[])
            extra = [lasts[o] for o in engs if o != e and o in lasts]
            r.deps = sorted(set(r.deps) | set(extra), key=lambda d: d.gidx)

    def emit(self):
        nc = self.nc
        ne = len(self.ENGS)
        eidx = {e: i for i, e in enumerate(self.ENGS)}
        grp_of = {}
        groups = []
        for ce in self.CENGS:
            prev = None
            for r in self.eng_recs[ce]:
                if r.cond is None:
                    prev = None
                    continue
                if prev is not None and prev.cond is not None and prev.cond[0] == r.cond[0] and prev.cond[1] <= r.cond[1] \
                        and prev.idx == r.idx - 1:
                    groups[-1].append(r)
                else:
                    groups.append([r])
                grp_of[r.gidx] = len(groups) - 1
                prev = r
        clock = {e: [-1] * ne for e in self.ENGS}
        dma_known = {e: set() for e in self.ENGS}
        final_deps = []
        cur_grp = {e: None for e in self.CENGS}
        saved = {e: None for e in self.CENGS}
        for r in self.recs:
            if r.eng in self.CENGS:
                g = grp_of.get(r.gidx)
                if g != cur_grp[r.eng]:
                    if cur_grp[r.eng] is not None:
                        clock[r.eng] = saved[r.eng][0]
                        dma_known[r.eng] = saved[r.eng][1]
                    if g is not None:
                        saved[r.eng] = (list(clock[r.eng]), set(dma_known[r.eng]))
                    cur_grp[r.eng] = g
            ck = clock[r.eng]
            need = []
            for d in r.deps:
                if d.dma:
                    if d.gidx in dma_known[r.eng]:
                        continue
                    need.append(d)
                else:
                    if ck[eidx[d.eng]] >= d.idx:
                        continue
                    need.append(d)
            best = {}
            nd = []
            for d in need:
                if d.dma:
                    nd.append(d)
                else:
                    if d.eng not in best or best[d.eng].idx < d.idx:
                        best[d.eng] = d
            nd.extend(best.values())
            for d in nd:
                d.sig = True
                if d.dma:
                    dma_known[r.eng].add(d.gidx)
                    dvc = d.vc
                else:
                    dvc = list(d.vc)
                    dvc[eidx[d.eng]] = max(dvc[eidx[d.eng]], d.idx)
                for i in range(ne):
                    if dvc[i] > ck[i]:
                        ck[i] = dvc[i]
            if r.eng in self.CENGS and cur_grp[r.eng] is not None:
                r.vc = list(saved[r.eng][0])
            else:
                r.vc = list(ck)
            final_deps.append(nd)
        cnt = {e: 0 for e in self.ENGS}
        dcnt = {}
        for r in self.recs:
            if r.dma:
                key = (r.eng, r.dmasem)
                dcnt[key] = dcnt.get(key, 0) + 16
                r.dmaval = dcnt[key]
            elif r.sig:
                cnt[r.eng] += 1
                r.sv = cnt[r.eng]
        sems = {}
        for e in self.ENGS:
            sems[e] = nc.alloc_semaphore("sem_" + e)
        dsems = {}
        for e in self.ENGS:
            if self.dma_count[e] > 0:
                dsems[e] = [nc.alloc_semaphore("dsem_%s_%d" % (e, i)) for i in range(self.n_dma_sems)]
        self.nwait = 0

        def emit_one(r, nd):
            eo = self.engobj[r.eng]
            for d in nd:
                if d.dma:
                    eo.wait_ge(dsems[d.eng][d.dmasem], d.dmaval)
                else:
                    eo.wait_ge(sems[d.eng], d.sv)
                self.nwait += 1
            ins = r.fn()
            if r.dma:
                ins.then_inc(dsems[r.eng][r.dmasem], 16)
            elif r.sig:
                ins.then_inc(sems[r.eng], 1)

        def emit_group(recs_):
            eng = recs_[0].eng
            eo = self.engobj[eng]
            key = recs_[0].cond[0]
            val = self.cond_vals[(key, eng)]
            levels = []
            for r in recs_:
                if not levels or levels[-1][0] != r.cond[1]:
                    levels.append((r.cond[1], []))
                levels[-1][1].append(r)

            def rec_level(li):
                if li == len(levels):
                    return
                c, rs = levels[li]
                nsig = sum(1 for lv in levels[li:] for r in lv[1] if r.sig)
                with eo.If(val > c):
                    for r in rs:
                        emit_one(r, final_deps[r.gidx])
                    rec_level(li + 1)
                with eo.Else():
                    if nsig > 0:
                        eo.drain()
                        eo.sem_inc(sems[eng], nsig)
            rec_level(0)

        done = set()
        for r, nd in zip(self.recs, final_deps):
            if r.gidx in done:
                continue
            g = grp_of.get(r.gidx)
            if g is not None:
                emit_group(groups[g])
                for x in groups[g]:
                    done.add(x.gidx)
            else:
                emit_one(r, nd)
        for (e, slot), r in self.dma_last.items():
            self.engobj[e].wait_ge(dsems[e][slot], r.dmaval)
        self.stats = dict(n=len(self.recs), waits=self.nwait, per_eng={e: len(v) for e, v in self.eng_recs.items()},
                          ngroups=len(groups))
        return self.stats

    def dma(self, eng, out, in_, **kw):
        o = self.engobj[eng]
        return self.op(eng, lambda: o.dma_start(out=out, in_=in_, **kw), reads=[in_], writes=[out], dma=True)

    def mm(self, out, lhsT, rhs, start=True, stop=True, **kw):
        t = self.nc.tensor
        return self.op("pe", lambda: t.matmul(out, lhsT, rhs, start=start, stop=stop, **kw), reads=[lhsT, rhs], writes=[out])

    def tr(self, out, in_, ident):
        t = self.nc.tensor
        return self.op("pe", lambda: t.transpose(out, in_, ident), reads=[in_, ident], writes=[out])

    def act(self, out, in_, func, bias=None, scale=None, accum_out=None):
        s = self.nc.scalar
        kw = {}
        rd = [in_]
        if bias is not None:
            kw["bias"] = bias
            if not isinstance(bias, (int, float)):
                rd.append(bias)
        if scale is not None:
            kw["scale"] = scale
            if not isinstance(scale, (int, float)):
                rd.append(scale)
        wr = [out]
        if accum_out is not None:
            kw["accum_out"] = accum_out
            wr.append(accum_out)
        return self.op("act", lambda: s.activation(out=out, in_=in_, func=func, **kw), reads=rd, writes=wr)

    def _veng(self, eng):
        return self.engobj[eng]

    def tt(self, eng, out, in0, in1, op):
        e = self._veng(eng)
        return self.op(eng, lambda: e.tensor_tensor(out=out, in0=in0, in1=in1, op=op), reads=[in0, in1], writes=[out])

    def ts(self, eng, out, in0, s1, s2=None, op0=ALU.mult, op1=None):
        e = self._veng(eng)
        rd = [in0]
        for s in (s1, s2):
            if s is not None and not isinstance(s, (int, float)):
                rd.append(s)
        kw = {}
        if op1 is not None:
            kw["op1"] = op1
        return self.op(eng, lambda: e.tensor_scalar(out=out, in0=in0, scalar1=s1, scalar2=s2, op0=op0, **kw), reads=rd, writes=[out])

    def stt(self, out, in0, scalar, in1, op0, op1, eng="dve"):
        e = self._veng(eng)
        rd = [in0, in1]
        if not isinstance(scalar, (int, float)):
            rd.append(scalar)
        return self.op(eng, lambda: e.scalar_tensor_tensor(out=out, in0=in0, scalar=scalar, in1=in1, op0=op0, op1=op1), reads=rd, writes=[out])

    def copy(self, eng, out, in_):
        if eng == "act":
            s = self.nc.scalar
            return self.op("act", lambda: s.copy(out=out, in_=in_), reads=[in_], writes=[out])
        e = self._veng(eng)
        return self.op(eng, lambda: e.tensor_copy(out=out, in_=in_), reads=[in_], writes=[out])

    def memset(self, eng, out, val):
        e = self._veng(eng)
        return self.op(eng, lambda: e.memset(out, val), reads=[], writes=[out])


def _fw_generic(self, eng, name, reads, writes, *args, **kw):
    e = self.engobj[eng]
    f = getattr(e, name)
    return self.op(eng, lambda: f(*args, **kw), reads=reads, writes=writes)


FW.gen = _fw_generic


import numpy as np
import ml_dtypes

D = 1024
T = 2048
NH = 24
DFF = 2816
NFF = 22
NEXP = 8
IN_COLS = 7686
QOFF, KOFF, VOFF, FOFF, GOFF = 0, 1536, 3072, 4608, 4614
EPS = 1e-6
SLOPES = (2.0 ** (-8.0 * np.arange(1, 19) / 18)).astype(np.float32)
DIL = ((128, 1), (512, 4), (2048, 16))
DIL_SO = (0, 4, 14)
MOBA_SO = 8
NEGM = -30000.0


def split3(v):
    v = np.asarray(v, np.float32)
    hi = v.astype(ml_dtypes.bfloat16)
    r1 = v - hi.astype(np.float32)
    mid = r1.astype(ml_dtypes.bfloat16)
    r2 = r1 - mid.astype(np.float32)
    lo = r2.astype(ml_dtypes.bfloat16)
    return hi, mid, lo


def host_consts():
    c = {}
    c["ident_f"] = np.eye(128, dtype=np.float32)
    c["ident_b"] = np.eye(128, dtype=np.float32).astype(ml_dtypes.bfloat16)
    ob = np.zeros((128, 128), np.float32)
    ob[0:64, 0:64] = 1.0
    c["onesblk"] = ob.astype(ml_dtypes.bfloat16)
    c["ones_b"] = np.ones((128, 128), np.float32).astype(ml_dtypes.bfloat16)
    c["ones_f"] = np.ones((128, 128), np.float32)
    k = np.arange(128)[:, None]
    q = np.arange(128)[None, :]
    c["tri"] = (q >= k).astype(np.float32).astype(ml_dtypes.bfloat16)
    db = np.zeros((128, 12, 256), np.float32)
    for g, (w, d) in enumerate(DIL):
        for j in range(4):
            sl = SLOPES[DIL_SO[g] + j]
            left = np.where(q >= k, -sl * d * (q - k), NEGM)
            right = np.where(k >= q, -sl * d * (128 + q - k), NEGM)
            db[:, g * 4 + j, 0:128] = left
            db[:, g * 4 + j, 128:256] = right
    c["dilbias"] = db.reshape(128, 12 * 256)
    t = np.arange(T, dtype=np.float32)
    aq = np.zeros((6, 6, T), ml_dtypes.bfloat16)
    ak = np.zeros((6, 6, T), ml_dtypes.bfloat16)
    for h in range(6):
        sl = SLOPES[MOBA_SO + h]
        qh = split3(-8.0 * sl * t)
        kh = split3(8.0 * sl * t)
        for i in range(3):
            aq[h, i] = qh[i]
            aq[h, 3 + i] = 1.0
            ak[h, i] = 1.0
            ak[h, 3 + i] = kh[i]
    c["moba_aq"] = aq
    c["moba_ak"] = ak
    bi = np.zeros((8, T), np.float32)
    for b in range(8):
        bi[b, b * 256:(b + 1) * 256] = 1.0
    c["blkind"] = bi.astype(ml_dtypes.bfloat16)
    pos = (np.arange(16)[None, :, None] * 128 + np.arange(128)[:, None, None])
    own = pos // 256
    B = np.arange(8)[None, None, :]
    c["mg_sb1"] = np.where(B < own, 0.0, -1e30).astype(np.float32).reshape(128, 128)
    c["mg_v1"] = (B < own).astype(np.float32).reshape(128, 128)
    c["mg_e"] = (B == own).astype(np.float32).reshape(128, 128)
    c["fox_ones"] = np.ones((3, T), np.float32).astype(ml_dtypes.bfloat16)
    c["ustrict"] = np.triu(np.ones((128, 128), np.float32), 1).astype(ml_dtypes.bfloat16)
    c["ebase"] = np.tile(np.arange(8, dtype=np.float32) * 2048.0, (128, 16))
    return c


CONST_DT = dict(ident_f=F32, ident_b=BF16, onesblk=BF16, ones_b=BF16, ones_f=F32, tri=BF16, dilbias=F32,
                moba_aq=BF16, moba_ak=BF16, blkind=BF16, mg_sb1=F32, mg_v1=F32, mg_e=F32, fox_ones=BF16, ustrict=BF16, ebase=F32)


def host_layout(inp, b):
    m = {}
    m["x"] = np.ascontiguousarray(inp["x"][b])
    m["cT"] = np.ascontiguousarray(inp["c"][b].reshape(8, 128).T)
    m["b_adaT"] = np.ascontiguousarray(inp["b_ada"].reshape(2, 48, 128).transpose(2, 0, 1))
    m["nmixT"] = np.ascontiguousarray(inp["norm_mix"].reshape(2, 8, 128).transpose(2, 0, 1))
    m["nffnT"] = np.ascontiguousarray(inp["norm_ffn"].reshape(2, 8, 128).transpose(2, 0, 1))
    m["qgT"] = np.ascontiguousarray(inp["q_gain"].transpose(2, 0, 1))
    m["kgT"] = np.ascontiguousarray(inp["k_gain"].transpose(2, 0, 1))
    m["bfg"] = np.ascontiguousarray(inp["b_fgate"].T)
    m["b_router"] = np.ascontiguousarray(inp["b_router"])
    for k in ("w_ada", "w_in", "w_br_fox", "w_br_moba", "w_br_dil", "w_out", "w_ffn_gate", "w_ffn_up",
              "w_ffn_down", "w_router", "w_exp_gate", "w_exp_up", "w_exp_down"):
        m[k] = inp[k]
    return m


IN_SHAPES = dict(
    x=[T, D], cT=[128, 8], b_adaT=[128, 2, 48], nmixT=[128, 2, 8], nffnT=[128, 2, 8], qgT=[64, 2, 24], kgT=[64, 2, 24],
    bfg=[6, 2], b_router=[1, 8], w_ada=[2, D, 6 * D], w_in=[2, D, IN_COLS], w_br_fox=[2, 384, D], w_br_moba=[2, 384, D],
    w_br_dil=[2, 256, D], w_out=[2, D, D], w_ffn_gate=[1, D, DFF], w_ffn_up=[1, D, DFF], w_ffn_down=[1, DFF, D],
    w_router=[1, D, 8], w_exp_gate=[1, 8, D, DFF], w_exp_up=[1, 8, D, DFF], w_exp_down=[1, 8, DFF, D])


def prod(xs):
    r = 1
    for x in xs:
        r *= x
    return r


A_OT = 0
A_Y = 32768
A_VA = 32768
A_VB = 32768 + 12544
A_QK = 57856
A_X = 65536
A_WQK = 74240
A_WV = 82432
A_ACCN = 88576
A_ODD = 96768
A_PT = 100864
A_Q2 = 103936
A_RQ = 105984
A_RD = 110080
A_BC = 112128
A_MG = 114176
A_KM = 118272
A_FST = 65536
A_Z = 131072
A_DILM = 131072
A_Z2 = 137216
A_END = 151552
A_ACT = 0
A_WGU = 45056
A_WD = 53248
A_SG = 58880
A_CBC = A_Z2
A_RT = A_Z2 + 4096


class K:
    def __init__(self, debug=None, nlayers=2, stop_after=None, heads=None, sparse=True):
        self.debug = debug or []
        self.nlayers = nlayers
        self.stop_after = stop_after
        self.heads = heads
        nc = bass.Bass("TRN2", target_bir_lowering=False)
        self.nc = nc
        self.fw = FW(nc)
        self.inp = {}
        for k, shp in IN_SHAPES.items():
            self.inp[k] = nc.dram_tensor(k, shp, F32, kind="ExternalInput").ap()
        self.cst = {}
        hc = host_consts()
        self.hc = hc
        for k, v in hc.items():
            self.cst[k] = nc.dram_tensor("c_" + k, list(v.shape), CONST_DT[k], kind="ExternalInput").ap()
        self.out = nc.dram_tensor("out", [T, D], F32, kind="ExternalOutput").ap()
        self.xs = nc.dram_tensor("xs_scr", [128, 8 * T], F32, kind="Internal").ap()
        self.fsc = nc.dram_tensor("f_scr", [2, 6, 3 * T], BF16, kind="Internal").ap()
        self.HS = nc.dram_tensor("hs_scr", [NEXP * T, 1026], BF16, kind="Internal").ap()
        self.YS = nc.dram_tensor("ys_scr", [NEXP * T, D], BF16, kind="Internal").ap()
        self.XTOK = nc.dram_tensor("xtok_scr", [T, D], F32, kind="Internal").ap()
        self.sparse = sparse
        self.dbg = {}
        self._rr = 0
        self._cnt = 0
        self.build()

    def sb(self, name, shape, dt):
        return self.nc.alloc_sbuf_tensor("s_" + name, shape, dt)

    def carve(self, off, shape, dt):
        n = prod(shape[1:])
        nb = n * dtsize(dt)
        assert off % 4 == 0 and off + nb <= A_END, (off, nb)
        v = self.AR[0:shape[0], off // 2:(off + nb) // 2]
        if dt != BF16:
            v = v.bitcast(dt)
        if len(shape) == 3:
            v = v.rearrange("p (a b) -> p a b", a=shape[1])
        elif len(shape) == 4:
            v = v.rearrange("p (a b c) -> p a b c", a=shape[1], b=shape[2])
        return v

    def dbg_out(self, name, shape, dt=F32):
        t = self.nc.dram_tensor("dbg_" + name, shape, dt, kind="ExternalOutput").ap()
        self.dbg[name] = t
        return t

    def evac_eng(self):
        self._rr += 1
        return "act" if self._rr % 2 else "dve"

    def wview(self, w2d, c0, ncols):
        return w2d.rearrange("(kc p) n -> p kc n", p=128)[:, :, c0:c0 + ncols]

    def build(self):
        nc, fw = self.nc, self.fw
        C = {}
        for k in ("ident_f", "ident_b", "onesblk", "ones_b", "ones_f", "tri", "mg_sb1", "mg_v1", "mg_e", "ustrict", "ebase"):
            v = self.hc[k]
            C[k] = self.sb("k_" + k, list(v.shape), CONST_DT[k])
            fw.dma("sp", C[k][:], self.cst[k][:, :])
        self.C = C
        self.cT = self.sb("cT", [128, 8], F32)
        fw.dma("sp", self.cT[:], self.inp["cT"][:, :])
        self.b_adaT = self.sb("b_adaT", [128, 2, 48], F32)
        fw.dma("sp", self.b_adaT[:], self.inp["b_adaT"][:, :, :])
        self.nmixT = self.sb("nmixT", [128, 2, 8], F32)
        fw.dma("sp", self.nmixT[:], self.inp["nmixT"][:, :, :])
        self.nffnT = self.sb("nffnT", [128, 2, 8], F32)
        fw.dma("sp", self.nffnT[:], self.inp["nffnT"][:, :, :])
        self.qg = self.sb("qg", [64, 2, 24], F32)
        fw.dma("sp", self.qg[:], self.inp["qgT"][:, :, :])
        self.kg = self.sb("kg", [64, 2, 24], F32)
        fw.dma("sp", self.kg[:], self.inp["kgT"][:, :, :])
        self.bfg = self.sb("bfg", [6, 2], F32)
        fw.dma("sp", self.bfg[:], self.inp["bfg"][:, :])
        self.negb = self.sb("negb", [6, 2], F32)
        fw.ts("dve", self.negb[:], self.bfg[:], -1.0, None, op0=ALU.mult)
        self.AR = self.sb("arena", [128, A_END // 2], BF16)
        self.hT = self.sb("hT", [128, 8, T], BF16)
        self.xT = self.carve(A_X, [128, 8, T], F32)
        self.psb = [nc.alloc_psum_tensor("psb%d" % i, [128, 512], F32) for i in range(8)]
        self.cond = self.sb("cond", [128, 8], BF16)
        self.modT = self.sb("modT", [128, 2, 48], F32)
        self.a1 = self.sb("a1", [128, 2, 8], F32)
        self.a2 = self.sb("a2", [128, 2, 8], F32)
        self.sqb = [self.sb("sqb%d" % i, [128, 512], BF16) for i in range(3)]
        self.rstd = [self.sb("rstd%d" % i, [128, 512], F32) for i in range(2)]
        self.ntmp = [self.sb("ntmp%d" % i, [128, 512], F32) for i in range(3)]
        self.OT = [self.carve(A_OT + i * 4096, [128, T], BF16) for i in range(8)]
        self.VA = self.carve(A_VA, [128, 16, 6, 65], BF16)
        self.VB = self.carve(A_VB, [128, 16, 6, 65], BF16)
        self.Qb = [self.carve(A_QK + (2 * s) * 4096, [128, T], BF16) for s in range(2)]
        self.Kb = [self.carve(A_QK + (2 * s + 1) * 4096, [128, T], BF16) for s in range(2)]
        self.wqk = [self.carve(A_WQK + s * 4096, [128, 8, 256], BF16) for s in range(2)]
        self.wv = self.carve(A_WV, [128, 8, 384], BF16)
        self.accN = self.carve(A_ACCN, [128, T], F32)
        self.oddt = self.carve(A_ODD, [128, T], BF16)
        self.ptb = [self.carve(A_PT + i * 1024, [128, 512], BF16) for i in range(3)]
        self.q2b = [self.carve(A_Q2 + i * 1024, [128, 512], BF16) for i in range(2)]
        self.rqb = [self.carve(A_RQ + i * 2048, [128, 512], F32) for i in range(2)]
        self.rd = self.carve(A_RD, [128, 512], F32)
        self.bcs = self.carve(A_BC, [128, 512], F32)
        self.dilM = self.carve(A_DILM, [128, 12, 256], BF16)
        self.rbt4 = [self.carve(119296 + i * 1024, [128, 512], BF16) for i in range(4)]
        self.rbt = self.rbt4[0:2]
        self.rlt = [self.carve(119296 + 4096 + i * 2048, [128, 512], F32) for i in range(2)]

        self.stage_load_x()
        self.stage_adaln()
        for l in range(self.nlayers):
            self.stage_rmsnorm(l, 0)
            if self.stop_after == "norm1":
                break
            fw.dma("sp", self.xs[:, :], self.xT.rearrange("p a b -> p (a b)"))
            if l == 0:
                self.stage_dilmask()
            self.stage_attention(l)
            if self.stop_after == "attn":
                break
            fw.dma("sp", self.xT.rearrange("p a b -> p (a b)"), self.xs[:, :])
            self.stage_outproj(l)
            if self.stop_after == "outproj":
                break
            self.stage_rmsnorm(l, 1, rt_ps=(self.psb[7] if l % 2 == 1 else None))
            if l % 2 == 0:
                self.ffn_expert(l, self.inp["w_ffn_gate"][0], self.inp["w_ffn_up"][0], self.inp["w_ffn_down"][0], None)
                if self.sparse and self.stop_after is None and l == 0:
                    zt = self.carve(A_VB, [128, 4104], BF16)
                    fw.memset("pool", zt, 0.0)
                    hsv = self.HS.rearrange("(p r) c -> p (r c)", p=128)
                    for i in range(32):
                        fw.dma("sp", hsv[:, i * 4104:(i + 1) * 4104], zt)
            elif self.sparse and l == self.nlayers - 1 and self.stop_after is None:
                self.stage_moe_sparse(l)
                self.final_done = True
            else:
                self.stage_moe(l)
            if self.stop_after == "ffn":
                break
        if self.stop_after is None and not getattr(self, "final_done", False):
            self.stage_output()
        for nm in self.debug:
            if nm == "hT":
                o = self.dbg_out("hT", [128, 8, T], BF16)
                fw.dma("sp", o[:, :, :], self.hT[:])
            elif nm == "xT":
                o = self.dbg_out("xT", [128, 8, T], F32)
                fw.dma("sp", o[:, :, :], self.xT)
            elif nm == "OT":
                o = self.dbg_out("OT", [8, 128, T], BF16)
                for i in range(8):
                    fw.dma("sp", o[i], self.OT[i])
            elif nm == "modT":
                o = self.dbg_out("modT", [128, 2, 48], F32)
                fw.dma("sp", o[:, :, :], self.modT[:])
        self.stats = fw.emit()

    def stage_load_x(self):
        fw, C = self.fw, self.C
        xin = [self.carve(i * 4096, [128, D], F32) for i in range(4)]
        for g in range(4):
            for i in range(4):
                tt = g * 4 + i
                fw.dma("sp", xin[i], self.inp["x"][tt * 128:(tt + 1) * 128, :])
            for dc in range(8):
                pb = self.psb[dc % 4]
                for i in range(4):
                    fw.tr(pb[:, i * 128:(i + 1) * 128], xin[i][:, dc * 128:(dc + 1) * 128], C["ident_f"][:])
                fw.copy(self.evac_eng(), self.xT[:, dc, g * 512:(g + 1) * 512], pb[:, :])

    def stage_adaln(self):
        fw, C = self.fw, self.C
        fw.act(self.cond[:], self.cT[:], AF.Silu)
        wbuf = [self.carve(i * 8192, [128, 8, 512], BF16) for i in range(2)]
        n = 0
        for l in range(self.nlayers):
            pm = self.psb[4 + l]
            wv = self.inp["w_ada"][l].rearrange("(kc p) n -> p kc n", p=128)
            for half in range(12):
                wb = wbuf[n % 2]
                n += 1
                fw.dma("pool", wb, wv[:, :, half * 512:(half + 1) * 512])
                for c4 in range(4):
                    j = half * 4 + c4
                    for kc in range(8):
                        fw.mm(pm[:, j:j + 1], wb[:, kc, c4 * 128:(c4 + 1) * 128], self.cond[:, kc:kc + 1],
                              start=(kc == 0), stop=(kc == 7))
            fw.tt("dve", self.modT[:, l, :], pm[:, 0:48], self.b_adaT[:, l, :], ALU.add)
            fw.stt(self.a1[:, l, :], self.modT[:, l, 8:16], 1.0, self.nmixT[:, l, :], ALU.add, ALU.mult)
            fw.stt(self.a2[:, l, :], self.modT[:, l, 32:40], 1.0, self.nffnT[:, l, :], ALU.add, ALU.mult)

    def stage_rmsnorm(self, l, which, rt_ps=None):
        fw, C = self.fw, self.C
        a = self.a1 if which == 0 else self.a2
        shoff = 0 if which == 0 else 24
        for tt in range(4):
            cs = slice(tt * 512, (tt + 1) * 512)
            pss = self.psb[tt % 2]
            for dc in range(8):
                sq = self.sqb[dc % 3]
                fw.act(sq[:], self.xT[:, dc, cs], AF.Square)
                fw.mm(pss[:, :], C["ones_b"][:], sq[:], start=(dc == 0), stop=(dc == 7))
            rs = self.rstd[tt % 2]
            fw.act(rs[:], pss[:, :], AF.Ln, bias=EPS, scale=1.0 / D)
            fw.act(rs[:], rs[:], AF.Exp, scale=-0.5)
            if rt_ps is not None:
                for i in range(4):
                    fw.mm(rt_ps[:, tt * 4 + i:tt * 4 + i + 1], rs[0:1, i * 128:(i + 1) * 128], C["ones_f"][0:1, 0:1])
            for dc in range(8):
                nt = self.ntmp[dc % 3]
                fw.stt(nt[:], self.xT[:, dc, cs], a[:, l, dc:dc + 1], rs[:], ALU.mult, ALU.mult)
                fw.act(self.hT[:, dc, cs], nt[:], AF.Identity, bias=self.modT[:, l, shoff + dc:shoff + dc + 1])

    def stage_dilmask(self):
        fw = self.fw
        tmp = self.carve(A_X, [128, 3072], F32)
        fw.dma("sp", tmp, self.cst["dilbias"][:, :])
        fw.act(self.dilM.rearrange("p a b -> p (a b)"), tmp, AF.Exp)

    def fox_prep(self, l):
        fw = self.fw
        wf = self.carve(A_Z2, [128, 8, 6], BF16)
        fw.dma("pool", wf, self.wview(self.inp["w_in"][l], FOFF, 6))
        fl = self.carve(A_FST, [6, T], F32)
        G = self.carve(A_FST + 8192, [6, T], F32)
        r1 = self.carve(A_FST + 16384, [6, T], F32)
        kp = self.carve(A_FST + 24576, [6, 3, T], BF16)
        qp = self.carve(A_FST + 36864, [6, 3, T], BF16)
        for tt in range(4):
            cs = slice(tt * 512, (tt + 1) * 512)
            ps = self.psb[tt % 2]
            for kc in range(8):
                fw.mm(ps[0:6, :], wf[:, kc, :], self.hT[:, kc, cs], start=(kc == 0), stop=(kc == 7))
            fw.act(fl[:, cs], ps[0:6, :], AF.Exp, scale=-1.0, bias=self.negb[0:6, l:l + 1])
        fw.act(fl, fl, AF.Ln, bias=1.0)
        fw.memset("dve", r1, 1.0)
        fw.gen("dve", "tensor_tensor_scan", [fl, r1], [G], out=G, data0=r1, data1=fl, initial=0.0,
               op0=ALU.mult, op1=ALU.add)
        fw.ts("dve", kp[:, 0, :], G, 8.0, None, op0=ALU.mult)
        fw.stt(r1, G, 8.0, kp[:, 0, :], ALU.mult, ALU.subtract)
        fw.copy("dve", kp[:, 1, :], r1)
        fw.tt("dve", r1, r1, kp[:, 1, :], ALU.subtract)
        fw.copy("dve", kp[:, 2, :], r1)
        kpf = kp.rearrange("p a b -> p (a b)")
        qpf = qp.rearrange("p a b -> p (a b)")
        fw.ts("dve", qpf, kpf, -1.0, None, op0=ALU.mult)
        fw.dma("sp", self.fsc[0], qpf)
        fw.dma("sp", self.fsc[1], kpf)

    def proj_qk(self, l, h, s, d):
        fw, C = self.fw, self.C
        w = self.wqk[s]
        Wl = self.inp["w_in"][l]
        fw.dma("pool", w[:, :, 0:128], self.wview(Wl, QOFF + h * 64, 128))
        fw.dma("pool", w[:, :, 128:256], self.wview(Wl, KOFF + h * 64, 128))
        fw.memset("pool", self.Qb[s][64:128, :], 0.0)
        fw.memset("pool", self.Kb[s][64:128, :], 0.0)
        for which, dst, gain in ((0, self.Qb[s], self.qg), (1, self.Kb[s], self.kg)):
            for tt in range(4):
                cs = slice(tt * 512, (tt + 1) * 512)
                ps = self.psb[tt % 2]
                for kc in range(8):
                    fw.mm(ps[:, :], w[:, kc, which * 128:(which + 1) * 128], self.hT[:, kc, cs],
                          start=(kc == 0), stop=(kc == 7))
                q2 = self.q2b[tt % 2]
                fw.act(q2, ps[:, :], AF.Square)
                p2 = self.psb[2]
                fw.mm(p2[:, :], C["onesblk"][:], q2, start=True, stop=True)
                rq = self.rqb[tt % 2]
                fw.act(rq[0:64, :], p2[0:64, :], AF.Ln, bias=EPS, scale=1.0 / 64)
                fw.act(rq[0:64, :], rq[0:64, :], AF.Exp, scale=-0.5)
                if d == 1:
                    o, i0, i1 = dst[0:64, cs], ps[0:64, :], rq[0:64, :]
                else:
                    n0, n1 = tt * 512 // d, (tt + 1) * 512 // d
                    o = dst[0:64, :].rearrange("p (r n) -> p n r", r=d)[:, n0:n1, :]
                    i0 = ps[0:64, :].rearrange("p (n r) -> p n r", r=d)
                    i1 = rq[0:64, :].rearrange("p (n r) -> p n r", r=d)
                fw.stt(o, i0, gain[:, l, h:h + 1], i1, ALU.mult, ALU.mult)

    def load_wv(self, l, grp):
        self.fw.dma("pool", self.wv, self.wview(self.inp["w_in"][l], VOFF + grp * 384, 384))

    def proj_v(self, nh, d, Vbuf, slot0, wcol0):
        fw = self.fw
        L = T // d
        nblk = L // 128
        for pb in range(16):
            r, nb = pb // nblk, pb % nblk
            ps = self.psb[pb % 2]
            if d == 1:
                tok = slice(pb * 128, (pb + 1) * 128)
            else:
                st0 = nb * 128 * d + r
                tok = slice(st0, st0 + 127 * d + 1, d)
            for kc in range(8):
                fw.mm(ps[:, 0:nh * 64], self.hT[:, kc, tok], self.wv[:, kc, wcol0:wcol0 + nh * 64],
                      start=(kc == 0), stop=(kc == 7))
            fw.copy(self.evac_eng(), Vbuf[:, pb, slot0:slot0 + nh, 0:64],
                    ps[:, 0:nh * 64].rearrange("p (h e) -> p h e", h=nh))

    def recip_row(self, den, k, eng):
        fw = self.fw
        rb = self.rbt[k % 2] if eng == "dve" else self.rbt4[k % 4]
        if eng == "dve":
            nc = self.nc
            o_ = rb[64:65, :]

            def f(o_=o_, den=den):
                with nc.allow_low_precision("bf16 reciprocal row feeds a bf16 broadcast matmul"):
                    return nc.vector.reciprocal(out=o_, in_=den)
            fw.op("dve", f, reads=[den], writes=[o_])
        else:
            lt = self.rlt[k % 2]
            fw.act(lt[64:65, :], den, AF.Ln)
            fw.act(rb[64:65, :], lt[64:65, :], AF.Exp, scale=-1.0)
        return rb

    def bcast_mul(self, src, rb, dest):
        fw, C = self.fw, self.C
        bp = self.psb[2]
        fw.mm(bp[:, :], C["ones_b"][64:65, :], rb[64:65, :], start=True, stop=True)
        fw.tt("dve", dest, src, bp[0:64, :], ALU.mult)

    def attn_causal(self, s, Vbuf, vslot, dest, hook=None, pending=None):
        fw, C = self.fw, self.C
        Qh, Kh = self.Qb[s], self.Kb[s]
        otS = (self.rd, self.bcs)
        for j in range(4):
            ot = self.psb[6 + j % 2]
            nkb = 4 * j + 4

            def S(kb):
                c0 = max(0, kb - 4 * j) * 128
                st = self.psb[3 + kb % 3]
                fw.mm(st[:, c0:512], Kh[:, kb * 128:(kb + 1) * 128], Qh[:, j * 512 + c0:(j + 1) * 512],
                      start=True, stop=True)
            S(0)
            if nkb > 1:
                S(1)
            for kb in range(nkb):
                if kb + 2 < nkb:
                    S(kb + 2)
                c0 = max(0, kb - 4 * j) * 128
                st = self.psb[3 + kb % 3]
                pt = self.ptb[kb % 3]
                fw.act(pt[:, c0:512], st[:, c0:512], AF.Exp, scale=0.125)
                if kb >= 4 * j:
                    fw.tt("pool", pt[:, c0:c0 + 128], pt[:, c0:c0 + 128], C["tri"][:], ALU.mult)
                fw.mm(ot[0:65, c0:512], Vbuf[:, kb, vslot, 0:65], pt[:, c0:512], start=(kb == 0), stop=(kb == nkb - 1))
                if kb == min(5, nkb - 1) and pending is not None:
                    pending()
                    pending = None
                if hook is not None and j == 2 and kb == 5:
                    hook()
                    hook = None
            if pending is not None:
                pending()
                pending = None

            o = otS[j % 2]
            fw.copy("act", o[0:65, :], ot[0:65, :])
            rb = self.recip_row(o[64:65, :], j, "dve")

            def fin(j=j, o=o, rb=rb):
                self.bcast_mul(o[0:64, :], rb, dest[:, j * 512:(j + 1) * 512])
            pending = fin
        return pending

    def fox_aug(self, h, s):
        fw = self.fw
        Qh, Kh = self.Qb[s], self.Kb[s]
        fw.dma("sp", Qh[64:67, :], self.fsc[0, h].rearrange("(a t) -> a t", a=3))
        fw.dma("sp", Qh[67:70, :], self.cst["fox_ones"][:, :])
        fw.dma("sp", Kh[64:67, :], self.cst["fox_ones"][:, :])
        fw.dma("sp", Kh[67:70, :], self.fsc[1, h].rearrange("(a t) -> a t", a=3))

    def moba_prep(self, h6, s):
        fw, C = self.fw, self.C
        Qh, Kh = self.Qb[s], self.Kb[s]
        km = self.carve(A_KM, [128, 8], F32)
        kmb = self.carve(A_KM + 64, [128, 8], BF16)
        fw.gen("dve", "tensor_reduce", [Kh[0:64, :]], [km[0:64, :]], out=km[0:64, :],
               in_=Kh[0:64, :].rearrange("p (b n) -> p b n", b=8), axis=AX.X, op=ALU.add)
        fw.ts("dve", kmb[0:64, :], km[0:64, :], 1.0 / 256, None, op0=ALU.mult)
        gp = self.psb[2]
        for qb in range(16):
            fw.mm(gp[:, qb * 8:(qb + 1) * 8], Qh[0:64, qb * 128:(qb + 1) * 128], kmb[0:64, :], start=True, stop=True)
        gm = self.carve(A_MG, [128, 128], F32)
        m8 = self.carve(A_MG + 512, [128, 128], F32)
        sel = self.carve(A_MG + 1024, [128, 128], F32)
        mbb = self.carve(A_MG + 1536, [128, 128], BF16)
        fw.tt("dve", gm, gp[:, 0:128], C["mg_sb1"][:], ALU.add)
        for qb in range(16):
            sl = slice(qb * 8, (qb + 1) * 8)
            fw.gen("dve", "max", [gm[:, sl]], [m8[:, sl]], out=m8[:, sl], in_=gm[:, sl])
        gm3 = gm.rearrange("p (a b) -> p a b", b=8)
        m83 = m8.rearrange("p (a b) -> p a b", b=8)
        sel3 = sel.rearrange("p (a b) -> p a b", b=8)
        fw.tt("dve", sel3, gm3, m83[:, :, 2:3].to_broadcast([128, 16, 8]), ALU.is_ge)
        fw.tt("dve", sel, sel, C["mg_v1"][:], ALU.mult)
        fw.tt("dve", sel, sel, C["mg_e"][:], ALU.add)
        fw.ts("dve", mbb, sel, 240000.0, -240000.0, op0=ALU.mult, op1=ALU.add)
        for tt in range(4):
            pp = self.psb[tt % 2]
            for i in range(4):
                qb = tt * 4 + i
                fw.mm(pp[64:72, i * 128:(i + 1) * 128], mbb[:, qb * 8:(qb + 1) * 8], C["ident_b"][:], start=True, stop=True)
            fw.copy(self.evac_eng(), Qh[64:72, tt * 512:(tt + 1) * 512], pp[64:72, :])
        fw.dma("sp", Qh[72:78, :], self.cst["moba_aq"][h6])
        fw.dma("sp", Kh[64:72, :], self.cst["blkind"][:, :])
        fw.dma("sp", Kh[72:78, :], self.cst["moba_ak"][h6])

    def attn_dil(self, g, j, s, Vbuf, vslot, first, hook=None, pending=None):
        fw = self.fw
        d = DIL[g][1]
        L = T // d
        nblk = L // 128
        Qh, Kh = self.Qb[s], self.Kb[s]
        M = self.dilM[:, g * 4 + j, :]
        accN = self.accN

        def S(pbq):
            nb = pbq % nblk
            st = self.psb[3 + pbq % 3]
            qs = slice(pbq * 128, (pbq + 1) * 128)
            fw.mm(st[:, 0:128], Kh[:, qs], Qh[:, qs], start=True, stop=True)
            if nb > 0:
                fw.mm(st[:, 128:256], Kh[:, (pbq - 1) * 128:pbq * 128], Qh[:, qs], start=True, stop=True)
        S(0)
        S(1)
        for c in range(4):
            ot = self.psb[6 + c % 2]
            for i in range(4):
                pbq = c * 4 + i
                nb = pbq % nblk
                if pbq + 2 < 16:
                    S(pbq + 2)
                st = self.psb[3 + pbq % 3]
                pt = self.ptb[pbq % 3]
                w = 256 if nb > 0 else 128
                fw.act(pt[:, 0:w], st[:, 0:w], AF.Exp, scale=0.125)
                fw.tt("pool", pt[:, 0:w], pt[:, 0:w], M[:, 0:w], ALU.mult)
                oc = ot[0:65, i * 128:(i + 1) * 128]
                fw.mm(oc, Vbuf[:, pbq, vslot, 0:65], pt[:, 0:128], start=True, stop=(nb == 0))
                if nb > 0:
                    fw.mm(oc, Vbuf[:, pbq - 1, vslot, 0:65], pt[:, 128:256], start=False, stop=True)
                if pending is not None and pbq == 2:
                    pending()
                    pending = None
            if d == 1:
                dst, src = accN[0:65, c * 512:(c + 1) * 512], ot[0:65, :]
            elif d == 4:
                dst, src = accN[0:65, c:c + 4 * 511 + 1:4], ot[0:65, :]
            else:
                dst = accN[0:65, :].rearrange("p (n r) -> p r n", r=16)[:, 4 * c:4 * c + 4, :]
                src = ot[0:65, :].rearrange("p (r n) -> p r n", r=4)
            if first:
                fw.copy("dve", dst, src)
            else:
                fw.tt("dve", dst, src, dst, ALU.add)
            if hook is not None and c == 1:
                hook()
                hook = None

    def stage_attention(self, l):
        fw = self.fw
        heads = self.heads or ("fox", "moba", "dil")
        fw.memset("pool", self.VA[:, :, :, 64:65], 1.0)
        fw.memset("pool", self.VB[:, :, :, 64:65], 1.0)
        jobs = []
        if "fox" in heads:
            self.fox_prep(l)
            for h in range(6):
                s = h % 2

                def prep(h=h, s=s):
                    if h == 0:
                        self.load_wv(l, 0)
                        self.proj_v(6, 1, self.VA, 0, 0)
                    self.proj_qk(l, h, s, 1)
                    self.fox_aug(h, s)

                def attn(hook, pending, h=h, s=s):
                    dest = self.OT[h // 2][0:64, :] if h % 2 == 0 else self.oddt[0:64, :]
                    p = self.attn_causal(s, self.VA, h, dest, hook=hook, pending=pending)
                    if h % 2 == 1:
                        def fin2(p=p, h=h):
                            p()
                            fw.dma("sp", self.OT[h // 2][64:128, :], self.oddt[0:64, :])
                        return fin2
                    return p
                jobs.append((prep, attn))
        if "moba" in heads:
            for h6 in range(6):
                h = 6 + h6
                s = h % 2

                def prep(h=h, h6=h6, s=s):
                    if h6 == 0:
                        self.load_wv(l, 1)
                        self.proj_v(6, 1, self.VB, 0, 0)
                    self.proj_qk(l, h, s, 1)
                    self.moba_prep(h6, s)

                def attn(hook, pending, h6=h6, s=s):
                    dest = self.OT[3 + h6 // 2][0:64, :] if h6 % 2 == 0 else self.oddt[0:64, :]
                    p = self.attn_causal(s, self.VB, h6, dest, hook=hook, pending=pending)
                    if h6 % 2 == 1:
                        def fin2(p=p, h6=h6):
                            p()
                            fw.dma("sp", self.OT[3 + h6 // 2][64:128, :], self.oddt[0:64, :])
                        return fin2
                    return p
                jobs.append((prep, attn))
        if "dil" in heads:
            n = 0
            for j in range(4):
                for g in range(3):
                    h = 12 + g * 4 + j
                    Vbuf, vslot = (self.VA, h - 12) if h < 18 else (self.VB, h - 18)
                    s = n % 2
                    first_dil = (n == 0)
                    n += 1

                    def prep(h=h, s=s, g=g, first_dil=first_dil):
                        if first_dil:
                            self.load_wv(l, 2)
                            self.proj_v(4, 1, self.VA, 0, 0)
                            self.proj_v(2, 4, self.VA, 4, 256)
                        if h == 20:
                            self.load_wv(l, 3)
                            self.proj_v(2, 4, self.VB, 0, 0)
                            self.proj_v(4, 16, self.VB, 2, 128)
                        self.proj_qk(l, h, s, DIL[g][1])

                    def attn(hook, pending, g=g, j=j, s=s, Vbuf=Vbuf, vslot=vslot):
                        self.attn_dil(g, j, s, Vbuf, vslot, g == 0, hook=hook, pending=pending)
                        if g == 2:
                            dest = self.OT[6 + j // 2][0:64, :] if j % 2 == 0 else self.oddt[0:64, :]
                            rbs = []
                            for tt in range(4):
                                cs = slice(tt * 512, (tt + 1) * 512)
                                rbs.append(self.recip_row(self.accN[64:65, cs], tt, "act"))

                            def fin(j=j, dest=dest):
                                for tt in range(4):
                                    cs = slice(tt * 512, (tt + 1) * 512)
                                    rb = self.rbt4[tt]
                                    self.bcast_mul(self.accN[0:64, cs], rb, dest[:, cs])
                                if j % 2 == 1:
                                    fw.dma("sp", self.OT[6 + j // 2][64:128, :], self.oddt[0:64, :])
                            return fin
                        return None
                    jobs.append((prep, attn))
        pending = None
        if jobs:
            jobs[0][0]()
        for i, (prep, attn) in enumerate(jobs):
            hook = jobs[i + 1][0] if i + 1 < len(jobs) else None
            pending = attn(hook, pending)
        if pending is not None:
            pending()

    def stage_outproj(self, l):
        fw = self.fw
        yT = [self.carve(A_Y + i * 4096, [128, T], BF16) for i in range(8)]
        wbr = [self.carve(A_Z2 + i * 2048, [128, 8, 128], BF16) for i in range(2)]
        wg = [self.carve(A_Z2 + 4096 + i * 2048, [128, 8, 128], BF16) for i in range(4)]
        Wl = self.inp["w_in"][l]
        ng = 0
        for fc in range(8):
            fs = slice(fc * 128, (fc + 1) * 128)
            wb = wbr[fc % 2]
            fw.dma("pool", wb[:, 0:3, :], self.inp["w_br_fox"][l].rearrange("(kc p) n -> p kc n", p=128)[:, :, fs])
            fw.dma("pool", wb[:, 3:6, :], self.inp["w_br_moba"][l].rearrange("(kc p) n -> p kc n", p=128)[:, :, fs])
            fw.dma("pool", wb[:, 6:8, :], self.inp["w_br_dil"][l].rearrange("(kc p) n -> p kc n", p=128)[:, :, fs])
            wgs = []
            for br in range(3):
                w = wg[ng % 4]
                ng += 1
                fw.dma("pool", w, self.wview(Wl, GOFF + br * 1024 + fc * 128, 128))
                wgs.append(w)
            for tt in range(4):
                cs = slice(tt * 512, (tt + 1) * 512)
                nt0, nt1 = self.ntmp[0], self.ntmp[1 + tt % 2]
                for br, (k0, nk) in enumerate(((0, 3), (3, 3), (6, 2))):
                    pz = self.psb[br]
                    for kc in range(nk):
                        fw.mm(pz[:, :], wb[:, k0 + kc, :], self.OT[k0 + kc][:, cs], start=(kc == 0), stop=(kc == nk - 1))
                    pg = self.psb[3 + br]
                    for kc in range(8):
                        fw.mm(pg[:, :], wgs[br][:, kc, :], self.hT[:, kc, cs], start=(kc == 0), stop=(kc == 7))
                    sg = self.sqb[br]
                    fw.act(sg[:], pg[:, :], AF.Sigmoid)
                    if br == 0:
                        fw.tt("dve", nt0[:], pz[:, :], sg[:], ALU.mult)
                    else:
                        fw.tt("dve", nt1[:], pz[:, :], sg[:], ALU.mult)
                        if br == 1:
                            fw.tt("pool", nt0[:], nt0[:], nt1[:], ALU.add)
                        else:
                            fw.tt("pool", yT[fc][:, cs], nt0[:], nt1[:], ALU.add)
        for fc in range(8):
            w = wg[ng % 4]
            ng += 1
            fw.dma("pool", w, self.wview(self.inp["w_out"][l], fc * 128, 128))
            for tt in range(4):
                cs = slice(tt * 512, (tt + 1) * 512)
                ps = self.psb[6 + tt % 2]
                for kc in range(8):
                    fw.mm(ps[:, :], w[:, kc, :], yT[kc][:, cs], start=(kc == 0), stop=(kc == 7))
                fw.stt(self.xT[:, fc, cs], ps[:, :], self.modT[:, l, 16 + fc:17 + fc], self.xT[:, fc, cs], ALU.mult, ALU.add)

    def ffn_expert(self, l, wg_ap, wu_ap, wd_ap, combbc):
        fw = self.fw
        actT = self.carve(A_ACT, [128, 11, T], BF16)
        wgu = [self.carve(A_WGU + i * 2048, [128, 8, 128], BF16) for i in range(4)]
        wdt = [self.carve(A_WD + i * 2816, [128, 11, 128], BF16) for i in range(2)]
        sgt = [self.carve(A_SG + i * 1024, [128, 512], BF16) for i in range(3)]
        if not hasattr(self, "_ffc"):
            self._ffc = 0
            self._wdc = 0
        for half in range(2):
            for fi in range(11):
                ffc = half * 11 + fi
                wgt = wgu[(2 * self._ffc) % 4]
                wut = wgu[(2 * self._ffc + 1) % 4]
                self._ffc += 1
                fw.dma("pool", wgt, self.wview(wg_ap, ffc * 128, 128))
                fw.dma("pool", wut, self.wview(wu_ap, ffc * 128, 128))
                for tt in range(4):
                    cs = slice(tt * 512, (tt + 1) * 512)
                    pg = self.psb[(2 * tt) % 4]
                    pu = self.psb[(2 * tt + 1) % 4]
                    for kc in range(8):
                        fw.mm(pg[:, :], wgt[:, kc, :], self.hT[:, kc, cs], start=(kc == 0), stop=(kc == 7))
                    for kc in range(8):
                        fw.mm(pu[:, :], wut[:, kc, :], self.hT[:, kc, cs], start=(kc == 0), stop=(kc == 7))
                    sg = sgt[tt % 3]
                    fw.act(sg, pg[:, :], AF.Silu)
                    if combbc is not None:
                        fw.tt("pool", sg, sg, combbc[:, cs], ALU.mult)
                    fw.tt("dve", actT[:, fi, cs], pu[:, :], sg, ALU.mult)
            for fc in range(8):
                wd = wdt[self._wdc % 2]
                self._wdc += 1
                fw.dma("pool", wd, wd_ap.rearrange("(f p) n -> p f n", p=128)[:, half * 11:(half + 1) * 11, fc * 128:(fc + 1) * 128])
                for tt in range(4):
                    cs = slice(tt * 512, (tt + 1) * 512)
                    ps = self.psb[4 + tt % 2]
                    for fi in range(11):
                        fw.mm(ps[:, :], wd[:, fi, :], actT[:, fi, cs], start=(fi == 0), stop=(fi == 10))
                    fw.stt(self.xT[:, fc, cs], ps[:, :], self.modT[:, l, 40 + fc:41 + fc], self.xT[:, fc, cs], ALU.mult, ALU.add)

    def stage_moe(self, l):
        fw, C = self.fw, self.C
        R0 = A_Z2 + 8192
        wr = self.carve(R0, [128, 8, 8], F32)
        wr2 = self.carve(R0 + 256, [128, 8, 8], F32)
        brt = self.carve(R0 + 512, [128, 8], F32)
        crow = self.carve(R0 + 544, [128, 8], F32)
        cb = self.carve(R0 + 576, [128, 8], F32)
        rt = self.carve(R0 + 608, [128, 16], F32)
        lg = self.carve(R0 + 1024, [128, 16, 8], F32)
        m8 = self.carve(R0 + 1536, [128, 16, 8], F32)
        eq = self.carve(R0 + 2048, [128, 16, 8], F32)
        comb = self.carve(R0 + 2560, [128, 16, 8], F32)
        w1 = self.carve(R0 + 3072, [128, 16], F32)
        w2 = self.carve(R0 + 3136, [128, 16], F32)
        e21 = self.carve(R0 + 3200, [128, 16], F32)
        fw.dma("sp", wr, self.inp["w_router"][0].rearrange("(kc p) e -> p kc e", p=128))
        fw.dma("sp", brt[0:1, :], self.inp["b_router"][0:1, :])
        for kc in range(8):
            fw.ts("dve", wr2[:, kc, :], wr[:, kc, :], self.a2[:, l, kc:kc + 1], None, op0=ALU.mult)
        pc = self.psb[2]
        for kc in range(8):
            fw.mm(pc[0:1, 0:8], self.modT[:, l, 24 + kc:25 + kc], wr[:, kc, :], start=(kc == 0), stop=(kc == 7))
        fw.tt("dve", crow[0:1, :], pc[0:1, 0:8], brt[0:1, :], ALU.add)
        pcb = self.psb[3]
        fw.mm(pcb[:, 0:8], C["ones_f"][0:1, :], crow[0:1, :], start=True, stop=True)
        fw.copy("dve", cb, pcb[:, 0:8])
        pl = self.psb[6]
        for t16 in range(16):
            for kc in range(8):
                fw.mm(pl[:, t16 * 8:(t16 + 1) * 8], self.xT[:, kc, t16 * 128:(t16 + 1) * 128], wr2[:, kc, :],
                      start=(kc == 0), stop=(kc == 7))
        fw.copy("dve", rt, self.psb[7][:, 0:16])
        fw.tt("dve", lg, pl[:, 0:128].rearrange("p (a b) -> p a b", b=8), rt.unsqueeze(2).to_broadcast([128, 16, 8]), ALU.mult)
        fw.tt("dve", lg, lg, cb.unsqueeze(1).to_broadcast([128, 16, 8]), ALU.add)
        for t16 in range(16):
            fw.gen("dve", "max", [lg[:, t16, :]], [m8[:, t16, :]], out=m8[:, t16, :], in_=lg[:, t16, :])
        fw.tt("dve", e21, m8[:, :, 1], m8[:, :, 0], ALU.subtract)
        fw.act(e21, e21, AF.Exp)
        fw.ts("dve", w1, e21, 1.0, None, op0=ALU.add)
        fw.gen("dve", "reciprocal", [w1], [w1], out=w1, in_=w1)
        fw.tt("dve", w2, e21, w1, ALU.mult)
        fw.tt("dve", eq, lg, m8[:, :, 0:1].to_broadcast([128, 16, 8]), ALU.is_equal)
        fw.tt("dve", comb, eq, w1.unsqueeze(2).to_broadcast([128, 16, 8]), ALU.mult)
        fw.tt("dve", eq, lg, m8[:, :, 1:2].to_broadcast([128, 16, 8]), ALU.is_equal)
        fw.tt("dve", eq, eq, w2.unsqueeze(2).to_broadcast([128, 16, 8]), ALU.mult)
        fw.tt("dve", comb, comb, eq, ALU.add)
        if "comb" in self.debug:
            o = self.dbg_out("comb", [128, 16, 8], F32)
            fw.dma("sp", o[:, :, :], comb)
        cbc = [self.carve(A_Z2 + i * 4096, [128, T], BF16) for i in range(2)]
        for e in range(NEXP):
            cc = cbc[e % 2]
            for tt in range(4):
                nt = self.ntmp[tt % 3]
                for i in range(4):
                    fw.ts("dve", nt[:, i * 128:(i + 1) * 128], C["ident_f"][:], comb[:, tt * 4 + i, e:e + 1], None, op0=ALU.mult)
                pb = self.psb[6 + tt % 2]
                fw.mm(pb[:, :], C["ones_f"][:], nt[:], start=True, stop=True)
                fw.copy("act", cc[:, tt * 512:(tt + 1) * 512], pb[:, :])
            self.ffn_expert(l, self.inp["w_exp_gate"][0, e], self.inp["w_exp_up"][0, e], self.inp["w_exp_down"][0, e], cc)

    def stage_output(self):
        fw, C = self.fw, self.C
        xo = [self.carve(i * 4096, [128, D], F32) for i in range(2)]
        for t16 in range(16):
            xb = xo[t16 % 2]
            for half in range(2):
                pb = self.psb[(t16 * 2 + half) % 4]
                for i in range(4):
                    dc = half * 4 + i
                    fw.tr(pb[:, i * 128:(i + 1) * 128], self.xT[:, dc, t16 * 128:(t16 + 1) * 128], C["ident_f"][:])
                fw.copy(self.evac_eng(), xb[:, half * 512:(half + 1) * 512], pb[:, :])
            fw.dma("sp", self.out[t16 * 128:(t16 + 1) * 128, :], xb)

    def stage_moe_sparse(self, l):
        fw, C, nc = self.fw, self.C, self.nc
        I32 = mybir.dt.int32
        R0 = A_Z2
        wr = self.carve(R0, [128, 8, 8], F32)
        wr2 = self.carve(R0 + 256, [128, 8, 8], F32)
        brt = self.carve(R0 + 512, [128, 8], F32)
        crow = self.carve(R0 + 544, [128, 8], F32)
        cb = self.carve(R0 + 576, [128, 8], F32)
        rt = self.carve(R0 + 608, [128, 16], F32)
        e21 = self.carve(R0 + 672, [128, 16], F32)
        lg = self.carve(R0 + 1024, [128, 16, 8], F32)
        m8 = self.carve(R0 + 1536, [128, 16, 8], F32)
        eq1 = self.carve(R0 + 2048, [128, 16, 8], F32)
        eq2 = self.carve(R0 + 2560, [128, 16, 8], F32)
        tot = self.carve(R0 + 3072, [128, 16, 8], F32)
        pre = self.carve(R0 + 3584, [128, 16, 8], F32)
        pos = self.carve(R0 + 4096, [128, 16, 8], F32)
        tmp = self.carve(R0 + 4608, [128, 16, 8], F32)
        maskb = self.carve(R0 + 5120, [128, 128], BF16)
        gf = self.carve(R0 + 5376, [128, 16], F32)
        cntf = self.carve(R0 + 5440, [128, 8], F32)
        w1 = self.sb("moe_w1", [128, 16], F32)[:, :]
        w2 = self.sb("moe_w2", [128, 16], F32)[:, :]
        gi = [self.sb("moe_gi%d" % k, [128, 16], I32)[:, :] for k in range(2)]
        cnti = self.sb("moe_cnt", [1, 8], I32)[:, :]
        fw.dma("sp", wr, self.inp["w_router"][0].rearrange("(kc p) e -> p kc e", p=128))
        fw.dma("sp", brt[0:1, :], self.inp["b_router"][0:1, :])
        for kc in range(8):
            fw.ts("dve", wr2[:, kc, :], wr[:, kc, :], self.a2[:, l, kc:kc + 1], None, op0=ALU.mult)
        pc = self.psb[2]
        for kc in range(8):
            fw.mm(pc[0:1, 0:8], self.modT[:, l, 24 + kc:25 + kc], wr[:, kc, :], start=(kc == 0), stop=(kc == 7))
        fw.tt("dve", crow[0:1, :], pc[0:1, 0:8], brt[0:1, :], ALU.add)
        pcb = self.psb[3]
        fw.mm(pcb[:, 0:8], C["ones_f"][0:1, :], crow[0:1, :], start=True, stop=True)
        fw.copy("dve", cb, pcb[:, 0:8])
        pl = self.psb[6]
        for t16 in range(16):
            for kc in range(8):
                fw.mm(pl[:, t16 * 8:(t16 + 1) * 8], self.xT[:, kc, t16 * 128:(t16 + 1) * 128], wr2[:, kc, :],
                      start=(kc == 0), stop=(kc == 7))
        fw.copy("dve", rt, self.psb[7][:, 0:16])
        bc3 = [128, 16, 8]
        fw.tt("dve", lg, pl[:, 0:128].rearrange("p (a b) -> p a b", b=8), rt.unsqueeze(2).to_broadcast(bc3), ALU.mult)
        fw.tt("dve", lg, lg, cb.unsqueeze(1).to_broadcast(bc3), ALU.add)
        for t16 in range(16):
            fw.gen("dve", "max", [lg[:, t16, :]], [m8[:, t16, :]], out=m8[:, t16, :], in_=lg[:, t16, :])
        fw.tt("dve", e21, m8[:, :, 1], m8[:, :, 0], ALU.subtract)
        fw.act(e21, e21, AF.Exp)
        fw.ts("dve", w1, e21, 1.0, None, op0=ALU.add)
        fw.gen("dve", "reciprocal", [w1], [w1], out=w1, in_=w1)
        fw.tt("dve", w2, e21, w1, ALU.mult)
        fw.tt("dve", eq1, lg, m8[:, :, 0:1].to_broadcast(bc3), ALU.is_equal)
        fw.tt("dve", eq2, lg, m8[:, :, 1:2].to_broadcast(bc3), ALU.is_equal)
        fl = lambda a: a.rearrange("p a b -> p (a b)")
        fw.tt("dve", maskb, fl(eq1), fl(eq2), ALU.add)
        ptot, pwi = self.psb[4], self.psb[5]
        fw.mm(ptot[:, 0:128], C["ones_b"][:], maskb, start=True, stop=True)
        for t16 in range(16):
            fw.mm(pwi[:, t16 * 8:(t16 + 1) * 8], C["ustrict"][:], maskb[:, t16 * 8:(t16 + 1) * 8], start=True, stop=True)
        fw.copy("dve", fl(tot), ptot[:, 0:128])
        fw.memset("dve", pre[:, 0, :], 0.0)
        for t16 in range(1, 16):
            fw.tt("dve", pre[:, t16, :], pre[:, t16 - 1, :], tot[:, t16 - 1, :], ALU.add)
        fw.tt("dve", cntf, pre[:, 15, :], tot[:, 15, :], ALU.add)
        fw.copy("dve", cnti[0:1, :], cntf[0:1, :])
        fw.tt("dve", fl(pos), pwi[:, 0:128], fl(pre), ALU.add)
        fw.tt("dve", fl(pos), fl(pos), C["ebase"][:], ALU.add)
        for k, eq in enumerate((eq1, eq2)):
            fw.tt("dve", tmp, pos, eq, ALU.mult)
            fw.gen("dve", "tensor_reduce", [tmp], [gf], out=gf, in_=tmp, axis=AX.X, op=ALU.add)
            fw.copy("dve", gi[k], gf)
        if "moeidx" in self.debug:
            o = self.dbg_out("gi0", [128, 16], I32)
            fw.dma("sp", o[:, :], gi[0][:])
            o = self.dbg_out("gi1", [128, 16], I32)
            fw.dma("sp", o[:, :], gi[1][:])
            o = self.dbg_out("cnt", [1, 8], I32)
            fw.dma("sp", o[:, :], cnti[:])
        rows = [[self.carve(k * 2056 + i * 4112, [128, 1028], BF16) for k in range(2)] for i in range(2)]
        xrow = [self.carve(16448 + i * 4096, [128, D], F32) for i in range(2)]
        wsrc = (w1, w2)
        for t16 in range(16):
            ts_ = slice(t16 * 128, (t16 + 1) * 128)
            ph = self.psb[t16 % 2].bitcast(BF16)
            for kc in range(8):
                fw.tr(ph[:, kc * 128:(kc + 1) * 128], self.hT[:, kc, ts_], C["ident_b"][:])
            for k in range(2):
                Rk = rows[t16 % 2][k]
                fw.copy("act" if k == 0 else "dve", Rk[:, 0:1024], ph[:, :])
                fw.copy("dve", Rk[:, 1024:1026].bitcast(F32), wsrc[k][:, t16:t16 + 1])
                g = nc.gpsimd
                idx = gi[k][:, t16:t16 + 1]
                src = Rk[:, 0:1026]
                fw.op("pool", (lambda src=src, idx=idx: g.indirect_dma_start(
                    out=self.HS[:, :], out_offset=bass.IndirectOffsetOnAxis(ap=idx, axis=0), in_=src, in_offset=None)),
                    reads=[src, idx], writes=[self.HS[:, :]], dma=True)
            xr = xrow[t16 % 2]
            for half in range(2):
                pb = self.psb[2 + half]
                for i in range(4):
                    dc = half * 4 + i
                    fw.tr(pb[:, i * 128:(i + 1) * 128], self.xT[:, dc, ts_], C["ident_f"][:])
                fw.copy("act" if half == 0 else "dve", xr[:, half * 512:(half + 1) * 512], pb[:, :])
            fw.dma("sp", self.XTOK[ts_, :], xr)
        E_WD, E_WGU, E_HTE, E_ACT, E_ACTT, E_G, E_YSL, E_SG, E_WS = 0, 22528, 56320, 89088, 134144, 139776, 143888, 147984, 149392
        wd = self.carve(E_WD, [128, 11, D], BF16)
        wgu = [self.carve(E_WGU + i * 5632, [128, 8, 352], BF16) for i in range(6)]
        hTe = self.carve(E_HTE, [128, 16, 1024], BF16)
        act_sl = self.carve(E_ACT, [128, 16, 1408], BF16)
        actT = [self.carve(E_ACTT + i * 2816, [128, 1408], BF16) for i in range(2)]
        G = [self.carve(E_G + i * 2056, [128, 1028], BF16) for i in range(2)]
        ysl = [self.carve(E_YSL + i * 2048, [128, D], BF16) for i in range(2)]
        sgt = [self.carve(E_SG + i * 704, [128, 352], BF16) for i in range(2)]
        ws = self.carve(E_WS, [128, 16], F32)
        y0 = self.hT.rearrange("p a b -> p (a b)").rearrange("p (c f) -> p c f", c=16)
        self._nw = 0
        for e in range(NEXP):
            key = "moe_e%d" % e

            for ce in fw.CENGS:
                def ld(key=key, e=e, ce=ce):
                    eo = fw.engobj[ce]
                    reg = eo.alloc_register("r_%s_%s" % (key, ce))
                    ins = eo.reg_load(reg, cnti[0:1, e:e + 1])
                    fw.cond_vals[(key, ce)] = eo.snap(reg)
                    return ins
                fw.op(ce, ld, reads=[cnti[0:1, e:e + 1]], writes=[])
            Wg, Wu, Wd = self.inp["w_exp_gate"][0, e], self.inp["w_exp_up"][0, e], self.inp["w_exp_down"][0, e]
            for c in range(16):
                Gc = G[c % 2]
                fw.dma("sp", Gc[:, 0:1026], self.HS[e * T + c * 128:e * T + (c + 1) * 128, :])
                fw.cur_cond = (key, c * 128)
                fw.copy("dve", ws[:, c:c + 1], Gc[:, 1024:1026].bitcast(F32))
                ph = self.psb[c % 2].bitcast(BF16)
                for kc in range(8):
                    fw.tr(ph[:, kc * 128:(kc + 1) * 128], Gc[:, kc * 128:(kc + 1) * 128], C["ident_b"][:])
                fw.copy("dve", hTe[:, c, :], ph[:, :])
                fw.cur_cond = None
            fw.barrier()
            for half in range(2):
                tiles = {}

                def issue_cg(cg, half=half, tiles=tiles, Wg=Wg, Wu=Wu):
                    col0 = half * 1408 + cg * 352
                    wgt, wut = wgu[(2 * self._nw) % 6], wgu[(2 * self._nw + 1) % 6]
                    self._nw += 1
                    fw.dma("pool", wgt, self.wview(Wg, col0, 352))
                    fw.dma("pool", wut, self.wview(Wu, col0, 352))
                    tiles[cg] = (wgt, wut)
                issue_cg(0)
                issue_cg(1)
                issue_cg(2)
                fw.dma("pool", wd, Wd.rearrange("(f p) n -> p f n", p=128)[:, half * 11:(half + 1) * 11, :])
                for cg in range(4):
                    wgt, wut = tiles[cg]
                    for c in range(16):
                        pg, pu = self.psb[2 + 2 * (c % 2)], self.psb[3 + 2 * (c % 2)]
                        fw.cur_cond = (key, c * 128)
                        for kc in range(8):
                            fw.mm(pg[:, 0:352], hTe[:, c, kc * 128:(kc + 1) * 128], wgt[:, kc, :], start=(kc == 0), stop=(kc == 7))
                        for kc in range(8):
                            fw.mm(pu[:, 0:352], hTe[:, c, kc * 128:(kc + 1) * 128], wut[:, kc, :], start=(kc == 0), stop=(kc == 7))
                        sg = sgt[c % 2]
                        fw.act(sg, pg[:, 0:352], AF.Silu)
                        fw.stt(act_sl[:, c, cg * 352:(cg + 1) * 352], pu[:, 0:352], ws[:, c:c + 1], sg, ALU.mult, ALU.mult)
                        fw.cur_cond = None
                    fw.barrier()
                    if cg + 3 < 4:
                        issue_cg(cg + 3)
                for c in range(16):
                    at = actT[c % 2]
                    pa, pb2 = self.psb[0].bitcast(BF16), self.psb[1].bitcast(BF16)
                    fw.cur_cond = (key, c * 128)
                    for f in range(11):
                        dstp = pa[:, f * 128:(f + 1) * 128] if f < 8 else pb2[:, (f - 8) * 128:(f - 7) * 128]
                        fw.tr(dstp, act_sl[:, c, f * 128:(f + 1) * 128], C["ident_b"][:])
                    fw.copy("dve", at[:, 0:1024], pa[:, :])
                    fw.copy("dve", at[:, 1024:1408], pb2[:, 0:384])
                    py = (self.psb[6], self.psb[7])
                    for fo in range(2):
                        for f in range(11):
                            fw.mm(py[fo][:, :], at[:, f * 128:(f + 1) * 128], wd[:, f, fo * 512:(fo + 1) * 512],
                                  start=(f == 0), stop=(f == 10))
                    if half == 0:
                        fw.copy("dve", y0[:, c, 0:512], py[0][:, :])
                        fw.copy("dve", y0[:, c, 512:1024], py[1][:, :])
                    else:
                        yb = ysl[c % 2]
                        fw.tt("dve", yb[:, 0:512], py[0][:, :], y0[:, c, 0:512], ALU.add)
                        fw.tt("dve", yb[:, 512:1024], py[1][:, :], y0[:, c, 512:1024], ALU.add)
                    fw.cur_cond = None
                    if half == 1:
                        fw.dma("sp", self.YS[e * T + c * 128:e * T + (c + 1) * 128, :], ysl[c % 2])
                fw.barrier()
        g2bc = self.carve(0, [128, D], F32)
        dg = self.carve(4096, [128, D], F32)
        for dc in range(8):
            fw.ts("dve", dg[:, dc * 128:(dc + 1) * 128], C["ident_f"][:], self.modT[:, l, 40 + dc:41 + dc], None, op0=ALU.mult)
        for half in range(2):
            pb = self.psb[2 + half]
            fw.mm(pb[:, :], C["ones_f"][:], dg[:, half * 512:(half + 1) * 512], start=True, stop=True)
            fw.copy("act", g2bc[:, half * 512:(half + 1) * 512], pb[:, :])
        ya = [[self.carve(8192 + (2 * i + k) * 2048, [128, D], BF16) for k in range(2)] for i in range(2)]
        ysum = [self.carve(16384 + i * 4096, [128, D], F32) for i in range(2)]
        xo = [self.carve(24576 + i * 4096, [128, D], F32) for i in range(2)]
        for t16 in range(16):
            ts_ = slice(t16 * 128, (t16 + 1) * 128)
            g = nc.gpsimd
            for k in range(2):
                dst = ya[t16 % 2][k]
                idx = gi[k][:, t16:t16 + 1]
                fw.op("pool", (lambda dst=dst, idx=idx: g.indirect_dma_start(
                    out=dst, out_offset=None, in_=self.YS[:, :], in_offset=bass.IndirectOffsetOnAxis(ap=idx, axis=0))),
                    reads=[self.YS[:, :], idx], writes=[dst], dma=True)
            xb = xo[t16 % 2]
            fw.dma("sp", xb, self.XTOK[ts_, :])
            y1, y2 = ya[t16 % 2]
            ysm = ysum[t16 % 2]
            fw.tt("dve", ysm, y1, y2, ALU.add)
            fw.tt("pool", ysm, ysm, g2bc, ALU.mult)
            fw.tt("dve", xb, xb, ysm, ALU.add)
            fw.dma("sp", self.out[ts_, :], xb)


_CACHE = {}


def kernel(**inputs):
    inputs = {k: np.asarray(v) for k, v in inputs.items()}
    k = K()
    in_maps = []
    for b in range(8):
        m = host_layout(inputs, b)
        for kk, v in k.hc.items():
            m["c_" + kk] = v
        in_maps.append(m)
    res = run_bass_kernel_spmd(k.nc, in_maps, core_ids=list(range(8)))
    out = np.stack([np.asarray(res.results[b]["out"]) for b in range(8)], axis=0)
    return out.astype(np.float32)
```

```python
from concourse.bass_utils import run_bass_kernel_spmd
import numpy as np
import concourse.bass as bass
import concourse.mybir as mybir

F32 = mybir.dt.float32
BF16 = mybir.dt.bfloat16
AF = mybir.ActivationFunctionType
ALU = mybir.AluOpType
AX = mybir.AxisListType

_DTSIZE = {}


def dtsize(dt):
    if dt not in _DTSIZE:
        _DTSIZE[dt] = np.dtype(mybir.dt.np(dt)).itemsize
    return _DTSIZE[dt]


class Rec:
    __slots__ = ("eng", "fn", "deps", "dma", "sig", "idx", "gidx", "dmasem", "dmaval", "vc", "cond", "sv")


class FW:
    ENGS = ("pe", "act", "dve", "pool", "sp")
    CENGS = ("pe", "act", "dve")

    def __init__(self, nc, n_dma_sems=24):
        self.nc = nc
        self.recs = []
        self.eng_recs = {e: [] for e in self.ENGS}
        self.hist = {}
        self.engobj = {"pe": nc.tensor, "act": nc.scalar, "dve": nc.vector, "pool": nc.gpsimd, "sp": nc.sync}
        self.n_dma_sems = n_dma_sems
        self.dma_count = {e: 0 for e in self.ENGS}
        self.dma_last = {}
        self.cur_cond = None
        self.cond_vals = {}

    def region(self, ap):
        t = ap.tensor
        name = t.name
        space = str(ap.space) if hasattr(ap, "space") else ""
        esz = dtsize(ap.dtype)
        apl = list(ap.ap)
        off = ap.offset
        is_dram = "DRAM" in space.upper() or "HBM" in space.upper() or type(t).__name__.startswith("DRam")
        if is_dram:
            lo = off
            hi = off
            for st, cnt in apl:
                if cnt > 1:
                    if st >= 0:
                        hi += st * (cnt - 1)
                    else:
                        lo += st * (cnt - 1)
            return (name, 0, 1, lo * esz, (hi + 1) * esz, False)
        tsz = dtsize(t.dtype)
        pstride = 1
        for s in t.shape[1:]:
            pstride *= s
        if esz != tsz:
            pstride = pstride * tsz // esz
        p0 = off // pstride
        f0 = off % pstride
        pst, pcnt = apl[0]
        if pst == 0:
            pcnt = 1
        p1 = p0 + pcnt
        lo = f0
        hi = f0
        for st, cnt in apl[1:]:
            if cnt > 1:
                if st >= 0:
                    hi += st * (cnt - 1)
                else:
                    lo += st * (cnt - 1)
        b0, b1 = lo * esz, (hi + 1) * esz
        is_psum = type(t).__name__.startswith("PSum")
        if is_psum:
            b0 = (b0 // 2048) * 2048
            b1 = ((b1 + 2047) // 2048) * 2048
            p0, p1 = 0, 128
        return (name, p0, p1, b0, b1, is_psum)

    def op(self, eng, fn, reads=(), writes=(), dma=False):
        r = Rec()
        r.cond = self.cur_cond if eng in self.CENGS else None
        r.sv = None
        r.eng = eng
        r.fn = fn
        r.dma = dma
        r.sig = False
        r.deps = []
        r.idx = len(self.eng_recs[eng])
        r.gidx = len(self.recs)
        r.dmasem = None
        deps = set()
        for ap in reads:
            self._access(r, self.region(ap), False, deps)
        for ap in writes:
            self._access(r, self.region(ap), True, deps)
        if dma:
            slot = self.dma_count[eng] % self.n_dma_sems
            self.dma_count[eng] += 1
            prev = self.dma_last.get((eng, slot))
            if prev is not None:
                deps.add(prev)
            self.dma_last[(eng, slot)] = r
            r.dmasem = slot
        r.deps = sorted(deps, key=lambda d: d.gidx)
        self.recs.append(r)
        self.eng_recs[eng].append(r)
        return r

    def _access(self, r, reg, is_write, deps):
        name, p0, p1, b0, b1, is_psum = reg
        lst = self.hist.setdefault(name, [])
        excl = is_write or is_psum
        keep = []
        for ent in lst:
            ep0, ep1, eb0, eb1, w, rds, wtrue = ent
            if ep1 <= p0 or p1 <= ep0 or eb1 <= b0 or b1 <= eb0:
                keep.append(ent)
                continue
            inside = ep0 >= p0 and ep1 <= p1 and eb0 >= b0 and eb1 <= b1
            if w is not None and w is not r:
                if w.dma or r.dma or w.eng != r.eng:
                    deps.add(w)
                elif wtrue and (not is_write) and r.eng != "pe":
                    deps.add(w)
            if excl:
                for rd in rds:
                    if rd is r:
                        continue
                    if rd.dma or r.dma or rd.eng != r.eng:
                        deps.add(rd)
                if inside:
                    continue
            else:
                if w is None and inside and len(rds) == 1 and (not rds[0].dma) and (not r.dma) and rds[0].eng == r.eng:
                    continue
            keep.append(ent)
        if excl:
            keep.append([p0, p1, b0, b1, r, [], is_write])
        else:
            keep.append([p0, p1, b0, b1, None, [r], False])
        self.hist[name] = keep

    def barrier(self, engs=("pe", "act", "dve")):
        lasts = {e: self.eng_recs[e][-1] for e in engs if self.eng_recs[e]}
        for e in engs:
            eo = self.engobj[e]
            r = self.op(e, (lambda eo=eo: eo.drain()), reads=[], writes=[])
            extra = [lasts[o] for o in engs if o != e and o in lasts]
            r.deps = sorted(set(r.deps) | set(extra), key=lambda d: d.gidx)

    def emit(self):
        nc = self.nc
        ne = len(self.ENGS)
        eidx = {e: i for i, e in enumerate(self.ENGS)}
        grp_of = {}
        groups = []
        for ce in self.CENGS:
            prev = None
            for r in self.eng_recs[ce]:
                if r.cond is None:
                    prev = None
                    continue
                if prev is not None and prev.cond is not None and prev.cond[0] == r.cond[0] and prev.cond[1] <= r.cond[1] \
                        and prev.idx == r.idx - 1:
                    groups[-1].append(r)
                else:
                    groups.append([r])
                grp_of[r.gidx] = len(groups) - 1
                prev = r
        clock = {e: [-1] * ne for e in self.ENGS}
        dma_known = {e: set() for e in self.ENGS}
        final_deps = []
        cur_grp = {e: None for e in self.CENGS}
        saved = {e: None for e in self.CENGS}
        for r in self.recs:
            if r.eng in self.CENGS:
                g = grp_of.get(r.gidx)
                if g != cur_grp[r.eng]:
                    if cur_grp[r.eng] is not None:
                        clock[r.eng] = saved[r.eng][0]
                        dma_known[r.eng] = saved[r.eng][1]
                    if g is not None:
                        saved[r.eng] = (list(clock[r.eng]), set(dma_known[r.eng]))
                    cur_grp[r.eng] = g
            ck = clock[r.eng]
            need = []
            for d in r.deps:
                if d.dma:
                    if d.gidx in dma_known[r.eng]:
                        continue
                    need.append(d)
                else:
                    if ck[eidx[d.eng]] >= d.idx:
                        continue
                    need.append(d)
            best = {}
            nd = []
            for d in need:
                if d.dma:
                    nd.append(d)
                else:
                    if d.eng not in best or best[d.eng].idx < d.idx:
                        best[d.eng] = d
            nd.extend(best.values())
            for d in nd:
                d.sig = True
                if d.dma:
                    dma_known[r.eng].add(d.gidx)
                    dvc = d.vc
                else:
                    dvc = list(d.vc)
                    dvc[eidx[d.eng]] = max(dvc[eidx[d.eng]], d.idx)
                for i in range(ne):
                    if dvc[i] > ck[i]:
                        ck[i] = dvc[i]
            if r.eng in self.CENGS and cur_grp[r.eng] is not None:
                r.vc = list(saved[r.eng][0])
            else:
                r.vc = list(ck)
            final_deps.append(nd)
        cnt = {e: 0 for e in self.ENGS}
        dcnt = {}
        for r in self.recs:
            if r.dma:
                key = (r.eng, r.dmasem)
                dcnt[key] = dcnt.get(key, 0) + 16
                r.dmaval = dcnt[key]
            elif r.sig:
                cnt[r.eng] += 1
                r.sv = cnt[r.eng]
        sems = {}
        for e in self.ENGS:
            sems[e] = nc.alloc_semaphore("sem_" + e)
        dsems = {}
        for e in self.ENGS:
            if self.dma_count[e] > 0:
                dsems[e] = [nc.alloc_semaphore("dsem_%s_%d" % (e, i)) for i in range(self.n_dma_sems)]
        self.nwait = 0

        def emit_one(r, nd):
            eo = self.engobj[r.eng]
            for d in nd:
                if d.dma:
                    eo.wait_ge(dsems[d.eng][d.dmasem], d.dmaval)
                else:
                    eo.wait_ge(sems[d.eng], d.sv)
                self.nwait += 1
            ins = r.fn()
            if r.dma:
                ins.then_inc(dsems[r.eng][r.dmasem], 16)
            elif r.sig:
                ins.then_inc(sems[r.eng], 1)

        def emit_group(recs_):
            eng = recs_[0].eng
            eo = self.engobj[eng]
            key = recs_[0].cond[0]
            val = self.cond_vals[(key, eng)]
            levels = []
            for r in recs_:
                if not levels or levels[-1][0] != r.cond[1]:
                    levels.append((r.cond[1], []))
                levels[-1][1].append(r)

            def rec_level(li):
                if li == len(levels):
                    return
                c, rs = levels[li]
                nsig = sum(1 for lv in levels[li:] for r in lv[1] if r.sig)
                with eo.If(val > c):
                    for r in rs:
                        emit_one(r, final_deps[r.gidx])
                    rec_level(li + 1)
                with eo.Else():
                    if nsig > 0:
                        eo.drain()
                        eo.sem_inc(sems[eng], nsig)
            rec_level(0)

        done = set()
        for r, nd in zip(self.recs, final_deps):
            if r.gidx in done:
                continue
            g = grp_of.get(r.gidx)
            if g is not None:
                emit_group(groups[g])
                for x in groups[g]:
                    done.add(x.gidx)
            else:
                emit_one(r, nd)
        for (e, slot), r in self.dma_last.items():
            self.engobj[e].wait_ge(dsems[e][slot], r.dmaval)
        self.stats = dict(n=len(self.recs), waits=self.nwait, per_eng={e: len(v) for e, v in self.eng_recs.items()},
                          ngroups=len(groups))
        return self.stats

    def dma(self, eng, out, in_, **kw):
        o = self.engobj[eng]
        return self.op(eng, lambda: o.dma_start(out=out, in_=in_, **kw), reads=[in_], writes=[out], dma=True)

    def mm(self, out, lhsT, rhs, start=True, stop=True, **kw):
        t = self.nc.tensor
        return self.op("pe", lambda: t.matmul(out, lhsT, rhs, start=start, stop=stop, **kw), reads=[lhsT, rhs], writes=[out])

    def tr(self, out, in_, ident):
        t = self.nc.tensor
        return self.op("pe", lambda: t.transpose(out, in_, ident), reads=[in_, ident], writes=[out])

    def act(self, out, in_, func, bias=None, scale=None, accum_out=None):
        s = self.nc.scalar
        kw = {}
        rd = [in_]
        if bias is not None:
            kw["bias"] = bias
            if not isinstance(bias, (int, float)):
                rd.append(bias)
        if scale is not None:
            kw["scale"] = scale
            if not isinstance(scale, (int, float)):
                rd.append(scale)
        wr = [out]
        if accum_out is not None:
            kw["accum_out"] = accum_out
            wr.append(accum_out)
        return self.op("act", lambda: s.activation(out=out, in_=in_, func=func, **kw), reads=rd, writes=wr)

    def _veng(self, eng):
        return self.engobj[eng]

    def tt(self, eng, out, in0, in1, op):
        e = self._veng(eng)
        return self.op(eng, lambda: e.tensor_tensor(out=out, in0=in0, in1=in1, op=op), reads=[in0, in1], writes=[out])

    def ts(self, eng, out, in0, s1, s2=None, op0=ALU.mult, op1=None):
        e = self._veng(eng)
        rd = [in0]
        for s in (s1, s2):
            if s is not None and not isinstance(s, (int, float)):
                rd.append(s)
        kw = {}
        if op1 is not None:
            kw["op1"] = op1
        return self.op(eng, lambda: e.tensor_scalar(out=out, in0=in0, scalar1=s1, scalar2=s2, op0=op0, **kw), reads=rd, writes=[out])

    def stt(self, out, in0, scalar, in1, op0, op1, eng="dve"):
        e = self._veng(eng)
        rd = [in0, in1]
        if not isinstance(scalar, (int, float)):
            rd.append(scalar)
        return self.op(eng, lambda: e.scalar_tensor_tensor(out=out, in0=in0, scalar=scalar, in1=in1, op0=op0, op1=op1), reads=rd, writes=[out])

    def copy(self, eng, out, in_):
        if eng == "act":
            s = self.nc.scalar
            return self.op("act", lambda: s.copy(out=out, in_=in_), reads=[in_], writes=[out])
        e = self._veng(eng)
        return self.op(eng, lambda: e.tensor_copy(out=out, in_=in_), reads=[in_], writes=[out])

    def memset(self, eng, out, val):
        e = self._veng(eng)
        return self.op(eng, lambda: e.memset(out, val), reads=[], writes=[out])


def _fw_generic(self, eng, name, reads, writes, *args, **kw):
    e = self.engobj[eng]
    f = getattr(e, name)
    return self.op(eng, lambda: f(*args, **kw), reads=reads, writes=writes)


FW.gen = _fw_generic


import numpy as np
import ml_dtypes

D = 1024
T = 2048
NH = 24
DFF = 2816
NFF = 22
NEXP = 8
IN_COLS = 7686
QOFF, KOFF, VOFF, FOFF, GOFF = 0, 1536, 3072, 4608, 4614
EPS = 1e-6
SLOPES = (2.0 ** (-8.0 * np.arange(1, 19) / 18)).astype(np.float32)
DIL = ((128, 1), (512, 4), (2048, 16))
DIL_SO = (0, 4, 14)
MOBA_SO = 8
NEGM = -30000.0


def split3(v):
    v = np.asarray(v, np.float32)
    hi = v.astype(ml_dtypes.bfloat16)
    r1 = v - hi.astype(np.float32)
    mid = r1.astype(ml_dtypes.bfloat16)
    r2 = r1 - mid.astype(np.float32)
    lo = r2.astype(ml_dtypes.bfloat16)
    return hi, mid, lo


def host_consts():
    c = {}
    c["ident_f"] = np.eye(128, dtype=np.float32)
    c["ident_b"] = np.eye(128, dtype=np.float32).astype(ml_dtypes.bfloat16)
    ob = np.zeros((128, 128), np.float32)
    ob[0:64, 0:64] = 1.0
    c["onesblk"] = ob.astype(ml_dtypes.bfloat16)
    c["ones_b"] = np.ones((128, 128), np.float32).astype(ml_dtypes.bfloat16)
    c["ones_f"] = np.ones((128, 128), np.float32)
    k = np.arange(128)[:, None]
    q = np.arange(128)[None, :]
    c["tri"] = (q >= k).astype(np.float32).astype(ml_dtypes.bfloat16)
    db = np.zeros((128, 12, 256), np.float32)
    for g, (w, d) in enumerate(DIL):
        for j in range(4):
            sl = SLOPES[DIL_SO[g] + j]
            left = np.where(q >= k, -sl * d * (q - k), NEGM)
            right = np.where(k >= q, -sl * d * (128 + q - k), NEGM)
            db[:, g * 4 + j, 0:128] = left
            db[:, g * 4 + j, 128:256] = right
    c["dilbias"] = db.reshape(128, 12 * 256)
    t = np.arange(T, dtype=np.float32)
    aq = np.zeros((6, 6, T), ml_dtypes.bfloat16)
    ak = np.zeros((6, 6, T), ml_dtypes.bfloat16)
    for h in range(6):
        sl = SLOPES[MOBA_SO + h]
        qh = split3(-8.0 * sl * t)
        kh = split3(8.0 * sl * t)
        for i in range(3):
            aq[h, i] = qh[i]
            aq[h, 3 + i] = 1.0
            ak[h, i] = 1.0
            ak[h, 3 + i] = kh[i]
    c["moba_aq"] = aq
    c["moba_ak"] = ak
    bi = np.zeros((8, T), np.float32)
    for b in range(8):
        bi[b, b * 256:(b + 1) * 256] = 1.0
    c["blkind"] = bi.astype(ml_dtypes.bfloat16)
    pos = (np.arange(16)[None, :, None] * 128 + np.arange(128)[:, None, None])
    own = pos // 256
    B = np.arange(8)[None, None, :]
    c["mg_sb1"] = np.where(B < own, 0.0, -1e30).astype(np.float32).reshape(128, 128)
    c["mg_v1"] = (B < own).astype(np.float32).reshape(128, 128)
    c["mg_e"] = (B == own).astype(np.float32).reshape(128, 128)
    c["fox_ones"] = np.ones((3, T), np.float32).astype(ml_dtypes.bfloat16)
    c["ustrict"] = np.triu(np.ones((128, 128), np.float32), 1).astype(ml_dtypes.bfloat16)
    c["ebase"] = np.tile(np.arange(8, dtype=np.float32) * 2048.0, (128, 16))
    return c


CONST_DT = dict(ident_f=F32, ident_b=BF16, onesblk=BF16, ones_b=BF16, ones_f=F32, tri=BF16, dilbias=F32,
                moba_aq=BF16, moba_ak=BF16, blkind=BF16, mg_sb1=F32, mg_v1=F32, mg_e=F32, fox_ones=BF16, ustrict=BF16, ebase=F32)


def host_layout(inp, b):
    m = {}
    m["x"] = np.ascontiguousarray(inp["x"][b])
    m["cT"] = np.ascontiguousarray(inp["c"][b].reshape(8, 128).T)
    m["b_adaT"] = np.ascontiguousarray(inp["b_ada"].reshape(2, 48, 128).transpose(2, 0, 1))
    m["nmixT"] = np.ascontiguousarray(inp["norm_mix"].reshape(2, 8, 128).transpose(2, 0, 1))
    m["nffnT"] = np.ascontiguousarray(inp["norm_ffn"].reshape(2, 8, 128).transpose(2, 0, 1))
    m["qgT"] = np.ascontiguousarray(inp["q_gain"].transpose(2, 0, 1))
    m["kgT"] = np.ascontiguousarray(inp["k_gain"].transpose(2, 0, 1))
    m["bfg"] = np.ascontiguousarray(inp["b_fgate"].T)
    m["b_router"] = np.ascontiguousarray(inp["b_router"])
    for k in ("w_ada", "w_in", "w_br_fox", "w_br_moba", "w_br_dil", "w_out", "w_ffn_gate", "w_ffn_up",
              "w_ffn_down", "w_router", "w_exp_gate", "w_exp_up", "w_exp_down"):
        m[k] = inp[k]
    return m


IN_SHAPES = dict(
    x=[T, D], cT=[128, 8], b_adaT=[128, 2, 48], nmixT=[128, 2, 8], nffnT=[128, 2, 8], qgT=[64, 2, 24], kgT=[64, 2, 24],
    bfg=[6, 2], b_router=[1, 8], w_ada=[2, D, 6 * D], w_in=[2, D, IN_COLS], w_br_fox=[2, 384, D], w_br_moba=[2, 384, D],
    w_br_dil=[2, 256, D], w_out=[2, D, D], w_ffn_gate=[1, D, DFF], w_ffn_up=[1, D, DFF], w_ffn_down=[1, DFF, D],
    w_router=[1, D, 8], w_exp_gate=[1, 8, D, DFF], w_exp_up=[1, 8, D, DFF], w_exp_down=[1, 8, DFF, D])


def prod(xs):
    r = 1
    for x in xs:
        r *= x
    return r


A_OT = 0
A_Y = 32768
A_VA = 32768
A_VB = 32768 + 12544
A_QK = 57856
A_X = 65536
A_WQK = 74240
A_WV = 82432
A_ACCN = 88576
A_ODD = 96768
A_PT = 100864
A_Q2 = 103936
A_RQ = 105984
A_RD = 110080
A_BC = 112128
A_MG = 114176
A_KM = 118272
A_FST = 65536
A_Z = 131072
A_DILM = 131072
A_Z2 = 137216
A_END = 151552
A_ACT = 0
A_WGU = 45056
A_WD = 53248
A_SG = 58880
A_CBC = A_Z2
A_RT = A_Z2 + 4096


class K:
    def __init__(self, debug=None, nlayers=2, stop_after=None, heads=None, sparse=True):
        self.debug = debug or []
        self.nlayers = nlayers
        self.stop_after = stop_after
        self.heads = heads
        nc = bass.Bass("TRN2", target_bir_lowering=False)
        self.nc = nc
        self.fw = FW(nc)
        self.inp = {}
        for k, shp in IN_SHAPES.items():
            self.inp[k] = nc.dram_tensor(k, shp, F32, kind="ExternalInput").ap()
        self.cst = {}
        hc = host_consts()
        self.hc = hc
        for k, v in hc.items():
            self.cst[k] = nc.dram_tensor("c_" + k, list(v.shape), CONST_DT[k], kind="ExternalInput").ap()
        self.out = nc.dram_tensor("out", [T, D], F32, kind="ExternalOutput").ap()
        self.xs = nc.dram_tensor("xs_scr", [128, 8 * T], F32, kind="Internal").ap()
        self.fsc = nc.dram_tensor("f_scr", [2, 6, 3 * T], BF16, kind="Internal").ap()
        self.HS = nc.dram_tensor("hs_scr", [NEXP * T, 1026], BF16, kind="Internal").ap()
        self.YS = nc.dram_tensor("ys_scr", [NEXP * T, D], BF16, kind="Internal").ap()
        self.XTOK = nc.dram_tensor("xtok_scr", [T, D], F32, kind="Internal").ap()
        self.sparse = sparse
        self.dbg = {}
        self._rr = 0
        self._cnt = 0
        self.build()

    def sb(self, name, shape, dt):
        return self.nc.alloc_sbuf_tensor("s_" + name, shape, dt)

    def carve(self, off, shape, dt):
        n = prod(shape[1:])
        nb = n * dtsize(dt)
        assert off % 4 == 0 and off + nb <= A_END, (off, nb)
        v = self.AR[0:shape[0], off // 2:(off + nb) // 2]
        if dt != BF16:
            v = v.bitcast(dt)
        if len(shape) == 3:
            v = v.rearrange("p (a b) -> p a b", a=shape[1])
        elif len(shape) == 4:
            v = v.rearrange("p (a b c) -> p a b c", a=shape[1], b=shape[2])
        return v

    def dbg_out(self, name, shape, dt=F32):
        t = self.nc.dram_tensor("dbg_" + name, shape, dt, kind="ExternalOutput").ap()
        self.dbg[name] = t
        return t

    def evac_eng(self):
        self._rr += 1
        return "act" if self._rr % 2 else "dve"

    def wview(self, w2d, c0, ncols):
        return w2d.rearrange("(kc p) n -> p kc n", p=128)[:, :, c0:c0 + ncols]

    def build(self):
        nc, fw = self.nc, self.fw
        C = {}
        for k in ("ident_f", "ident_b", "onesblk", "ones_b", "ones_f", "tri", "mg_sb1", "mg_v1", "mg_e", "ustrict", "ebase"):
            v = self.hc[k]
            C[k] = self.sb("k_" + k, list(v.shape), CONST_DT[k])
            fw.dma("sp", C[k][:], self.cst[k][:, :])
        self.C = C
        self.cT = self.sb("cT", [128, 8], F32)
        fw.dma("sp", self.cT[:], self.inp["cT"][:, :])
        self.b_adaT = self.sb("b_adaT", [128, 2, 48], F32)
        fw.dma("sp", self.b_adaT[:], self.inp["b_adaT"][:, :, :])
        self.nmixT = self.sb("nmixT", [128, 2, 8], F32)
        fw.dma("sp", self.nmixT[:], self.inp["nmixT"][:, :, :])
        self.nffnT = self.sb("nffnT", [128, 2, 8], F32)
        fw.dma("sp", self.nffnT[:], self.inp["nffnT"][:, :, :])
        self.qg = self.sb("qg", [64, 2, 24], F32)
        fw.dma("sp", self.qg[:], self.inp["qgT"][:, :, :])
        self.kg = self.sb("kg", [64, 2, 24], F32)
        fw.dma("sp", self.kg[:], self.inp["kgT"][:, :, :])
        self.bfg = self.sb("bfg", [6, 2], F32)
        fw.dma("sp", self.bfg[:], self.inp["bfg"][:, :])
        self.negb = self.sb("negb", [6, 2], F32)
        fw.ts("dve", self.negb[:], self.bfg[:], -1.0, None, op0=ALU.mult)
        self.AR = self.sb("arena", [128, A_END // 2], BF16)
        self.hT = self.sb("hT", [128, 8, T], BF16)
        self.xT = self.carve(A_X, [128, 8, T], F32)
        self.psb = [nc.alloc_psum_tensor("psb%d" % i, [128, 512], F32) for i in range(8)]
        self.cond = self.sb("cond", [128, 8], BF16)
        self.modT = self.sb("modT", [128, 2, 48], F32)
        self.a1 = self.sb("a1", [128, 2, 8], F32)
        self.a2 = self.sb("a2", [128, 2, 8], F32)
        self.sqb = [self.sb("sqb%d" % i, [128, 512], BF16) for i in range(3)]
        self.rstd = [self.sb("rstd%d" % i, [128, 512], F32) for i in range(2)]
        self.ntmp = [self.sb("ntmp%d" % i, [128, 512], F32) for i in range(3)]
        self.OT = [self.carve(A_OT + i * 4096, [128, T], BF16) for i in range(8)]
        self.VA = self.carve(A_VA, [128, 16, 6, 65], BF16)
        self.VB = self.carve(A_VB, [128, 16, 6, 65], BF16)
        self.Qb = [self.carve(A_QK + (2 * s) * 4096, [128, T], BF16) for s in range(2)]
        self.Kb = [self.carve(A_QK + (2 * s + 1) * 4096, [128, T], BF16) for s in range(2)]
        self.wqk = [self.carve(A_WQK + s * 4096, [128, 8, 256], BF16) for s in range(2)]
        self.wv = self.carve(A_WV, [128, 8, 384], BF16)
        self.accN = self.carve(A_ACCN, [128, T], F32)
        self.oddt = self.carve(A_ODD, [128, T], BF16)
        self.ptb = [self.carve(A_PT + i * 1024, [128, 512], BF16) for i in range(3)]
        self.q2b = [self.carve(A_Q2 + i * 1024, [128, 512], BF16) for i in range(2)]
        self.rqb = [self.carve(A_RQ + i * 2048, [128, 512], F32) for i in range(2)]
        self.rd = self.carve(A_RD, [128, 512], F32)
        self.bcs = self.carve(A_BC, [128, 512], F32)
        self.dilM = self.carve(A_DILM, [128, 12, 256], BF16)
        self.rbt4 = [self.carve(119296 + i * 1024, [128, 512], BF16) for i in range(4)]
        self.rbt = self.rbt4[0:2]
        self.rlt = [self.carve(119296 + 4096 + i * 2048, [128, 512], F32) for i in range(2)]

        self.stage_load_x()
        self.stage_adaln()
        for l in range(self.nlayers):
            self.stage_rmsnorm(l, 0)
            if self.stop_after == "norm1":
                break
            fw.dma("sp", self.xs[:, :], self.xT.rearrange("p a b -> p (a b)"))
            if l == 0:
                self.stage_dilmask()
            self.stage_attention(l)
            if self.stop_after == "attn":
                break
            fw.dma("sp", self.xT.rearrange("p a b -> p (a b)"), self.xs[:, :])
            self.stage_outproj(l)
            if self.stop_after == "outproj":
                break
            self.stage_rmsnorm(l, 1, rt_ps=(self.psb[7] if l % 2 == 1 else None))
            if l % 2 == 0:
                self.ffn_expert(l, self.inp["w_ffn_gate"][0], self.inp["w_ffn_up"][0], self.inp["w_ffn_down"][0], None)
                if self.sparse and self.stop_after is None and l == 0:
                    zt = self.carve(A_VB, [128, 4104], BF16)
                    fw.memset("pool", zt, 0.0)
                    hsv = self.HS.rearrange("(p r) c -> p (r c)", p=128)
                    for i in range(32):
                        fw.dma("sp", hsv[:, i * 4104:(i + 1) * 4104], zt)
            elif self.sparse and l == self.nlayers - 1 and self.stop_after is None:
                self.stage_moe_sparse(l)
                self.final_done = True
            else:
                self.stage_moe(l)
            if self.stop_after == "ffn":
                break
        if self.stop_after is None and not getattr(self, "final_done", False):
            self.stage_output()
        for nm in self.debug:
            if nm == "hT":
                o = self.dbg_out("hT", [128, 8, T], BF16)
                fw.dma("sp", o[:, :, :], self.hT[:])
            elif nm == "xT":
                o = self.dbg_out("xT", [128, 8, T], F32)
                fw.dma("sp", o[:, :, :], self.xT)
            elif nm == "OT":
                o = self.dbg_out("OT", [8, 128, T], BF16)
                for i in range(8):
                    fw.dma("sp", o[i], self.OT[i])
            elif nm == "modT":
                o = self.dbg_out("modT", [128, 2, 48], F32)
                fw.dma("sp", o[:, :, :], self.modT[:])
        self.stats = fw.emit()

    def stage_load_x(self):
        fw, C = self.fw, self.C
        xin = [self.carve(i * 4096, [128, D], F32) for i in range(4)]
        for g in range(4):
            for i in range(4):
                tt = g * 4 + i
                fw.dma("sp", xin[i], self.inp["x"][tt * 128:(tt + 1) * 128, :])
            for dc in range(8):
                pb = self.psb[dc % 4]
                for i in range(4):
                    fw.tr(pb[:, i * 128:(i + 1) * 128], xin[i][:, dc * 128:(dc + 1) * 128], C["ident_f"][:])
                fw.copy(self.evac_eng(), self.xT[:, dc, g * 512:(g + 1) * 512], pb[:, :])

    def stage_adaln(self):
        fw, C = self.fw, self.C
        fw.act(self.cond[:], self.cT[:], AF.Silu)
        wbuf = [self.carve(i * 8192, [128, 8, 512], BF16) for i in range(2)]
        n = 0
        for l in range(self.nlayers):
            pm = self.psb[4 + l]
            wv = self.inp["w_ada"][l].rearrange("(kc p) n -> p kc n", p=128)
            for half in range(12):
                wb = wbuf[n % 2]
                n += 1
                fw.dma("pool", wb, wv[:, :, half * 512:(half + 1) * 512])
                for c4 in range(4):
                    j = half * 4 + c4
                    for kc in range(8):
                        fw.mm(pm[:, j:j + 1], wb[:, kc, c4 * 128:(c4 + 1) * 128], self.cond[:, kc:kc + 1],
                              start=(kc == 0), stop=(kc == 7))
            fw.tt("dve", self.modT[:, l, :], pm[:, 0:48], self.b_adaT[:, l, :], ALU.add)
            fw.stt(self.a1[:, l, :], self.modT[:, l, 8:16], 1.0, self.nmixT[:, l, :], ALU.add, ALU.mult)
            fw.stt(self.a2[:, l, :], self.modT[:, l, 32:40], 1.0, self.nffnT[:, l, :], ALU.add, ALU.mult)

    def stage_rmsnorm(self, l, which, rt_ps=None):
        fw, C = self.fw, self.C
        a = self.a1 if which == 0 else self.a2
        shoff = 0 if which == 0 else 24
        for tt in range(4):
            cs = slice(tt * 512, (tt + 1) * 512)
            pss = self.psb[tt % 2]
            for dc in range(8):
                sq = self.sqb[dc % 3]
                fw.act(sq[:], self.xT[:, dc, cs], AF.Square)
                fw.mm(pss[:, :], C["ones_b"][:], sq[:], start=(dc == 0), stop=(dc == 7))
            rs = self.rstd[tt % 2]
            fw.act(rs[:], pss[:, :], AF.Ln, bias=EPS, scale=1.0 / D)
            fw.act(rs[:], rs[:], AF.Exp, scale=-0.5)
            if rt_ps is not None:
                for i in range(4):
                    fw.mm(rt_ps[:, tt * 4 + i:tt * 4 + i + 1], rs[0:1, i * 128:(i + 1) * 128], C["ones_f"][0:1, 0:1])
            for dc in range(8):
                nt = self.ntmp[dc % 3]
                fw.stt(nt[:], self.xT[:, dc, cs], a[:, l, dc:dc + 1], rs[:], ALU.mult, ALU.mult)
                fw.act(self.hT[:, dc, cs], nt[:], AF.Identity, bias=self.modT[:, l, shoff + dc:shoff + dc + 1])

    def stage_dilmask(self):
        fw = self.fw
        tmp = self.carve(A_X, [128, 3072], F32)
        fw.dma("sp", tmp, self.cst["dilbias"][:, :])
        fw.act(self.dilM.rearrange("p a b -> p (a b)"), tmp, AF.Exp)

    def fox_prep(self, l):
        fw = self.fw
        wf = self.carve(A_Z2, [128, 8, 6], BF16)
        fw.dma("pool", wf, self.wview(self.inp["w_in"][l], FOFF, 6))
        fl = self.carve(A_FST, [6, T], F32)
        G = self.carve(A_FST + 8192, [6, T], F32)
        r1 = self.carve(A_FST + 16384, [6, T], F32)
        kp = self.carve(A_FST + 24576, [6, 3, T], BF16)
        qp = self.carve(A_FST + 36864, [6, 3, T], BF16)
        for tt in range(4):
            cs = slice(tt * 512, (tt + 1) * 512)
            ps = self.psb[tt % 2]
            for kc in range(8):
                fw.mm(ps[0:6, :], wf[:, kc, :], self.hT[:, kc, cs], start=(kc == 0), stop=(kc == 7))
            fw.act(fl[:, cs], ps[0:6, :], AF.Exp, scale=-1.0, bias=self.negb[0:6, l:l + 1])
        fw.act(fl, fl, AF.Ln, bias=1.0)
        fw.memset("dve", r1, 1.0)
        fw.gen("dve", "tensor_tensor_scan", [fl, r1], [G], out=G, data0=r1, data1=fl, initial=0.0,
               op0=ALU.mult, op1=ALU.add)
        fw.ts("dve", kp[:, 0, :], G, 8.0, None, op0=ALU.mult)
        fw.stt(r1, G, 8.0, kp[:, 0, :], ALU.mult, ALU.subtract)
        fw.copy("dve", kp[:, 1, :], r1)
        fw.tt("dve", r1, r1, kp[:, 1, :], ALU.subtract)
        fw.copy("dve", kp[:, 2, :], r1)
        kpf = kp.rearrange("p a b -> p (a b)")
        qpf = qp.rearrange("p a b -> p (a b)")
        fw.ts("dve", qpf, kpf, -1.0, None, op0=ALU.mult)
        fw.dma("sp", self.fsc[0], qpf)
        fw.dma("sp", self.fsc[1], kpf)

    def proj_qk(self, l, h, s, d):
        fw, C = self.fw, self.C
        w = self.wqk[s]
        Wl = self.inp["w_in"][l]
        fw.dma("pool", w[:, :, 0:128], self.wview(Wl, QOFF + h * 64, 128))
        fw.dma("pool", w[:, :, 128:256], self.wview(Wl, KOFF + h * 64, 128))
        fw.memset("pool", self.Qb[s][64:128, :], 0.0)
        fw.memset("pool", self.Kb[s][64:128, :], 0.0)
        for which, dst, gain in ((0, self.Qb[s], self.qg), (1, self.Kb[s], self.kg)):
            for tt in range(4):
                cs = slice(tt * 512, (tt + 1) * 512)
                ps = self.psb[tt % 2]
                for kc in range(8):
                    fw.mm(ps[:, :], w[:, kc, which * 128:(which + 1) * 128], self.hT[:, kc, cs],
                          start=(kc == 0), stop=(kc == 7))
                q2 = self.q2b[tt % 2]
                fw.act(q2, ps[:, :], AF.Square)
                p2 = self.psb[2]
                fw.mm(p2[:, :], C["onesblk"][:], q2, start=True, stop=True)
                rq = self.rqb[tt % 2]
                fw.act(rq[0:64, :], p2[0:64, :], AF.Ln, bias=EPS, scale=1.0 / 64)
                fw.act(rq[0:64, :], rq[0:64, :], AF.Exp, scale=-0.5)
                if d == 1:
                    o, i0, i1 = dst[0:64, cs], ps[0:64, :], rq[0:64, :]
                else:
                    n0, n1 = tt * 512 // d, (tt + 1) * 512 // d
                    o = dst[0:64, :].rearrange("p (r n) -> p n r", r=d)[:, n0:n1, :]
                    i0 = ps[0:64, :].rearrange("p (n r) -> p n r", r=d)
                    i1 = rq[0:64, :].rearrange("p (n r) -> p n r", r=d)
                fw.stt(o, i0, gain[:, l, h:h + 1], i1, ALU.mult, ALU.mult)

    def load_wv(self, l, grp):
        self.fw.dma("pool", self.wv, self.wview(self.inp["w_in"][l], VOFF + grp * 384, 384))

    def proj_v(self, nh, d, Vbuf, slot0, wcol0):
        fw = self.fw
        L = T // d
        nblk = L // 128
        for pb in range(16):
            r, nb = pb // nblk, pb % nblk
            ps = self.psb[pb % 2]
            if d == 1:
                tok = slice(pb * 128, (pb + 1) * 128)
            else:
                st0 = nb * 128 * d + r
                tok = slice(st0, st0 + 127 * d + 1, d)
            for kc in range(8):
                fw.mm(ps[:, 0:nh * 64], self.hT[:, kc, tok], self.wv[:, kc, wcol0:wcol0 + nh * 64],
                      start=(kc == 0), stop=(kc == 7))
            fw.copy(self.evac_eng(), Vbuf[:, pb, slot0:slot0 + nh, 0:64],
                    ps[:, 0:nh * 64].rearrange("p (h e) -> p h e", h=nh))

    def recip_row(self, den, k, eng):
        fw = self.fw
        rb = self.rbt[k % 2] if eng == "dve" else self.rbt4[k % 4]
        if eng == "dve":
            nc = self.nc
            o_ = rb[64:65, :]

            def f(o_=o_, den=den):
                with nc.allow_low_precision("bf16 reciprocal row feeds a bf16 broadcast matmul"):
                    return nc.vector.reciprocal(out=o_, in_=den)
            fw.op("dve", f, reads=[den], writes=[o_])
        else:
            lt = self.rlt[k % 2]
            fw.act(lt[64:65, :], den, AF.Ln)
            fw.act(rb[64:65, :], lt[64:65, :], AF.Exp, scale=-1.0)
        return rb

    def bcast_mul(self, src, rb, dest):
        fw, C = self.fw, self.C
        bp = self.psb[2]
        fw.mm(bp[:, :], C["ones_b"][64:65, :], rb[64:65, :], start=True, stop=True)
        fw.tt("dve", dest, src, bp[0:64, :], ALU.mult)

    def attn_causal(self, s, Vbuf, vslot, dest, hook=None, pending=None):
        fw, C = self.fw, self.C
        Qh, Kh = self.Qb[s], self.Kb[s]
        otS = (self.rd, self.bcs)
        for j in range(4):
            ot = self.psb[6 + j % 2]
            nkb = 4 * j + 4

            def S(kb):
                c0 = max(0, kb - 4 * j) * 128
                st = self.psb[3 + kb % 3]
                fw.mm(st[:, c0:512], Kh[:, kb * 128:(kb + 1) * 128], Qh[:, j * 512 + c0:(j + 1) * 512],
                      start=True, stop=True)
            S(0)
            if nkb > 1:
                S(1)
            for kb in range(nkb):
                if kb + 2 < nkb:
                    S(kb + 2)
                c0 = max(0, kb - 4 * j) * 128
                st = self.psb[3 + kb % 3]
                pt = self.ptb[kb % 3]
                fw.act(pt[:, c0:512], st[:, c0:512], AF.Exp, scale=0.125)
                if kb >= 4 * j:
                    fw.tt("pool", pt[:, c0:c0 + 128], pt[:, c0:c0 + 128], C["tri"][:], ALU.mult)
                fw.mm(ot[0:65, c0:512], Vbuf[:, kb, vslot, 0:65], pt[:, c0:512], start=(kb == 0), stop=(kb == nkb - 1))
                if kb == min(5, nkb - 1) and pending is not None:
                    pending()
                    pending = None
                if hook is not None and j == 2 and kb == 5:
                    hook()
                    hook = None
            if pending is not None:
                pending()
                pending = None

            o = otS[j % 2]
            fw.copy("act", o[0:65, :], ot[0:65, :])
            rb = self.recip_row(o[64:65, :], j, "dve")

            def fin(j=j, o=o, rb=rb):
                self.bcast_mul(o[0:64, :], rb, dest[:, j * 512:(j + 1) * 512])
            pending = fin
        return pending

    def fox_aug(self, h, s):
        fw = self.fw
        Qh, Kh = self.Qb[s], self.Kb[s]
        fw.dma("sp", Qh[64:67, :], self.fsc[0, h].rearrange("(a t) -> a t", a=3))
        fw.dma("sp", Qh[67:70, :], self.cst["fox_ones"][:, :])
        fw.dma("sp", Kh[64:67, :], self.cst["fox_ones"][:, :])
        fw.dma("sp", Kh[67:70, :], self.fsc[1, h].rearrange("(a t) -> a t", a=3))

    def moba_prep(self, h6, s):
        fw, C = self.fw, self.C
        Qh, Kh = self.Qb[s], self.Kb[s]
        km = self.carve(A_KM, [128, 8], F32)
        kmb = self.carve(A_KM + 64, [128, 8], BF16)
        fw.gen("dve", "tensor_reduce", [Kh[0:64, :]], [km[0:64, :]], out=km[0:64, :],
               in_=Kh[0:64, :].rearrange("p (b n) -> p b n", b=8), axis=AX.X, op=ALU.add)
        fw.ts("dve", kmb[0:64, :], km[0:64, :], 1.0 / 256, None, op0=ALU.mult)
        gp = self.psb[2]
        for qb in range(16):
            fw.mm(gp[:, qb * 8:(qb + 1) * 8], Qh[0:64, qb * 128:(qb + 1) * 128], kmb[0:64, :], start=True, stop=True)
        gm = self.carve(A_MG, [128, 128], F32)
        m8 = self.carve(A_MG + 512, [128, 128], F32)
        sel = self.carve(A_MG + 1024, [128, 128], F32)
        mbb = self.carve(A_MG + 1536, [128, 128], BF16)
        fw.tt("dve", gm, gp[:, 0:128], C["mg_sb1"][:], ALU.add)
        for qb in range(16):
            sl = slice(qb * 8, (qb + 1) * 8)
            fw.gen("dve", "max", [gm[:, sl]], [m8[:, sl]], out=m8[:, sl], in_=gm[:, sl])
        gm3 = gm.rearrange("p (a b) -> p a b", b=8)
        m83 = m8.rearrange("p (a b) -> p a b", b=8)
        sel3 = sel.rearrange("p (a b) -> p a b", b=8)
        fw.tt("dve", sel3, gm3, m83[:, :, 2:3].to_broadcast([128, 16, 8]), ALU.is_ge)
        fw.tt("dve", sel, sel, C["mg_v1"][:], ALU.mult)
        fw.tt("dve", sel, sel, C["mg_e"][:], ALU.add)
        fw.ts("dve", mbb, sel, 240000.0, -240000.0, op0=ALU.mult, op1=ALU.add)
        for tt in range(4):
            pp = self.psb[tt % 2]
            for i in range(4):
                qb = tt * 4 + i
                fw.mm(pp[64:72, i * 128:(i + 1) * 128], mbb[:, qb * 8:(qb + 1) * 8], C["ident_b"][:], start=True, stop=True)
            fw.copy(self.evac_eng(), Qh[64:72, tt * 512:(tt + 1) * 512], pp[64:72, :])
        fw.dma("sp", Qh[72:78, :], self.cst["moba_aq"][h6])
        fw.dma("sp", Kh[64:72, :], self.cst["blkind"][:, :])
        fw.dma("sp", Kh[72:78, :], self.cst["moba_ak"][h6])

    def attn_dil(self, g, j, s, Vbuf, vslot, first, hook=None, pending=None):
        fw = self.fw
        d = DIL[g][1]
        L = T // d
        nblk = L // 128
        Qh, Kh = self.Qb[s], self.Kb[s]
        M = self.dilM[:, g * 4 + j, :]
        accN = self.accN

        def S(pbq):
            nb = pbq % nblk
            st = self.psb[3 + pbq % 3]
            qs = slice(pbq * 128, (pbq + 1) * 128)
            fw.mm(st[:, 0:128], Kh[:, qs], Qh[:, qs], start=True, stop=True)
            if nb > 0:
                fw.mm(st[:, 128:256], Kh[:, (pbq - 1) * 128:pbq * 128], Qh[:, qs], start=True, stop=True)
        S(0)
        S(1)
        for c in range(4):
            ot = self.psb[6 + c % 2]
            for i in range(4):
                pbq = c * 4 + i
                nb = pbq % nblk
                if pbq + 2 < 16:
                    S(pbq + 2)
                st = self.psb[3 + pbq % 3]
                pt = self.ptb[pbq % 3]
                w = 256 if nb > 0 else 128
                fw.act(pt[:, 0:w], st[:, 0:w], AF.Exp, scale=0.125)
                fw.tt("pool", pt[:, 0:w], pt[:, 0:w], M[:, 0:w], ALU.mult)
                oc = ot[0:65, i * 128:(i + 1) * 128]
                fw.mm(oc, Vbuf[:, pbq, vslot, 0:65], pt[:, 0:128], start=True, stop=(nb == 0))
                if nb > 0:
                    fw.mm(oc, Vbuf[:, pbq - 1, vslot, 0:65], pt[:, 128:256], start=False, stop=True)
                if pending is not None and pbq == 2:
                    pending()
                    pending = None
            if d == 1:
                dst, src = accN[0:65, c * 512:(c + 1) * 512], ot[0:65, :]
            elif d == 4:
                dst, src = accN[0:65, c:c + 4 * 511 + 1:4], ot[0:65, :]
            else:
                dst = accN[0:65, :].rearrange("p (n r) -> p r n", r=16)[:, 4 * c:4 * c + 4, :]
                src = ot[0:65, :].rearrange("p (r n) -> p r n", r=4)
            if first:
                fw.copy("dve", dst, src)
            else:
                fw.tt("dve", dst, src, dst, ALU.add)
            if hook is not None and c == 1:
                hook()
                hook = None

    def stage_attention(self, l):
        fw = self.fw
        heads = self.heads or ("fox", "moba", "dil")
        fw.memset("pool", self.VA[:, :, :, 64:65], 1.0)
        fw.memset("pool", self.VB[:, :, :, 64:65], 1.0)
        jobs = []
        if "fox" in heads:
            self.fox_prep(l)
            for h in range(6):
                s = h % 2

                def prep(h=h, s=s):
                    if h == 0:
                        self.load_wv(l, 0)
                        self.proj_v(6, 1, self.VA, 0, 0)
                    self.proj_qk(l, h, s, 1)
                    self.fox_aug(h, s)

                def attn(hook, pending, h=h, s=s):
                    dest = self.OT[h // 2][0:64, :] if h % 2 == 0 else self.oddt[0:64, :]
                    p = self.attn_causal(s, self.VA, h, dest, hook=hook, pending=pending)
                    if h % 2 == 1:
                        def fin2(p=p, h=h):
                            p()
                            fw.dma("sp", self.OT[h // 2][64:128, :], self.oddt[0:64, :])
                        return fin2
                    return p
                jobs.append((prep, attn))
        if "moba" in heads:
            for h6 in range(6):
                h = 6 + h6
                s = h % 2

                def prep(h=h, h6=h6, s=s):
                    if h6 == 0:
                        self.load_wv(l, 1)
                        self.proj_v(6, 1, self.VB, 0, 0)
                    self.proj_qk(l, h, s, 1)
                    self.moba_prep(h6, s)

                def attn(hook, pending, h6=h6, s=s):
                    dest = self.OT[3 + h6 // 2][0:64, :] if h6 % 2 == 0 else self.oddt[0:64, :]
                    p = self.attn_causal(s, self.VB, h6, dest, hook=hook, pending=pending)
                    if h6 % 2 == 1:
                        def fin2(p=p, h6=h6):
                            p()
                            fw.dma("sp", self.OT[3 + h6 // 2][64:128, :], self.oddt[0:64, :])
                        return fin2
                    return p
                jobs.append((prep, attn))
        if "dil" in heads:
            n = 0
            for j in range(4):
                for g in range(3):
                    h = 12 + g * 4 + j
                    Vbuf, vslot = (self.VA, h - 12) if h < 18 else (self.VB, h - 18)
                    s = n % 2
                    first_dil = (n == 0)
                    n += 1

                    def prep(h=h, s=s, g=g, first_dil=first_dil):
                        if first_dil:
                            self.load_wv(l, 2)
                            self.proj_v(4, 1, self.VA, 0, 0)
                            self.proj_v(2, 4, self.VA, 4, 256)
                        if h == 20:
                            self.load_wv(l, 3)
                            self.proj_v(2, 4, self.VB, 0, 0)
                            self.proj_v(4, 16, self.VB, 2, 128)
                        self.proj_qk(l, h, s, DIL[g][1])

                    def attn(hook, pending, g=g, j=j, s=s, Vbuf=Vbuf, vslot=vslot):
                        self.attn_dil(g, j, s, Vbuf, vslot, g == 0, hook=hook, pending=pending)
                        if g == 2:
                            dest = self.OT[6 + j // 2][0:64, :] if j % 2 == 0 else self.oddt[0:64, :]
                            rbs = []
                            for tt in range(4):
                                cs = slice(tt * 512, (tt + 1) * 512)
                                rbs.append(self.recip_row(self.accN[64:65, cs], tt, "act"))

                            def fin(j=j, dest=dest):
                                for tt in range(4):
                                    cs = slice(tt * 512, (tt + 1) * 512)
                                    rb = self.rbt4[tt]
                                    self.bcast_mul(self.accN[0:64, cs], rb, dest[:, cs])
                                if j % 2 == 1:
                                    fw.dma("sp", self.OT[6 + j // 2][64:128, :], self.oddt[0:64, :])
                            return fin
                        return None
                    jobs.append((prep, attn))
        pending = None
        if jobs:
            jobs[0][0]()
        for i, (prep, attn) in enumerate(jobs):
            hook = jobs[i + 1][0] if i + 1 < len(jobs) else None
            pending = attn(hook, pending)
        if pending is not None:
            pending()

    def stage_outproj(self, l):
        fw = self.fw
        yT = [self.carve(A_Y + i * 4096, [128, T], BF16) for i in range(8)]
        wbr = [self.carve(A_Z2 + i * 2048, [128, 8, 128], BF16) for i in range(2)]
        wg = [self.carve(A_Z2 + 4096 + i * 2048, [128, 8, 128], BF16) for i in range(4)]
        Wl = self.inp["w_in"][l]
        ng = 0
        for fc in range(8):
            fs = slice(fc * 128, (fc + 1) * 128)
            wb = wbr[fc % 2]
            fw.dma("pool", wb[:, 0:3, :], self.inp["w_br_fox"][l].rearrange("(kc p) n -> p kc n", p=128)[:, :, fs])
            fw.dma("pool", wb[:, 3:6, :], self.inp["w_br_moba"][l].rearrange("(kc p) n -> p kc n", p=128)[:, :, fs])
            fw.dma("pool", wb[:, 6:8, :], self.inp["w_br_dil"][l].rearrange("(kc p) n -> p kc n", p=128)[:, :, fs])
            wgs = []
            for br in range(3):
                w = wg[ng % 4]
                ng += 1
                fw.dma("pool", w, self.wview(Wl, GOFF + br * 1024 + fc * 128, 128))
                wgs.append(w)
            for tt in range(4):
                cs = slice(tt * 512, (tt + 1) * 512)
                nt0, nt1 = self.ntmp[0], self.ntmp[1 + tt % 2]
                for br, (k0, nk) in enumerate(((0, 3), (3, 3), (6, 2))):
                    pz = self.psb[br]
                    for kc in range(nk):
                        fw.mm(pz[:, :], wb[:, k0 + kc, :], self.OT[k0 + kc][:, cs], start=(kc == 0), stop=(kc == nk - 1))
                    pg = self.psb[3 + br]
                    for kc in range(8):
                        fw.mm(pg[:, :], wgs[br][:, kc, :], self.hT[:, kc, cs], start=(kc == 0), stop=(kc == 7))
                    sg = self.sqb[br]
                    fw.act(sg[:], pg[:, :], AF.Sigmoid)
                    if br == 0:
                        fw.tt("dve", nt0[:], pz[:, :], sg[:], ALU.mult)
                    else:
                        fw.tt("dve", nt1[:], pz[:, :], sg[:], ALU.mult)
                        if br == 1:
                            fw.tt("pool", nt0[:], nt0[:], nt1[:], ALU.add)
                        else:
                            fw.tt("pool", yT[fc][:, cs], nt0[:], nt1[:], ALU.add)
        for fc in range(8):
            w = wg[ng % 4]
            ng += 1
            fw.dma("pool", w, self.wview(self.inp["w_out"][l], fc * 128, 128))
            for tt in range(4):
                cs = slice(tt * 512, (tt + 1) * 512)
                ps = self.psb[6 + tt % 2]
                for kc in range(8):
                    fw.mm(ps[:, :], w[:, kc, :], yT[kc][:, cs], start=(kc == 0), stop=(kc == 7))
                fw.stt(self.xT[:, fc, cs], ps[:, :], self.modT[:, l, 16 + fc:17 + fc], self.xT[:, fc, cs], ALU.mult, ALU.add)

    def ffn_expert(self, l, wg_ap, wu_ap, wd_ap, combbc):
        fw = self.fw
        actT = self.carve(A_ACT, [128, 11, T], BF16)
        wgu = [self.carve(A_WGU + i * 2048, [128, 8, 128], BF16) for i in range(4)]
        wdt = [self.carve(A_WD + i * 2816, [128, 11, 128], BF16) for i in range(2)]
        sgt = [self.carve(A_SG + i * 1024, [128, 512], BF16) for i in range(3)]
        if not hasattr(self, "_ffc"):
            self._ffc = 0
            self._wdc = 0
        for half in range(2):
            for fi in range(11):
                ffc = half * 11 + fi
                wgt = wgu[(2 * self._ffc) % 4]
                wut = wgu[(2 * self._ffc + 1) % 4]
                self._ffc += 1
                fw.dma("pool", wgt, self.wview(wg_ap, ffc * 128, 128))
                fw.dma("pool", wut, self.wview(wu_ap, ffc * 128, 128))
                for tt in range(4):
                    cs = slice(tt * 512, (tt + 1) * 512)
                    pg = self.psb[(2 * tt) % 4]
                    pu = self.psb[(2 * tt + 1) % 4]
                    for kc in range(8):
                        fw.mm(pg[:, :], wgt[:, kc, :], self.hT[:, kc, cs], start=(kc == 0), stop=(kc == 7))
                    for kc in range(8):
                        fw.mm(pu[:, :], wut[:, kc, :], self.hT[:, kc, cs], start=(kc == 0), stop=(kc == 7))
                    sg = sgt[tt % 3]
                    fw.act(sg, pg[:, :], AF.Silu)
                    if combbc is not None:
                        fw.tt("pool", sg, sg, combbc[:, cs], ALU.mult)
                    fw.tt("dve", actT[:, fi, cs], pu[:, :], sg, ALU.mult)
            for fc in range(8):
                wd = wdt[self._wdc % 2]
                self._wdc += 1
                fw.dma("pool", wd, wd_ap.rearrange("(f p) n -> p f n", p=128)[:, half * 11:(half + 1) * 11, fc * 128:(fc + 1) * 128])
                for tt in range(4):
                    cs = slice(tt * 512, (tt + 1) * 512)
                    ps = self.psb[4 + tt % 2]
                    for fi in range(11):
                        fw.mm(ps[:, :], wd[:, fi, :], actT[:, fi, cs], start=(fi == 0), stop=(fi == 10))
                    fw.stt(self.xT[:, fc, cs], ps[:, :], self.modT[:, l, 40 + fc:41 + fc], self.xT[:, fc, cs], ALU.mult, ALU.add)

    def stage_moe(self, l):
        fw, C = self.fw, self.C
        R0 = A_Z2 + 8192
        wr = self.carve(R0, [128, 8, 8], F32)
        wr2 = self.carve(R0 + 256, [128, 8, 8], F32)
        brt = self.carve(R0 + 512, [128, 8], F32)
        crow = self.carve(R0 + 544, [128, 8], F32)
        cb = self.carve(R0 + 576, [128, 8], F32)
        rt = self.carve(R0 + 608, [128, 16], F32)
        lg = self.carve(R0 + 1024, [128, 16, 8], F32)
        m8 = self.carve(R0 + 1536, [128, 16, 8], F32)
        eq = self.carve(R0 + 2048, [128, 16, 8], F32)
        comb = self.carve(R0 + 2560, [128, 16, 8], F32)
        w1 = self.carve(R0 + 3072, [128, 16], F32)
        w2 = self.carve(R0 + 3136, [128, 16], F32)
        e21 = self.carve(R0 + 3200, [128, 16], F32)
        fw.dma("sp", wr, self.inp["w_router"][0].rearrange("(kc p) e -> p kc e", p=128))
        fw.dma("sp", brt[0:1, :], self.inp["b_router"][0:1, :])
        for kc in range(8):
            fw.ts("dve", wr2[:, kc, :], wr[:, kc, :], self.a2[:, l, kc:kc + 1], None, op0=ALU.mult)
        pc = self.psb[2]
        for kc in range(8):
            fw.mm(pc[0:1, 0:8], self.modT[:, l, 24 + kc:25 + kc], wr[:, kc, :], start=(kc == 0), stop=(kc == 7))
        fw.tt("dve", crow[0:1, :], pc[0:1, 0:8], brt[0:1, :], ALU.add)
        pcb = self.psb[3]
        fw.mm(pcb[:, 0:8], C["ones_f"][0:1, :], crow[0:1, :], start=True, stop=True)
        fw.copy("dve", cb, pcb[:, 0:8])
        pl = self.psb[6]
        for t16 in range(16):
            for kc in range(8):
                fw.mm(pl[:, t16 * 8:(t16 + 1) * 8], self.xT[:, kc, t16 * 128:(t16 + 1) * 128], wr2[:, kc, :],
                      start=(kc == 0), stop=(kc == 7))
        fw.copy("dve", rt, self.psb[7][:, 0:16])
        fw.tt("dve", lg, pl[:, 0:128].rearrange("p (a b) -> p a b", b=8), rt.unsqueeze(2).to_broadcast([128, 16, 8]), ALU.mult)
        fw.tt("dve", lg, lg, cb.unsqueeze(1).to_broadcast([128, 16, 8]), ALU.add)
        for t16 in range(16):
            fw.gen("dve", "max", [lg[:, t16, :]], [m8[:, t16, :]], out=m8[:, t16, :], in_=lg[:, t16, :])
        fw.tt("dve", e21, m8[:, :, 1], m8[:, :, 0], ALU.subtract)
        fw.act(e21, e21, AF.Exp)
        fw.ts("dve", w1, e21, 1.0, None, op0=ALU.add)
        fw.gen("dve", "reciprocal", [w1], [w1], out=w1, in_=w1)
        fw.tt("dve", w2, e21, w1, ALU.mult)
        fw.tt("dve", eq, lg, m8[:, :, 0:1].to_broadcast([128, 16, 8]), ALU.is_equal)
        fw.tt("dve", comb, eq, w1.unsqueeze(2).to_broadcast([128, 16, 8]), ALU.mult)
        fw.tt("dve", eq, lg, m8[:, :, 1:2].to_broadcast([128, 16, 8]), ALU.is_equal)
        fw.tt("dve", eq, eq, w2.unsqueeze(2).to_broadcast([128, 16, 8]), ALU.mult)
        fw.tt("dve", comb, comb, eq, ALU.add)
        if "comb" in self.debug:
            o = self.dbg_out("comb", [128, 16, 8], F32)
            fw.dma("sp", o[:, :, :], comb)
        cbc = [self.carve(A_Z2 + i * 4096, [128, T], BF16) for i in range(2)]
        for e in range(NEXP):
            cc = cbc[e % 2]
            for tt in range(4):
                nt = self.ntmp[tt % 3]
                for i in range(4):
                    fw.ts("dve", nt[:, i * 128:(i + 1) * 128], C["ident_f"][:], comb[:, tt * 4 + i, e:e + 1], None, op0=ALU.mult)
                pb = self.psb[6 + tt % 2]
                fw.mm(pb[:, :], C["ones_f"][:], nt[:], start=True, stop=True)
                fw.copy("act", cc[:, tt * 512:(tt + 1) * 512], pb[:, :])
            self.ffn_expert(l, self.inp["w_exp_gate"][0, e], self.inp["w_exp_up"][0, e], self.inp["w_exp_down"][0, e], cc)

    def stage_output(self):
        fw, C = self.fw, self.C
        xo = [self.carve(i * 4096, [128, D], F32) for i in range(2)]
        for t16 in range(16):
            xb = xo[t16 % 2]
            for half in range(2):
                pb = self.psb[(t16 * 2 + half) % 4]
                for i in range(4):
                    dc = half * 4 + i
                    fw.tr(pb[:, i * 128:(i + 1) * 128], self.xT[:, dc, t16 * 128:(t16 + 1) * 128], C["ident_f"][:])
                fw.copy(self.evac_eng(), xb[:, half * 512:(half + 1) * 512], pb[:, :])
            fw.dma("sp", self.out[t16 * 128:(t16 + 1) * 128, :], xb)

    def stage_moe_sparse(self, l):
        fw, C, nc = self.fw, self.C, self.nc
        I32 = mybir.dt.int32
        R0 = A_Z2
        wr = self.carve(R0, [128, 8, 8], F32)
        wr2 = self.carve(R0 + 256, [128, 8, 8], F32)
        brt = self.carve(R0 + 512, [128, 8], F32)
        crow = self.carve(R0 + 544, [128, 8], F32)
        cb = self.carve(R0 + 576, [128, 8], F32)
        rt = self.carve(R0 + 608, [128, 16], F32)
        e21 = self.carve(R0 + 672, [128, 16], F32)
        lg = self.carve(R0 + 1024, [128, 16, 8], F32)
        m8 = self.carve(R0 + 1536, [128, 16, 8], F32)
        eq1 = self.carve(R0 + 2048, [128, 16, 8], F32)
        eq2 = self.carve(R0 + 2560, [128, 16, 8], F32)
        tot = self.carve(R0 + 3072, [128, 16, 8], F32)
        pre = self.carve(R0 + 3584, [128, 16, 8], F32)
        pos = self.carve(R0 + 4096, [128, 16, 8], F32)
        tmp = self.carve(R0 + 4608, [128, 16, 8], F32)
        maskb = self.carve(R0 + 5120, [128, 128], BF16)
        gf = self.carve(R0 + 5376, [128, 16], F32)
        cntf = self.carve(R0 + 5440, [128, 8], F32)
        w1 = self.sb("moe_w1", [128, 16], F32)[:, :]
        w2 = self.sb("moe_w2", [128, 16], F32)[:, :]
        gi = [self.sb("moe_gi%d" % k, [128, 16], I32)[:, :] for k in range(2)]
        cnti = self.sb("moe_cnt", [1, 8], I32)[:, :]
        fw.dma("sp", wr, self.inp["w_router"][0].rearrange("(kc p) e -> p kc e", p=128))
        fw.dma("sp", brt[0:1, :], self.inp["b_router"][0:1, :])
        for kc in range(8):
            fw.ts("dve", wr2[:, kc, :], wr[:, kc, :], self.a2[:, l, kc:kc + 1], None, op0=ALU.mult)
        pc = self.psb[2]
        for kc in range(8):
            fw.mm(pc[0:1, 0:8], self.modT[:, l, 24 + kc:25 + kc], wr[:, kc, :], start=(kc == 0), stop=(kc == 7))
        fw.tt("dve", crow[0:1, :], pc[0:1, 0:8], brt[0:1, :], ALU.add)
        pcb = self.psb[3]
        fw.mm(pcb[:, 0:8], C["ones_f"][0:1, :], crow[0:1, :], start=True, stop=True)
        fw.copy("dve", cb, pcb[:, 0:8])
        pl = self.psb[6]
        for t16 in range(16):
            for kc in range(8):
                fw.mm(pl[:, t16 * 8:(t16 + 1) * 8], self.xT[:, kc, t16 * 128:(t16 + 1) * 128], wr2[:, kc, :],
                      start=(kc == 0), stop=(kc == 7))
        fw.copy("dve", rt, self.psb[7][:, 0:16])
        bc3 = [128, 16, 8]
        fw.tt("dve", lg, pl[:, 0:128].rearrange("p (a b) -> p a b", b=8), rt.unsqueeze(2).to_broadcast(bc3), ALU.mult)
        fw.tt("dve", lg, lg, cb.unsqueeze(1).to_broadcast(bc3), ALU.add)
        for t16 in range(16):
            fw.gen("dve", "max", [lg[:, t16, :]], [m8[:, t16, :]], out=m8[:, t16, :], in_=lg[:, t16, :])
        fw.tt("dve", e21, m8[:, :, 1], m8[:, :, 0], ALU.subtract)
        fw.act(e21, e21, AF.Exp)
        fw.ts("dve", w1, e21, 1.0, None, op0=ALU.add)
        fw.gen("dve", "reciprocal", [w1], [w1], out=w1, in_=w1)
        fw.tt("dve", w2, e21, w1, ALU.mult)
        fw.tt("dve", eq1, lg, m8[:, :, 0:1].to_broadcast(bc3), ALU.is_equal)
        fw.tt("dve", eq2, lg, m8[:, :, 1:2].to_broadcast(bc3), ALU.is_equal)
        fl = lambda a: a.rearrange("p a b -> p (a b)")
        fw.tt("dve", maskb, fl(eq1), fl(eq2), ALU.add)
        ptot, pwi = self.psb[4], self.psb[5]
        fw.mm(ptot[:, 0:128], C["ones_b"][:], maskb, start=True, stop=True)
        for t16 in range(16):
            fw.mm(pwi[:, t16 * 8:(t16 + 1) * 8], C["ustrict"][:], maskb[:, t16 * 8:(t16 + 1) * 8], start=True, stop=True)
        fw.copy("dve", fl(tot), ptot[:, 0:128])
        fw.memset("dve", pre[:, 0, :], 0.0)
        for t16 in range(1, 16):
            fw.tt("dve", pre[:, t16, :], pre[:, t16 - 1, :], tot[:, t16 - 1, :], ALU.add)
        fw.tt("dve", cntf, pre[:, 15, :], tot[:, 15, :], ALU.add)
        fw.copy("dve", cnti[0:1, :], cntf[0:1, :])
        fw.tt("dve", fl(pos), pwi[:, 0:128], fl(pre), ALU.add)
        fw.tt("dve", fl(pos), fl(pos), C["ebase"][:], ALU.add)
        for k, eq in enumerate((eq1, eq2)):
            fw.tt("dve", tmp, pos, eq, ALU.mult)
            fw.gen("dve", "tensor_reduce", [tmp], [gf], out=gf, in_=tmp, axis=AX.X, op=ALU.add)
            fw.copy("dve", gi[k], gf)
        if "moeidx" in self.debug:
            o = self.dbg_out("gi0", [128, 16], I32)
            fw.dma("sp", o[:, :], gi[0][:])
            o = self.dbg_out("gi1", [128, 16], I32)
            fw.dma("sp", o[:, :], gi[1][:])
            o = self.dbg_out("cnt", [1, 8], I32)
            fw.dma("sp", o[:, :], cnti[:])
        rows = [[self.carve(k * 2056 + i * 4112, [128, 1028], BF16) for k in range(2)] for i in range(2)]
        xrow = [self.carve(16448 + i * 4096, [128, D], F32) for i in range(2)]
        wsrc = (w1, w2)
        for t16 in range(16):
            ts_ = slice(t16 * 128, (t16 + 1) * 128)
            ph = self.psb[t16 % 2].bitcast(BF16)
            for kc in range(8):
                fw.tr(ph[:, kc * 128:(kc + 1) * 128], self.hT[:, kc, ts_], C["ident_b"][:])
            for k in range(2):
                Rk = rows[t16 % 2][k]
                fw.copy("act" if k == 0 else "dve", Rk[:, 0:1024], ph[:, :])
                fw.copy("dve", Rk[:, 1024:1026].bitcast(F32), wsrc[k][:, t16:t16 + 1])
                g = nc.gpsimd
                idx = gi[k][:, t16:t16 + 1]
                src = Rk[:, 0:1026]
                fw.op("pool", (lambda src=src, idx=idx: g.indirect_dma_start(
                    out=self.HS[:, :], out_offset=bass.IndirectOffsetOnAxis(ap=idx, axis=0), in_=src, in_offset=None)),
                    reads=[src, idx], writes=[self.HS[:, :]], dma=True)
            xr = xrow[t16 % 2]
            for half in range(2):
                pb = self.psb[2 + half]
                for i in range(4):
                    dc = half * 4 + i
                    fw.tr(pb[:, i * 128:(i + 1) * 128], self.xT[:, dc, ts_], C["ident_f"][:])
                fw.copy("act" if half == 0 else "dve", xr[:, half * 512:(half + 1) * 512], pb[:, :])
            fw.dma("sp", self.XTOK[ts_, :], xr)
        E_WD, E_WGU, E_HTE, E_ACT, E_ACTT, E_G, E_YSL, E_SG, E_WS = 0, 22528, 56320, 89088, 134144, 139776, 143888, 147984, 149392
        wd = self.carve(E_WD, [128, 11, D], BF16)
        wgu = [self.carve(E_WGU + i * 5632, [128, 8, 352], BF16) for i in range(6)]
        hTe = self.carve(E_HTE, [128, 16, 1024], BF16)
        act_sl = self.carve(E_ACT, [128, 16, 1408], BF16)
        actT = [self.carve(E_ACTT + i * 2816, [128, 1408], BF16) for i in range(2)]
        G = [self.carve(E_G + i * 2056, [128, 1028], BF16) for i in range(2)]
        ysl = [self.carve(E_YSL + i * 2048, [128, D], BF16) for i in range(2)]
        sgt = [self.carve(E_SG + i * 704, [128, 352], BF16) for i in range(2)]
        ws = self.carve(E_WS, [128, 16], F32)
        y0 = self.hT.rearrange("p a b -> p (a b)").rearrange("p (c f) -> p c f", c=16)
        self._nw = 0

        def keyof(e):
            return "moe_e%d" % e

        def stage_G(e):
            key = keyof(e)
            for ce in fw.CENGS:
                def ld(key=key, e=e, ce=ce):
                    eo = fw.engobj[ce]
                    reg = eo.alloc_register("r_%s_%s" % (key, ce))
                    ins = eo.reg_load(reg, cnti[0:1, e:e + 1])
                    fw.cond_vals[(key, ce)] = eo.snap(reg)
                    return ins
                fw.op(ce, ld, reads=[cnti[0:1, e:e + 1]], writes=[])
            for c in range(16):
                Gc = G[c % 2]
                fw.dma("sp", Gc[:, 0:1026], self.HS[e * T + c * 128:e * T + (c + 1) * 128, :])
                fw.cur_cond = (key, c * 128)
                fw.copy("dve", ws[:, c:c + 1], Gc[:, 1024:1026].bitcast(F32))
                ph = self.psb[c % 2].bitcast(BF16)
                for kc in range(8):
                    fw.tr(ph[:, kc * 128:(kc + 1) * 128], Gc[:, kc * 128:(kc + 1) * 128], C["ident_b"][:])
                fw.copy("dve", hTe[:, c, :], ph[:, :])
                fw.cur_cond = None
            fw.barrier()

        def stage_U(e, half):
            key = keyof(e)
            Wg, Wu, Wd = self.inp["w_exp_gate"][0, e], self.inp["w_exp_up"][0, e], self.inp["w_exp_down"][0, e]
            tiles = {}

            def issue_cg(cg):
                col0 = half * 1408 + cg * 352
                wgt, wut = wgu[(2 * self._nw) % 6], wgu[(2 * self._nw + 1) % 6]
                self._nw += 1
                fw.dma("pool", wgt, self.wview(Wg, col0, 352))
                fw.dma("pool", wut, self.wview(Wu, col0, 352))
                tiles[cg] = (wgt, wut)
            issue_cg(0)
            issue_cg(1)
            issue_cg(2)
            fw.dma("pool", wd, Wd.rearrange("(f p) n -> p f n", p=128)[:, half * 11:(half + 1) * 11, :])
            for cg in range(4):
                wgt, wut = tiles[cg]
                for c in range(16):
                    pg, pu = self.psb[2 + 2 * (c % 2)], self.psb[3 + 2 * (c % 2)]
                    fw.cur_cond = (key, c * 128)
                    for kc in range(8):
                        fw.mm(pg[:, 0:352], hTe[:, c, kc * 128:(kc + 1) * 128], wgt[:, kc, :], start=(kc == 0), stop=(kc == 7))
                    for kc in range(8):
                        fw.mm(pu[:, 0:352], hTe[:, c, kc * 128:(kc + 1) * 128], wut[:, kc, :], start=(kc == 0), stop=(kc == 7))
                    sg = sgt[c % 2]
                    fw.act(sg, pg[:, 0:352], AF.Silu)
                    fw.stt(act_sl[:, c, cg * 352:(cg + 1) * 352], pu[:, 0:352], ws[:, c:c + 1], sg, ALU.mult, ALU.mult)
                    fw.cur_cond = None
                fw.barrier()
                if cg + 3 < 4:
                    issue_cg(cg + 3)

        def stage_D(e, half):
            key = keyof(e)
            for c in range(16):
                at = actT[c % 2]
                pa, pb2 = self.psb[0].bitcast(BF16), self.psb[1].bitcast(BF16)
                fw.cur_cond = (key, c * 128)
                for f in range(11):
                    dstp = pa[:, f * 128:(f + 1) * 128] if f < 8 else pb2[:, (f - 8) * 128:(f - 7) * 128]
                    fw.tr(dstp, act_sl[:, c, f * 128:(f + 1) * 128], C["ident_b"][:])
                fw.copy("dve", at[:, 0:1024], pa[:, :])
                fw.copy("dve", at[:, 1024:1408], pb2[:, 0:384])
                py = (self.psb[6], self.psb[7])
                for fo in range(2):
                    for f in range(11):
                        fw.mm(py[fo][:, :], at[:, f * 128:(f + 1) * 128], wd[:, f, fo * 512:(fo + 1) * 512],
                              start=(f == 0), stop=(f == 10))
                if half == 0:
                    fw.copy("dve", y0[:, c, 0:512], py[0][:, :])
                    fw.copy("dve", y0[:, c, 512:1024], py[1][:, :])
                else:
                    yb = ysl[c % 2]
                    fw.tt("dve", yb[:, 0:512], py[0][:, :], y0[:, c, 0:512], ALU.add)
                    fw.tt("dve", yb[:, 512:1024], py[1][:, :], y0[:, c, 512:1024], ALU.add)
                fw.cur_cond = None
                if half == 1:
                    fw.dma("sp", self.YS[e * T + c * 128:e * T + (c + 1) * 128, :], ysl[c % 2])
            fw.barrier()

        stage_G(0)
        for e in range(NEXP):
            stage_U(e, 0)
            stage_D(e, 0)
            stage_U(e, 1)
            if e + 1 < NEXP:
                stage_G(e + 1)
            stage_D(e, 1)
        g2bc = self.carve(0, [128, D], F32)
        dg = self.carve(4096, [128, D], F32)
        for dc in range(8):
            fw.ts("dve", dg[:, dc * 128:(dc + 1) * 128], C["ident_f"][:], self.modT[:, l, 40 + dc:41 + dc], None, op0=ALU.mult)
        for half in range(2):
            pb = self.psb[2 + half]
            fw.mm(pb[:, :], C["ones_f"][:], dg[:, half * 512:(half + 1) * 512], start=True, stop=True)
            fw.copy("act", g2bc[:, half * 512:(half + 1) * 512], pb[:, :])
        ya = [[self.carve(8192 + (2 * i + k) * 2048, [128, D], BF16) for k in range(2)] for i in range(2)]
        ysum = [self.carve(16384 + i * 4096, [128, D], F32) for i in range(2)]
        xo = [self.carve(24576 + i * 4096, [128, D], F32) for i in range(2)]
        for t16 in range(16):
            ts_ = slice(t16 * 128, (t16 + 1) * 128)
            g = nc.gpsimd
            for k in range(2):
                dst = ya[t16 % 2][k]
                idx = gi[k][:, t16:t16 + 1]
                fw.op("pool", (lambda dst=dst, idx=idx: g.indirect_dma_start(
                    out=dst, out_offset=None, in_=self.YS[:, :], in_offset=bass.IndirectOffsetOnAxis(ap=idx, axis=0))),
                    reads=[self.YS[:, :], idx], writes=[dst], dma=True)
            xb = xo[t16 % 2]
            fw.dma("sp", xb, self.XTOK[ts_, :])
            y1, y2 = ya[t16 % 2]
            ysm = ysum[t16 % 2]
            fw.tt("dve", ysm, y1, y2, ALU.add)
            fw.tt("pool", ysm, ysm, g2bc, ALU.mult)
            fw.tt("dve", xb, xb, ysm, ALU.add)
            fw.dma("sp", self.out[ts_, :], xb)


_CACHE = {}


def kernel(**inputs):
    inputs = {k: np.asarray(v) for k, v in inputs.items()}
    k = K()
    in_maps = []
    for b in range(8):
        m = host_layout(inputs, b)
        for kk, v in k.hc.items():
            m["c_" + kk] = v
        in_maps.append(m)
    res = run_bass_kernel_spmd(k.nc, in_maps, core_ids=list(range(8)))
    out = np.stack([np.asarray(res.results[b]["out"]) for b in range(8)], axis=0)
    return out.astype(np.float32)
```

```python
from concourse.bass_utils import run_bass_kernel_spmd
import numpy as np
import concourse.bass as bass
import concourse.mybir as mybir

F32 = mybir.dt.float32
BF16 = mybir.dt.bfloat16
AF = mybir.ActivationFunctionType
ALU = mybir.AluOpType
AX = mybir.AxisListType

_DTSIZE = {}


def dtsize(dt):
    if dt not in _DTSIZE:
        _DTSIZE[dt] = np.dtype(mybir.dt.np(dt)).itemsize
    return _DTSIZE[dt]


class Rec:
    __slots__ = ("eng", "fn", "deps", "dma", "sig", "idx", "gidx", "dmasem", "dmaval", "vc", "cond", "sv")


class FW:
    ENGS = ("pe", "act", "dve", "pool", "sp")
    CENGS = ("pe", "act", "dve")

    def __init__(self, nc, n_dma_sems=24):
        self.nc = nc
        self.recs = []
        self.eng_recs = {e: [] for e in self.ENGS}
        self.hist = {}
        self.engobj = {"pe": nc.tensor, "act": nc.scalar, "dve": nc.vector, "pool": nc.gpsimd, "sp": nc.sync}
        self.n_dma_sems = n_dma_sems
        self.dma_count = {e: 0 for e in self.ENGS}
        self.dma_last = {}
        self.cur_cond = None
        self.cond_vals = {}

    def region(self, ap):
        t = ap.tensor
        name = t.name
        space = str(ap.space) if hasattr(ap, "space") else ""
        esz = dtsize(ap.dtype)
        apl = list(ap.ap)
        off = ap.offset
        is_dram = "DRAM" in space.upper() or "HBM" in space.upper() or type(t).__name__.startswith("DRam")
        if is_dram:
            lo = off
            hi = off
            for st, cnt in apl:
                if cnt > 1:
                    if st >= 0:
                        hi += st * (cnt - 1)
                    else:
                        lo += st * (cnt - 1)
            return (name, 0, 1, lo * esz, (hi + 1) * esz, False)
        tsz = dtsize(t.dtype)
        pstride = 1
        for s in t.shape[1:]:
            pstride *= s
        if esz != tsz:
            pstride = pstride * tsz // esz
        p0 = off // pstride
        f0 = off % pstride
        pst, pcnt = apl[0]
        if pst == 0:
            pcnt = 1
        p1 = p0 + pcnt
        lo = f0
        hi = f0
        for st, cnt in apl[1:]:
            if cnt > 1:
                if st >= 0:
                    hi += st * (cnt - 1)
                else:
                    lo += st * (cnt - 1)
        b0, b1 = lo * esz, (hi + 1) * esz
        is_psum = type(t).__name__.startswith("PSum")
        if is_psum:
            b0 = (b0 // 2048) * 2048
            b1 = ((b1 + 2047) // 2048) * 2048
            p0, p1 = 0, 128
        return (name, p0, p1, b0, b1, is_psum)

    def op(self, eng, fn, reads=(), writes=(), dma=False):
        r = Rec()
        r.cond = self.cur_cond if eng in self.CENGS else None
        r.sv = None
        r.eng = eng
        r.fn = fn
        r.dma = dma
        r.sig = False
        r.deps = []
        r.idx = len(self.eng_recs[eng])
        r.gidx = len(self.recs)
        r.dmasem = None
        deps = set()
        for ap in reads:
            self._access(r, self.region(ap), False, deps)
        for ap in writes:
            self._access(r, self.region(ap), True, deps)
        if dma:
            slot = self.dma_count[eng] % self.n_dma_sems
            self.dma_count[eng] += 1
            prev = self.dma_last.get((eng, slot))
            if prev is not None:
                deps.add(prev)
            self.dma_last[(eng, slot)] = r
            r.dmasem = slot
        r.deps = sorted(deps, key=lambda d: d.gidx)
        self.recs.append(r)
        self.eng_recs[eng].append(r)
        return r

    def _access(self, r, reg, is_write, deps):
        name, p0, p1, b0, b1, is_psum = reg
        lst = self.hist.setdefault(name, [])
        excl = is_write or is_psum
        keep = []
        for ent in lst:
            ep0, ep1, eb0, eb1, w, rds, wtrue = ent
            if ep1 <= p0 or p1 <= ep0 or eb1 <= b0 or b1 <= eb0:
                keep.append(ent)
                continue
            inside = ep0 >= p0 and ep1 <= p1 and eb0 >= b0 and eb1 <= b1
            if w is not None and w is not r:
                if w.dma or r.dma or w.eng != r.eng:
                    deps.add(w)
                elif wtrue and (not is_write) and r.eng != "pe":
                    deps.add(w)
            if excl:
                for rd in rds:
                    if rd is r:
                        continue
                    if rd.dma or r.dma or rd.eng != r.eng:
                        deps.add(rd)
                if inside:
                    continue
            else:
                if w is None and inside and len(rds) == 1 and (not rds[0].dma) and (not r.dma) and rds[0].eng == r.eng:
                    continue
            keep.append(ent)
        if excl:
            keep.append([p0, p1, b0, b1, r, [], is_write])
        else:
            keep.append([p0, p1, b0, b1, None, [r], False])
        self.hist[name] = keep

    def barrier(self, engs=("pe", "act", "dve")):
        lasts = {e: self.eng_recs[e][-1] for e in engs if self.eng_recs[e]}
        for e in engs:
            eo = self.engobj[e]
            r = self.op(e, (lambda eo=eo: eo.drain()), reads=[], writes=[])
            extra = [lasts[o] for o in engs if o != e and o in lasts]
            r.deps = sorted(set(r.deps) | set(extra), key=lambda d: d.gidx)

    def emit(self):
        nc = self.nc
        ne = len(self.ENGS)
        eidx = {e: i for i, e in enumerate(self.ENGS)}
        grp_of = {}
        groups = []
        for ce in self.CENGS:
            prev = None
            for r in self.eng_recs[ce]:
                if r.cond is None:
                    prev = None
                    continue
                if prev is not None and prev.cond is not None and prev.cond[0] == r.cond[0] and prev.cond[1] <= r.cond[1] \
                        and prev.idx == r.idx - 1:
                    groups[-1].append(r)
                else:
                    groups.append([r])
                grp_of[r.gidx] = len(groups) - 1
                prev = r
        clock = {e: [-1] * ne for e in self.ENGS}
        dma_known = {e: set() for e in self.ENGS}
        final_deps = []
        cur_grp = {e: None for e in self.CENGS}
        saved = {e: None for e in self.CENGS}
        for r in self.recs:
            if r.eng in self.CENGS:
                g = grp_of.get(r.gidx)
                if g != cur_grp[r.eng]:
                    if cur_grp[r.eng] is not None:
                        clock[r.eng] = saved[r.eng][0]
                        dma_known[r.eng] = saved[r.eng][1]
                    if g is not None:
                        saved[r.eng] = (list(clock[r.eng]), set(dma_known[r.eng]))
                    cur_grp[r.eng] = g
            ck = clock[r.eng]
            need = []
            for d in r.deps:
                if d.dma:
                    if d.gidx in dma_known[r.eng]:
                        continue
                    need.append(d)
                else:
                    if ck[eidx[d.eng]] >= d.idx:
                        continue
                    need.append(d)
            best = {}
            nd = []
            for d in need:
                if d.dma:
                    nd.append(d)
                else:
                    if d.eng not in best or best[d.eng].idx < d.idx:
                        best[d.eng] = d
            nd.extend(best.values())
            for d in nd:
                d.sig = True
                if d.dma:
                    dma_known[r.eng].add(d.gidx)
                    dvc = d.vc
                else:
                    dvc = list(d.vc)
                    dvc[eidx[d.eng]] = max(dvc[eidx[d.eng]], d.idx)
                for i in range(ne):
                    if dvc[i] > ck[i]:
                        ck[i] = dvc[i]
            if r.eng in self.CENGS and cur_grp[r.eng] is not None:
                r.vc = list(saved[r.eng][0])
            else:
                r.vc = list(ck)
            final_deps.append(nd)
        cnt = {e: 0 for e in self.ENGS}
        dcnt = {}
        for r in self.recs:
            if r.dma:
                key = (r.eng, r.dmasem)
                dcnt[key] = dcnt.get(key, 0) + 16
                r.dmaval = dcnt[key]
            elif r.sig:
                cnt[r.eng] += 1
                r.sv = cnt[r.eng]
        sems = {}
        for e in self.ENGS:
            sems[e] = nc.alloc_semaphore("sem_" + e)
        dsems = {}
        for e in self.ENGS:
            if self.dma_count[e] > 0:
                dsems[e] = [nc.alloc_semaphore("dsem_%s_%d" % (e, i)) for i in range(self.n_dma_sems)]
        self.nwait = 0

        def emit_one(r, nd):
            eo = self.engobj[r.eng]
            for d in nd:
                if d.dma:
                    eo.wait_ge(dsems[d.eng][d.dmasem], d.dmaval)
                else:
                    eo.wait_ge(sems[d.eng], d.sv)
                self.nwait += 1
            ins = r.fn()
            if r.dma:
                ins.then_inc(dsems[r.eng][r.dmasem], 16)
            elif r.sig:
                ins.then_inc(sems[r.eng], 1)

        def emit_group(recs_):
            eng = recs_[0].eng
            eo = self.engobj[eng]
            key = recs_[0].cond[0]
            val = self.cond_vals[(key, eng)]
            levels = []
            for r in recs_:
                if not levels or levels[-1][0] != r.cond[1]:
                    levels.append((r.cond[1], []))
                levels[-1][1].append(r)

            def rec_level(li):
                if li == len(levels):
                    return
                c, rs = levels[li]
                nsig = sum(1 for lv in levels[li:] for r in lv[1] if r.sig)
                with eo.If(val > c):
                    for r in rs:
                        emit_one(r, final_deps[r.gidx])
                    rec_level(li + 1)
                with eo.Else():
                    if nsig > 0:
                        eo.drain()
                        eo.sem_inc(sems[eng], nsig)
            rec_level(0)

        done = set()
        for r, nd in zip(self.recs, final_deps):
            if r.gidx in done:
                continue
            g = grp_of.get(r.gidx)
            if g is not None:
                emit_group(groups[g])
                for x in groups[g]:
                    done.add(x.gidx)
            else:
                emit_one(r, nd)
        for (e, slot), r in self.dma_last.items():
            self.engobj[e].wait_ge(dsems[e][slot], r.dmaval)
        self.stats = dict(n=len(self.recs), waits=self.nwait, per_eng={e: len(v) for e, v in self.eng_recs.items()},
                          ngroups=len(groups))
        return self.stats

    def dma(self, eng, out, in_, **kw):
        o = self.engobj[eng]
        return self.op(eng, lambda: o.dma_start(out=out, in_=in_, **kw), reads=[in_], writes=[out], dma=True)

    def mm(self, out, lhsT, rhs, start=True, stop=True, **kw):
        t = self.nc.tensor
        return self.op("pe", lambda: t.matmul(out, lhsT, rhs, start=start, stop=stop, **kw), reads=[lhsT, rhs], writes=[out])

    def tr(self, out, in_, ident):
        t = self.nc.tensor
        return self.op("pe", lambda: t.transpose(out, in_, ident), reads=[in_, ident], writes=[out])

    def act(self, out, in_, func, bias=None, scale=None, accum_out=None):
        s = self.nc.scalar
        kw = {}
        rd = [in_]
        if bias is not None:
            kw["bias"] = bias
            if not isinstance(bias, (int, float)):
                rd.append(bias)
        if scale is not None:
            kw["scale"] = scale
            if not isinstance(scale, (int, float)):
                rd.append(scale)
        wr = [out]
        if accum_out is not None:
            kw["accum_out"] = accum_out
            wr.append(accum_out)
        return self.op("act", lambda: s.activation(out=out, in_=in_, func=func, **kw), reads=rd, writes=wr)

    def _veng(self, eng):
        return self.engobj[eng]

    def tt(self, eng, out, in0, in1, op):
        e = self._veng(eng)
        return self.op(eng, lambda: e.tensor_tensor(out=out, in0=in0, in1=in1, op=op), reads=[in0, in1], writes=[out])

    def ts(self, eng, out, in0, s1, s2=None, op0=ALU.mult, op1=None):
        e = self._veng(eng)
        rd = [in0]
        for s in (s1, s2):
            if s is not None and not isinstance(s, (int, float)):
                rd.append(s)
        kw = {}
        if op1 is not None:
            kw["op1"] = op1
        return self.op(eng, lambda: e.tensor_scalar(out=out, in0=in0, scalar1=s1, scalar2=s2, op0=op0, **kw), reads=rd, writes=[out])

    def stt(self, out, in0, scalar, in1, op0, op1, eng="dve"):
        e = self._veng(eng)
        rd = [in0, in1]
        if not isinstance(scalar, (int, float)):
            rd.append(scalar)
        return self.op(eng, lambda: e.scalar_tensor_tensor(out=out, in0=in0, scalar=scalar, in1=in1, op0=op0, op1=op1), reads=rd, writes=[out])

    def copy(self, eng, out, in_):
        if eng == "act":
            s = self.nc.scalar
            return self.op("act", lambda: s.copy(out=out, in_=in_), reads=[in_], writes=[out])
        e = self._veng(eng)
        return self.op(eng, lambda: e.tensor_copy(out=out, in_=in_), reads=[in_], writes=[out])

    def memset(self, eng, out, val):
        e = self._veng(eng)
        return self.op(eng, lambda: e.memset(out, val), reads=[], writes=[out])


def _fw_generic(self, eng, name, reads, writes, *args, **kw):
    e = self.engobj[eng]
    f = getattr(e, name)
    return self.op(eng, lambda: f(*args, **kw), reads=reads, writes=writes)


FW.gen = _fw_generic


import numpy as np
import ml_dtypes

D = 1024
T = 2048
NH = 24
DFF = 2816
NFF = 22
NEXP = 8
IN_COLS = 7686
QOFF, KOFF, VOFF, FOFF, GOFF = 0, 1536, 3072, 4608, 4614
EPS = 1e-6
SLOPES = (2.0 ** (-8.0 * np.arange(1, 19) / 18)).astype(np.float32)
DIL = ((128, 1), (512, 4), (2048, 16))
DIL_SO = (0, 4, 14)
MOBA_SO = 8
NEGM = -30000.0


def split3(v):
    v = np.asarray(v, np.float32)
    hi = v.astype(ml_dtypes.bfloat16)
    r1 = v - hi.astype(np.float32)
    mid = r1.astype(ml_dtypes.bfloat16)
    r2 = r1 - mid.astype(np.float32)
    lo = r2.astype(ml_dtypes.bfloat16)
    return hi, mid, lo


def host_consts():
    c = {}
    c["ident_f"] = np.eye(128, dtype=np.float32)
    c["ident_b"] = np.eye(128, dtype=np.float32).astype(ml_dtypes.bfloat16)
    ob = np.zeros((128, 128), np.float32)
    ob[0:64, 0:64] = 1.0
    c["onesblk"] = ob.astype(ml_dtypes.bfloat16)
    c["ones_b"] = np.ones((128, 128), np.float32).astype(ml_dtypes.bfloat16)
    c["ones_f"] = np.ones((128, 128), np.float32)
    k = np.arange(128)[:, None]
    q = np.arange(128)[None, :]
    c["tri"] = (q >= k).astype(np.float32).astype(ml_dtypes.bfloat16)
    db = np.zeros((128, 12, 256), np.float32)
    for g, (w, d) in enumerate(DIL):
        for j in range(4):
            sl = SLOPES[DIL_SO[g] + j]
            left = np.where(q >= k, -sl * d * (q - k), NEGM)
            right = np.where(k >= q, -sl * d * (128 + q - k), NEGM)
            db[:, g * 4 + j, 0:128] = left
            db[:, g * 4 + j, 128:256] = right
    c["dilbias"] = db.reshape(128, 12 * 256)
    t = np.arange(T, dtype=np.float32)
    aq = np.zeros((6, 6, T), ml_dtypes.bfloat16)
    ak = np.zeros((6, 6, T), ml_dtypes.bfloat16)
    for h in range(6):
        sl = SLOPES[MOBA_SO + h]
        qh = split3(-8.0 * sl * t)
        kh = split3(8.0 * sl * t)
        for i in range(3):
            aq[h, i] = qh[i]
            aq[h, 3 + i] = 1.0
            ak[h, i] = 1.0
            ak[h, 3 + i] = kh[i]
    c["moba_aq"] = aq
    c["moba_ak"] = ak
    bi = np.zeros((8, T), np.float32)
    for b in range(8):
        bi[b, b * 256:(b + 1) * 256] = 1.0
    c["blkind"] = bi.astype(ml_dtypes.bfloat16)
    pos = (np.arange(16)[None, :, None] * 128 + np.arange(128)[:, None, None])
    own = pos // 256
    B = np.arange(8)[None, None, :]
    c["mg_sb1"] = np.where(B < own, 0.0, -1e30).astype(np.float32).reshape(128, 128)
    c["mg_v1"] = (B < own).astype(np.float32).reshape(128, 128)
    c["mg_e"] = (B == own).astype(np.float32).reshape(128, 128)
    c["fox_ones"] = np.ones((3, T), np.float32).astype(ml_dtypes.bfloat16)
    c["ustrict"] = np.triu(np.ones((128, 128), np.float32), 1).astype(ml_dtypes.bfloat16)
    c["ebase"] = np.tile(np.arange(8, dtype=np.float32) * 2048.0, (128, 16))
    return c


CONST_DT = dict(ident_f=F32, ident_b=BF16, onesblk=BF16, ones_b=BF16, ones_f=F32, tri=BF16, dilbias=F32,
                moba_aq=BF16, moba_ak=BF16, blkind=BF16, mg_sb1=F32, mg_v1=F32, mg_e=F32, fox_ones=BF16, ustrict=BF16, ebase=F32)


def host_layout(inp, b):
    m = {}
    m["x"] = np.ascontiguousarray(inp["x"][b])
    m["cT"] = np.ascontiguousarray(inp["c"][b].reshape(8, 128).T)
    m["b_adaT"] = np.ascontiguousarray(inp["b_ada"].reshape(2, 48, 128).transpose(2, 0, 1))
    m["nmixT"] = np.ascontiguousarray(inp["norm_mix"].reshape(2, 8, 128).transpose(2, 0, 1))
    m["nffnT"] = np.ascontiguousarray(inp["norm_ffn"].reshape(2, 8, 128).transpose(2, 0, 1))
    m["qgT"] = np.ascontiguousarray(inp["q_gain"].transpose(2, 0, 1))
    m["kgT"] = np.ascontiguousarray(inp["k_gain"].transpose(2, 0, 1))
    m["bfg"] = np.ascontiguousarray(inp["b_fgate"].T)
    m["b_router"] = np.ascontiguousarray(inp["b_router"])
    for k in ("w_ada", "w_in", "w_br_fox", "w_br_moba", "w_br_dil", "w_out", "w_ffn_gate", "w_ffn_up",
              "w_ffn_down", "w_router", "w_exp_gate", "w_exp_up", "w_exp_down"):
        m[k] = inp[k]
    return m


IN_SHAPES = dict(
    x=[T, D], cT=[128, 8], b_adaT=[128, 2, 48], nmixT=[128, 2, 8], nffnT=[128, 2, 8], qgT=[64, 2, 24], kgT=[64, 2, 24],
    bfg=[6, 2], b_router=[1, 8], w_ada=[2, D, 6 * D], w_in=[2, D, IN_COLS], w_br_fox=[2, 384, D], w_br_moba=[2, 384, D],
    w_br_dil=[2, 256, D], w_out=[2, D, D], w_ffn_gate=[1, D, DFF], w_ffn_up=[1, D, DFF], w_ffn_down=[1, DFF, D],
    w_router=[1, D, 8], w_exp_gate=[1, 8, D, DFF], w_exp_up=[1, 8, D, DFF], w_exp_down=[1, 8, DFF, D])


def prod(xs):
    r = 1
    for x in xs:
        r *= x
    return r


A_OT = 0
A_Y = 32768
A_VA = 32768
A_VB = 32768 + 12544
A_QK = 57856
A_X = 65536
A_WQK = 74240
A_WV = 82432
A_ACCN = 88576
A_ODD = 96768
A_PT = 100864
A_Q2 = 103936
A_RQ = 105984
A_RD = 110080
A_BC = 112128
A_MG = 114176
A_KM = 118272
A_FST = 65536
A_Z = 131072
A_DILM = 131072
A_Z2 = 137216
A_END = 151552
A_ACT = 0
A_WGU = 45056
A_WD = 53248
A_SG = 58880
A_CBC = A_Z2
A_RT = A_Z2 + 4096


class K:
    def __init__(self, debug=None, nlayers=2, stop_after=None, heads=None, sparse=True):
        self.debug = debug or []
        self.nlayers = nlayers
        self.stop_after = stop_after
        self.heads = heads
        nc = bass.Bass("TRN2", target_bir_lowering=False)
        self.nc = nc
        self.fw = FW(nc)
        self.inp = {}
        for k, shp in IN_SHAPES.items():
            self.inp[k] = nc.dram_tensor(k, shp, F32, kind="ExternalInput").ap()
        self.cst = {}
        hc = host_consts()
        self.hc = hc
        for k, v in hc.items():
            self.cst[k] = nc.dram_tensor("c_" + k, list(v.shape), CONST_DT[k], kind="ExternalInput").ap()
        self.out = nc.dram_tensor("out", [T, D], F32, kind="ExternalOutput").ap()
        self.xs = nc.dram_tensor("xs_scr", [128, 8 * T], F32, kind="Internal").ap()
        self.fsc = nc.dram_tensor("f_scr", [2, 6, 3 * T], BF16, kind="Internal").ap()
        self.HS = nc.dram_tensor("hs_scr", [NEXP * T, 1026], BF16, kind="Internal").ap()
        self.YS = nc.dram_tensor("ys_scr", [NEXP * T, D], BF16, kind="Internal").ap()
        self.XTOK = nc.dram_tensor("xtok_scr", [T, D], F32, kind="Internal").ap()
        self.sparse = sparse
        self.dbg = {}
        self._rr = 0
        self._cnt = 0
        self.build()

    def sb(self, name, shape, dt):
        return self.nc.alloc_sbuf_tensor("s_" + name, shape, dt)

    def carve(self, off, shape, dt):
        n = prod(shape[1:])
        nb = n * dtsize(dt)
        assert off % 4 == 0 and off + nb <= A_END, (off, nb)
        v = self.AR[0:shape[0], off // 2:(off + nb) // 2]
        if dt != BF16:
            v = v.bitcast(dt)
        if len(shape) == 3:
            v = v.rearrange("p (a b) -> p a b", a=shape[1])
        elif len(shape) == 4:
            v = v.rearrange("p (a b c) -> p a b c", a=shape[1], b=shape[2])
        return v

    def dbg_out(self, name, shape, dt=F32):
        t = self.nc.dram_tensor("dbg_" + name, shape, dt, kind="ExternalOutput").ap()
        self.dbg[name] = t
        return t

    def evac_eng(self):
        self._rr += 1
        return "act" if self._rr % 2 else "dve"

    def wview(self, w2d, c0, ncols):
        return w2d.rearrange("(kc p) n -> p kc n", p=128)[:, :, c0:c0 + ncols]

    def build(self):
        nc, fw = self.nc, self.fw
        C = {}
        for k in ("ident_f", "ident_b", "onesblk", "ones_b", "ones_f", "tri", "mg_sb1", "mg_v1", "mg_e", "ustrict", "ebase"):
            v = self.hc[k]
            C[k] = self.sb("k_" + k, list(v.shape), CONST_DT[k])
            fw.dma("sp", C[k][:], self.cst[k][:, :])
        self.C = C
        self.cT = self.sb("cT", [128, 8], F32)
        fw.dma("sp", self.cT[:], self.inp["cT"][:, :])
        self.b_adaT = self.sb("b_adaT", [128, 2, 48], F32)
        fw.dma("sp", self.b_adaT[:], self.inp["b_adaT"][:, :, :])
        self.nmixT = self.sb("nmixT", [128, 2, 8], F32)
        fw.dma("sp", self.nmixT[:], self.inp["nmixT"][:, :, :])
        self.nffnT = self.sb("nffnT", [128, 2, 8], F32)
        fw.dma("sp", self.nffnT[:], self.inp["nffnT"][:, :, :])
        self.qg = self.sb("qg", [64, 2, 24], F32)
        fw.dma("sp", self.qg[:], self.inp["qgT"][:, :, :])
        self.kg = self.sb("kg", [64, 2, 24], F32)
        fw.dma("sp", self.kg[:], self.inp["kgT"][:, :, :])
        self.bfg = self.sb("bfg", [6, 2], F32)
        fw.dma("sp", self.bfg[:], self.inp["bfg"][:, :])
        self.negb = self.sb("negb", [6, 2], F32)
        fw.ts("dve", self.negb[:], self.bfg[:], -1.0, None, op0=ALU.mult)
        self.AR = self.sb("arena", [128, A_END // 2], BF16)
        self.hT = self.sb("hT", [128, 8, T], BF16)
        self.xT = self.carve(A_X, [128, 8, T], F32)
        self.psb = [nc.alloc_psum_tensor("psb%d" % i, [128, 512], F32) for i in range(8)]
        self.cond = self.sb("cond", [128, 8], BF16)
        self.modT = self.sb("modT", [128, 2, 48], F32)
        self.a1 = self.sb("a1", [128, 2, 8], F32)
        self.a2 = self.sb("a2", [128, 2, 8], F32)
        self.sqb = [self.sb("sqb%d" % i, [128, 512], BF16) for i in range(3)]
        self.rstd = [self.sb("rstd%d" % i, [128, 512], F32) for i in range(2)]
        self.ntmp = [self.sb("ntmp%d" % i, [128, 512], F32) for i in range(3)]
        self.OT = [self.carve(A_OT + i * 4096, [128, T], BF16) for i in range(8)]
        self.VA = self.carve(A_VA, [128, 16, 6, 65], BF16)
        self.VB = self.carve(A_VB, [128, 16, 6, 65], BF16)
        self.Qb = [self.carve(A_QK + (2 * s) * 4096, [128, T], BF16) for s in range(2)]
        self.Kb = [self.carve(A_QK + (2 * s + 1) * 4096, [128, T], BF16) for s in range(2)]
        self.wqk = [self.carve(A_WQK + s * 4096, [128, 8, 256], BF16) for s in range(2)]
        self.wv = self.carve(A_WV, [128, 8, 384], BF16)
        self.accN = self.carve(A_ACCN, [128, T], F32)
        self.oddt = self.carve(A_ODD, [128, T], BF16)
        self.ptb = [self.carve(A_PT + i * 1024, [128, 512], BF16) for i in range(3)]
        self.q2b = [self.carve(A_Q2 + i * 1024, [128, 512], BF16) for i in range(2)]
        self.rqb = [self.carve(A_RQ + i * 2048, [128, 512], F32) for i in range(2)]
        self.rd = self.carve(A_RD, [128, 512], F32)
        self.bcs = self.carve(A_BC, [128, 512], F32)
        self.dilM = self.carve(A_DILM, [128, 12, 256], BF16)
        self.rbt4 = [self.carve(119296 + i * 1024, [128, 512], BF16) for i in range(4)]
        self.rbt = self.rbt4[0:2]
        self.rlt = [self.carve(119296 + 4096 + i * 2048, [128, 512], F32) for i in range(2)]

        self._nada = 0
        self.stage_load_x()
        self.stage_adaln([0])
        for l in range(self.nlayers):
            self.stage_rmsnorm(l, 0)
            if l == 0 and self.nlayers > 1:
                self.stage_adaln([1])
            if self.stop_after == "norm1":
                break
            fw.dma("sp", self.xs[:, :], self.xT.rearrange("p a b -> p (a b)"))
            if l == 0:
                self.stage_dilmask()
            self.stage_attention(l)
            if self.stop_after == "attn":
                break
            fw.dma("sp", self.xT.rearrange("p a b -> p (a b)"), self.xs[:, :])
            self.stage_outproj(l)
            if self.stop_after == "outproj":
                break
            self.stage_rmsnorm(l, 1, rt_ps=(self.psb[7] if l % 2 == 1 else None))
            if l % 2 == 0:
                self.ffn_expert(l, self.inp["w_ffn_gate"][0], self.inp["w_ffn_up"][0], self.inp["w_ffn_down"][0], None)
                if self.sparse and self.stop_after is None and l == 0:
                    zt = self.carve(A_VB, [128, 4104], BF16)
                    fw.memset("pool", zt, 0.0)
                    hsv = self.HS.rearrange("(p r) c -> p (r c)", p=128)
                    for i in range(32):
                        fw.dma("sp", hsv[:, i * 4104:(i + 1) * 4104], zt)
            elif self.sparse and l == self.nlayers - 1 and self.stop_after is None:
                self.stage_moe_sparse(l)
                self.final_done = True
            else:
                self.stage_moe(l)
            if self.stop_after == "ffn":
                break
        if self.stop_after is None and not getattr(self, "final_done", False):
            self.stage_output()
        for nm in self.debug:
            if nm == "hT":
                o = self.dbg_out("hT", [128, 8, T], BF16)
                fw.dma("sp", o[:, :, :], self.hT[:])
            elif nm == "xT":
                o = self.dbg_out("xT", [128, 8, T], F32)
                fw.dma("sp", o[:, :, :], self.xT)
            elif nm == "OT":
                o = self.dbg_out("OT", [8, 128, T], BF16)
                for i in range(8):
                    fw.dma("sp", o[i], self.OT[i])
            elif nm == "modT":
                o = self.dbg_out("modT", [128, 2, 48], F32)
                fw.dma("sp", o[:, :, :], self.modT[:])
        self.stats = fw.emit()

    def stage_load_x(self):
        fw, C = self.fw, self.C
        xin = [self.carve(i * 4096, [128, D], F32) for i in range(4)]
        for g in range(4):
            for i in range(4):
                tt = g * 4 + i
                fw.dma("sp", xin[i], self.inp["x"][tt * 128:(tt + 1) * 128, :])
            for dc in range(8):
                pb = self.psb[dc % 4]
                for i in range(4):
                    fw.tr(pb[:, i * 128:(i + 1) * 128], xin[i][:, dc * 128:(dc + 1) * 128], C["ident_f"][:])
                fw.copy(self.evac_eng(), self.xT[:, dc, g * 512:(g + 1) * 512], pb[:, :])

    def stage_adaln(self, layers):
        fw, C = self.fw, self.C
        if 0 in layers:
            fw.act(self.cond[:], self.cT[:], AF.Silu)
        wbuf = [self.carve(16384 + i * 8192, [128, 8, 512], BF16) for i in range(4)]
        for l in layers:
            pm = self.psb[4 + l]
            wv = self.inp["w_ada"][l].rearrange("(kc p) n -> p kc n", p=128)
            for half in range(12):
                wb = wbuf[self._nada % 4]
                self._nada += 1
                fw.dma("pool", wb, wv[:, :, half * 512:(half + 1) * 512])
                for c4 in range(4):
                    j = half * 4 + c4
                    for kc in range(8):
                        fw.mm(pm[:, j:j + 1], wb[:, kc, c4 * 128:(c4 + 1) * 128], self.cond[:, kc:kc + 1],
                              start=(kc == 0), stop=(kc == 7))
            fw.tt("dve", self.modT[:, l, :], pm[:, 0:48], self.b_adaT[:, l, :], ALU.add)
            fw.stt(self.a1[:, l, :], self.modT[:, l, 8:16], 1.0, self.nmixT[:, l, :], ALU.add, ALU.mult)
            fw.stt(self.a2[:, l, :], self.modT[:, l, 32:40], 1.0, self.nffnT[:, l, :], ALU.add, ALU.mult)

    def stage_rmsnorm(self, l, which, rt_ps=None):
        fw, C = self.fw, self.C
        a = self.a1 if which == 0 else self.a2
        shoff = 0 if which == 0 else 24
        for tt in range(4):
            cs = slice(tt * 512, (tt + 1) * 512)
            pss = self.psb[tt % 2]
            for dc in range(8):
                sq = self.sqb[dc % 3]
                fw.act(sq[:], self.xT[:, dc, cs], AF.Square)
                fw.mm(pss[:, :], C["ones_b"][:], sq[:], start=(dc == 0), stop=(dc == 7))
            rs = self.rstd[tt % 2]
            fw.act(rs[:], pss[:, :], AF.Ln, bias=EPS, scale=1.0 / D)
            fw.act(rs[:], rs[:], AF.Exp, scale=-0.5)
            if rt_ps is not None:
                for i in range(4):
                    fw.mm(rt_ps[:, tt * 4 + i:tt * 4 + i + 1], rs[0:1, i * 128:(i + 1) * 128], C["ones_f"][0:1, 0:1])
            for dc in range(8):
                nt = self.ntmp[dc % 3]
                fw.stt(nt[:], self.xT[:, dc, cs], a[:, l, dc:dc + 1], rs[:], ALU.mult, ALU.mult)
                fw.act(self.hT[:, dc, cs], nt[:], AF.Identity, bias=self.modT[:, l, shoff + dc:shoff + dc + 1])

    def stage_dilmask(self):
        fw = self.fw
        tmp = self.carve(A_X, [128, 3072], F32)
        fw.dma("sp", tmp, self.cst["dilbias"][:, :])
        fw.act(self.dilM.rearrange("p a b -> p (a b)"), tmp, AF.Exp)

    def fox_prep(self, l):
        fw = self.fw
        wf = self.carve(A_Z2, [128, 8, 6], BF16)
        fw.dma("pool", wf, self.wview(self.inp["w_in"][l], FOFF, 6))
        fl = self.carve(A_FST, [6, T], F32)
        G = self.carve(A_FST + 8192, [6, T], F32)
        r1 = self.carve(A_FST + 16384, [6, T], F32)
        kp = self.carve(A_FST + 24576, [6, 3, T], BF16)
        qp = self.carve(A_FST + 36864, [6, 3, T], BF16)
        for tt in range(4):
            cs = slice(tt * 512, (tt + 1) * 512)
            ps = self.psb[tt % 2]
            for kc in range(8):
                fw.mm(ps[0:6, :], wf[:, kc, :], self.hT[:, kc, cs], start=(kc == 0), stop=(kc == 7))
            fw.act(fl[:, cs], ps[0:6, :], AF.Exp, scale=-1.0, bias=self.negb[0:6, l:l + 1])
        fw.act(fl, fl, AF.Ln, bias=1.0)
        fw.memset("dve", r1, 1.0)
        fw.gen("dve", "tensor_tensor_scan", [fl, r1], [G], out=G, data0=r1, data1=fl, initial=0.0,
               op0=ALU.mult, op1=ALU.add)
        fw.ts("dve", kp[:, 0, :], G, 8.0, None, op0=ALU.mult)
        fw.stt(r1, G, 8.0, kp[:, 0, :], ALU.mult, ALU.subtract)
        fw.copy("dve", kp[:, 1, :], r1)
        fw.tt("dve", r1, r1, kp[:, 1, :], ALU.subtract)
        fw.copy("dve", kp[:, 2, :], r1)
        kpf = kp.rearrange("p a b -> p (a b)")
        qpf = qp.rearrange("p a b -> p (a b)")
        fw.ts("dve", qpf, kpf, -1.0, None, op0=ALU.mult)
        fw.dma("sp", self.fsc[0], qpf)
        fw.dma("sp", self.fsc[1], kpf)

    def proj_qk(self, l, h, s, d):
        fw, C = self.fw, self.C
        w = self.wqk[s]
        Wl = self.inp["w_in"][l]
        fw.dma("pool", w[:, :, 0:128], self.wview(Wl, QOFF + h * 64, 128))
        fw.dma("pool", w[:, :, 128:256], self.wview(Wl, KOFF + h * 64, 128))
        fw.memset("pool", self.Qb[s][64:128, :], 0.0)
        fw.memset("pool", self.Kb[s][64:128, :], 0.0)
        for which, dst, gain in ((0, self.Qb[s], self.qg), (1, self.Kb[s], self.kg)):
            for tt in range(4):
                cs = slice(tt * 512, (tt + 1) * 512)
                ps = self.psb[tt % 2]
                for kc in range(8):
                    fw.mm(ps[:, :], w[:, kc, which * 128:(which + 1) * 128], self.hT[:, kc, cs],
                          start=(kc == 0), stop=(kc == 7))
                q2 = self.q2b[tt % 2]
                fw.act(q2, ps[:, :], AF.Square)
                p2 = self.psb[2]
                fw.mm(p2[:, :], C["onesblk"][:], q2, start=True, stop=True)
                rq = self.rqb[tt % 2]
                fw.act(rq[0:64, :], p2[0:64, :], AF.Ln, bias=EPS, scale=1.0 / 64)
                fw.act(rq[0:64, :], rq[0:64, :], AF.Exp, scale=-0.5)
                if d == 1:
                    o, i0, i1 = dst[0:64, cs], ps[0:64, :], rq[0:64, :]
                else:
                    n0, n1 = tt * 512 // d, (tt + 1) * 512 // d
                    o = dst[0:64, :].rearrange("p (r n) -> p n r", r=d)[:, n0:n1, :]
                    i0 = ps[0:64, :].rearrange("p (n r) -> p n r", r=d)
                    i1 = rq[0:64, :].rearrange("p (n r) -> p n r", r=d)
                fw.stt(o, i0, gain[:, l, h:h + 1], i1, ALU.mult, ALU.mult)

    def load_wv(self, l, grp):
        self.fw.dma("pool", self.wv, self.wview(self.inp["w_in"][l], VOFF + grp * 384, 384))

    def proj_v(self, nh, d, Vbuf, slot0, wcol0):
        fw = self.fw
        L = T // d
        nblk = L // 128
        for pb in range(16):
            r, nb = pb // nblk, pb % nblk
            ps = self.psb[pb % 2]
            if d == 1:
                tok = slice(pb * 128, (pb + 1) * 128)
            else:
                st0 = nb * 128 * d + r
                tok = slice(st0, st0 + 127 * d + 1, d)
            for kc in range(8):
                fw.mm(ps[:, 0:nh * 64], self.hT[:, kc, tok], self.wv[:, kc, wcol0:wcol0 + nh * 64],
                      start=(kc == 0), stop=(kc == 7))
            fw.copy(self.evac_eng(), Vbuf[:, pb, slot0:slot0 + nh, 0:64],
                    ps[:, 0:nh * 64].rearrange("p (h e) -> p h e", h=nh))

    def recip_row(self, den, k, eng):
        fw = self.fw
        rb = self.rbt[k % 2] if eng == "dve" else self.rbt4[k % 4]
        if eng == "dve":
            nc = self.nc
            o_ = rb[64:65, :]

            def f(o_=o_, den=den):
                with nc.allow_low_precision("bf16 reciprocal row feeds a bf16 broadcast matmul"):
                    return nc.vector.reciprocal(out=o_, in_=den)
            fw.op("dve", f, reads=[den], writes=[o_])
        else:
            lt = self.rlt[k % 2]
            fw.act(lt[64:65, :], den, AF.Ln)
            fw.act(rb[64:65, :], lt[64:65, :], AF.Exp, scale=-1.0)
        return rb

    def bcast_mul(self, src, rb, dest):
        fw, C = self.fw, self.C
        bp = self.psb[2]
        fw.mm(bp[:, :], C["ones_b"][64:65, :], rb[64:65, :], start=True, stop=True)
        fw.tt("dve", dest, src, bp[0:64, :], ALU.mult)

    def attn_causal(self, s, Vbuf, vslot, dest, hook=None, pending=None):
        fw, C = self.fw, self.C
        Qh, Kh = self.Qb[s], self.Kb[s]
        otS = (self.rd, self.bcs)
        for j in range(4):
            ot = self.psb[6 + j % 2]
            nkb = 4 * j + 4

            def S(kb):
                c0 = max(0, kb - 4 * j) * 128
                st = self.psb[3 + kb % 3]
                fw.mm(st[:, c0:512], Kh[:, kb * 128:(kb + 1) * 128], Qh[:, j * 512 + c0:(j + 1) * 512],
                      start=True, stop=True)
            S(0)
            if nkb > 1:
                S(1)
            for kb in range(nkb):
                if kb + 2 < nkb:
                    S(kb + 2)
                c0 = max(0, kb - 4 * j) * 128
                st = self.psb[3 + kb % 3]
                pt = self.ptb[kb % 3]
                fw.act(pt[:, c0:512], st[:, c0:512], AF.Exp, scale=0.125)
                if kb >= 4 * j:
                    fw.tt("pool", pt[:, c0:c0 + 128], pt[:, c0:c0 + 128], C["tri"][:], ALU.mult)
                fw.mm(ot[0:65, c0:512], Vbuf[:, kb, vslot, 0:65], pt[:, c0:512], start=(kb == 0), stop=(kb == nkb - 1))
                if kb == min(5, nkb - 1) and pending is not None:
                    pending()
                    pending = None
                if hook is not None and j == 2 and kb == 5:
                    hook()
                    hook = None
            if pending is not None:
                pending()
                pending = None

            o = otS[j % 2]
            fw.copy("act", o[0:65, :], ot[0:65, :])
            rb = self.recip_row(o[64:65, :], j, "dve")

            def fin(j=j, o=o, rb=rb):
                self.bcast_mul(o[0:64, :], rb, dest[:, j * 512:(j + 1) * 512])
            pending = fin
        return pending

    def fox_aug(self, h, s):
        fw = self.fw
        Qh, Kh = self.Qb[s], self.Kb[s]
        fw.dma("sp", Qh[64:67, :], self.fsc[0, h].rearrange("(a t) -> a t", a=3))
        fw.dma("sp", Qh[67:70, :], self.cst["fox_ones"][:, :])
        fw.dma("sp", Kh[64:67, :], self.cst["fox_ones"][:, :])
        fw.dma("sp", Kh[67:70, :], self.fsc[1, h].rearrange("(a t) -> a t", a=3))

    def moba_prep(self, h6, s):
        fw, C = self.fw, self.C
        Qh, Kh = self.Qb[s], self.Kb[s]
        km = self.carve(A_KM, [128, 8], F32)
        kmb = self.carve(A_KM + 64, [128, 8], BF16)
        fw.gen("dve", "tensor_reduce", [Kh[0:64, :]], [km[0:64, :]], out=km[0:64, :],
               in_=Kh[0:64, :].rearrange("p (b n) -> p b n", b=8), axis=AX.X, op=ALU.add)
        fw.ts("dve", kmb[0:64, :], km[0:64, :], 1.0 / 256, None, op0=ALU.mult)
        gp = self.psb[2]
        for qb in range(16):
            fw.mm(gp[:, qb * 8:(qb + 1) * 8], Qh[0:64, qb * 128:(qb + 1) * 128], kmb[0:64, :], start=True, stop=True)
        gm = self.carve(A_MG, [128, 128], F32)
        m8 = self.carve(A_MG + 512, [128, 128], F32)
        sel = self.carve(A_MG + 1024, [128, 128], F32)
        mbb = self.carve(A_MG + 1536, [128, 128], BF16)
        fw.tt("dve", gm, gp[:, 0:128], C["mg_sb1"][:], ALU.add)
        for qb in range(16):
            sl = slice(qb * 8, (qb + 1) * 8)
            fw.gen("dve", "max", [gm[:, sl]], [m8[:, sl]], out=m8[:, sl], in_=gm[:, sl])
        gm3 = gm.rearrange("p (a b) -> p a b", b=8)
        m83 = m8.rearrange("p (a b) -> p a b", b=8)
        sel3 = sel.rearrange("p (a b) -> p a b", b=8)
        fw.tt("dve", sel3, gm3, m83[:, :, 2:3].to_broadcast([128, 16, 8]), ALU.is_ge)
        fw.tt("dve", sel, sel, C["mg_v1"][:], ALU.mult)
        fw.tt("dve", sel, sel, C["mg_e"][:], ALU.add)
        fw.ts("dve", mbb, sel, 240000.0, -240000.0, op0=ALU.mult, op1=ALU.add)
        for tt in range(4):
            pp = self.psb[tt % 2]
            for i in range(4):
                qb = tt * 4 + i
                fw.mm(pp[64:72, i * 128:(i + 1) * 128], mbb[:, qb * 8:(qb + 1) * 8], C["ident_b"][:], start=True, stop=True)
            fw.copy(self.evac_eng(), Qh[64:72, tt * 512:(tt + 1) * 512], pp[64:72, :])
        fw.dma("sp", Qh[72:78, :], self.cst["moba_aq"][h6])
        fw.dma("sp", Kh[64:72, :], self.cst["blkind"][:, :])
        fw.dma("sp", Kh[72:78, :], self.cst["moba_ak"][h6])

    def attn_dil(self, g, j, s, Vbuf, vslot, first, hook=None, pending=None):
        fw = self.fw
        d = DIL[g][1]
        L = T // d
        nblk = L // 128
        Qh, Kh = self.Qb[s], self.Kb[s]
        M = self.dilM[:, g * 4 + j, :]
        accN = self.accN

        def S(pbq):
            nb = pbq % nblk
            st = self.psb[3 + pbq % 3]
            qs = slice(pbq * 128, (pbq + 1) * 128)
            fw.mm(st[:, 0:128], Kh[:, qs], Qh[:, qs], start=True, stop=True)
            if nb > 0:
                fw.mm(st[:, 128:256], Kh[:, (pbq - 1) * 128:pbq * 128], Qh[:, qs], start=True, stop=True)
        S(0)
        S(1)
        for c in range(4):
            ot = self.psb[6 + c % 2]
            for i in range(4):
                pbq = c * 4 + i
                nb = pbq % nblk
                if pbq + 2 < 16:
                    S(pbq + 2)
                st = self.psb[3 + pbq % 3]
                pt = self.ptb[pbq % 3]
                w = 256 if nb > 0 else 128
                fw.act(pt[:, 0:w], st[:, 0:w], AF.Exp, scale=0.125)
                fw.tt("pool", pt[:, 0:w], pt[:, 0:w], M[:, 0:w], ALU.mult)
                oc = ot[0:65, i * 128:(i + 1) * 128]
                fw.mm(oc, Vbuf[:, pbq, vslot, 0:65], pt[:, 0:128], start=True, stop=(nb == 0))
                if nb > 0:
                    fw.mm(oc, Vbuf[:, pbq - 1, vslot, 0:65], pt[:, 128:256], start=False, stop=True)
                if pending is not None and pbq == 2:
                    pending()
                    pending = None
            if d == 1:
                dst, src = accN[0:65, c * 512:(c + 1) * 512], ot[0:65, :]
            elif d == 4:
                dst, src = accN[0:65, c:c + 4 * 511 + 1:4], ot[0:65, :]
            else:
                dst = accN[0:65, :].rearrange("p (n r) -> p r n", r=16)[:, 4 * c:4 * c + 4, :]
                src = ot[0:65, :].rearrange("p (r n) -> p r n", r=4)
            if first:
                fw.copy("dve", dst, src)
            else:
                fw.tt("dve", dst, src, dst, ALU.add)
            if hook is not None and c == 1:
                hook()
                hook = None

    def stage_attention(self, l):
        fw = self.fw
        heads = self.heads or ("fox", "moba", "dil")
        fw.memset("pool", self.VA[:, :, :, 64:65], 1.0)
        fw.memset("pool", self.VB[:, :, :, 64:65], 1.0)
        jobs = []
        if "fox" in heads:
            self.fox_prep(l)
            for h in range(6):
                s = h % 2

                def prep(h=h, s=s):
                    if h == 0:
                        self.load_wv(l, 0)
                        self.proj_v(6, 1, self.VA, 0, 0)
                    self.proj_qk(l, h, s, 1)
                    self.fox_aug(h, s)

                def attn(hook, pending, h=h, s=s):
                    dest = self.OT[h // 2][0:64, :] if h % 2 == 0 else self.oddt[0:64, :]
                    p = self.attn_causal(s, self.VA, h, dest, hook=hook, pending=pending)
                    if h % 2 == 1:
                        def fin2(p=p, h=h):
                            p()
                            fw.dma("sp", self.OT[h // 2][64:128, :], self.oddt[0:64, :])
                        return fin2
                    return p
                jobs.append((prep, attn))
        if "moba" in heads:
            for h6 in range(6):
                h = 6 + h6
                s = h % 2

                def prep(h=h, h6=h6, s=s):
                    if h6 == 0:
                        self.load_wv(l, 1)
                        self.proj_v(6, 1, self.VB, 0, 0)
                    self.proj_qk(l, h, s, 1)
                    self.moba_prep(h6, s)

                def attn(hook, pending, h6=h6, s=s):
                    dest = self.OT[3 + h6 // 2][0:64, :] if h6 % 2 == 0 else self.oddt[0:64, :]
                    p = self.attn_causal(s, self.VB, h6, dest, hook=hook, pending=pending)
                    if h6 % 2 == 1:
                        def fin2(p=p, h6=h6):
                            p()
                            fw.dma("sp", self.OT[3 + h6 // 2][64:128, :], self.oddt[0:64, :])
                        return fin2
                    return p
                jobs.append((prep, attn))
        if "dil" in heads:
            n = 0
            for j in range(4):
                for g in range(3):
                    h = 12 + g * 4 + j
                    Vbuf, vslot = (self.VA, h - 12) if h < 18 else (self.VB, h - 18)
                    s = n % 2
                    first_dil = (n == 0)
                    n += 1

                    def prep(h=h, s=s, g=g, first_dil=first_dil):
                        if first_dil:
                            self.load_wv(l, 2)
                            self.proj_v(4, 1, self.VA, 0, 0)
                            self.proj_v(2, 4, self.VA, 4, 256)
                        if h == 20:
                            self.load_wv(l, 3)
                            self.proj_v(2, 4, self.VB, 0, 0)
                            self.proj_v(4, 16, self.VB, 2, 128)
                        self.proj_qk(l, h, s, DIL[g][1])

                    def attn(hook, pending, g=g, j=j, s=s, Vbuf=Vbuf, vslot=vslot):
                        self.attn_dil(g, j, s, Vbuf, vslot, g == 0, hook=hook, pending=pending)
                        if g == 2:
                            dest = self.OT[6 + j // 2][0:64, :] if j % 2 == 0 else self.oddt[0:64, :]
                            rbs = []
                            for tt in range(4):
                                cs = slice(tt * 512, (tt + 1) * 512)
                                rbs.append(self.recip_row(self.accN[64:65, cs], tt, "act"))

                            def fin(j=j, dest=dest):
                                for tt in range(4):
                                    cs = slice(tt * 512, (tt + 1) * 512)
                                    rb = self.rbt4[tt]
                                    self.bcast_mul(self.accN[0:64, cs], rb, dest[:, cs])
                                if j % 2 == 1:
                                    fw.dma("sp", self.OT[6 + j // 2][64:128, :], self.oddt[0:64, :])
                            return fin
                        return None
                    jobs.append((prep, attn))
        pending = None
        if jobs:
            jobs[0][0]()
        for i, (prep, attn) in enumerate(jobs):
            hook = jobs[i + 1][0] if i + 1 < len(jobs) else None
            pending = attn(hook, pending)
        if pending is not None:
            pending()

    def stage_outproj(self, l):
        fw = self.fw
        yT = [self.carve(A_Y + i * 4096, [128, T], BF16) for i in range(8)]
        wbr = [self.carve(A_Z2 + i * 2048, [128, 8, 128], BF16) for i in range(2)]
        wg = [self.carve(A_Z2 + 4096 + i * 2048, [128, 8, 128], BF16) for i in range(4)]
        Wl = self.inp["w_in"][l]
        ng = 0
        for fc in range(8):
            fs = slice(fc * 128, (fc + 1) * 128)
            wb = wbr[fc % 2]
            fw.dma("pool", wb[:, 0:3, :], self.inp["w_br_fox"][l].rearrange("(kc p) n -> p kc n", p=128)[:, :, fs])
            fw.dma("pool", wb[:, 3:6, :], self.inp["w_br_moba"][l].rearrange("(kc p) n -> p kc n", p=128)[:, :, fs])
            fw.dma("pool", wb[:, 6:8, :], self.inp["w_br_dil"][l].rearrange("(kc p) n -> p kc n", p=128)[:, :, fs])
            wgs = []
            for br in range(3):
                w = wg[ng % 4]
                ng += 1
                fw.dma("pool", w, self.wview(Wl, GOFF + br * 1024 + fc * 128, 128))
                wgs.append(w)
            for tt in range(4):
                cs = slice(tt * 512, (tt + 1) * 512)
                nt0, nt1 = self.ntmp[0], self.ntmp[1 + tt % 2]
                for br, (k0, nk) in enumerate(((0, 3), (3, 3), (6, 2))):
                    pz = self.psb[br]
                    for kc in range(nk):
                        fw.mm(pz[:, :], wb[:, k0 + kc, :], self.OT[k0 + kc][:, cs], start=(kc == 0), stop=(kc == nk - 1))
                    pg = self.psb[3 + br]
                    for kc in range(8):
                        fw.mm(pg[:, :], wgs[br][:, kc, :], self.hT[:, kc, cs], start=(kc == 0), stop=(kc == 7))
                    sg = self.sqb[br]
                    fw.act(sg[:], pg[:, :], AF.Sigmoid)
                    if br == 0:
                        fw.tt("dve", nt0[:], pz[:, :], sg[:], ALU.mult)
                    else:
                        fw.tt("dve", nt1[:], pz[:, :], sg[:], ALU.mult)
                        if br == 1:
                            fw.tt("pool", nt0[:], nt0[:], nt1[:], ALU.add)
                        else:
                            fw.tt("pool", yT[fc][:, cs], nt0[:], nt1[:], ALU.add)
        for fc in range(8):
            w = wg[ng % 4]
            ng += 1
            fw.dma("pool", w, self.wview(self.inp["w_out"][l], fc * 128, 128))
            for tt in range(4):
                cs = slice(tt * 512, (tt + 1) * 512)
                ps = self.psb[6 + tt % 2]
                for kc in range(8):
                    fw.mm(ps[:, :], w[:, kc, :], yT[kc][:, cs], start=(kc == 0), stop=(kc == 7))
                fw.stt(self.xT[:, fc, cs], ps[:, :], self.modT[:, l, 16 + fc:17 + fc], self.xT[:, fc, cs], ALU.mult, ALU.add)

    def ffn_expert(self, l, wg_ap, wu_ap, wd_ap, combbc):
        fw = self.fw
        actT = self.carve(A_ACT, [128, 11, T], BF16)
        wgu = [self.carve(A_WGU + i * 2048, [128, 8, 128], BF16) for i in range(4)]
        wdt = [self.carve(A_WD + i * 2816, [128, 11, 128], BF16) for i in range(2)]
        sgt = [self.carve(A_SG + i * 1024, [128, 512], BF16) for i in range(3)]
        if not hasattr(self, "_ffc"):
            self._ffc = 0
            self._wdc = 0
        for half in range(2):
            for fi in range(11):
                ffc = half * 11 + fi
                wgt = wgu[(2 * self._ffc) % 4]
                wut = wgu[(2 * self._ffc + 1) % 4]
                self._ffc += 1
                fw.dma("pool", wgt, self.wview(wg_ap, ffc * 128, 128))
                fw.dma("pool", wut, self.wview(wu_ap, ffc * 128, 128))
                for tt in range(4):
                    cs = slice(tt * 512, (tt + 1) * 512)
                    pg = self.psb[(2 * tt) % 4]
                    pu = self.psb[(2 * tt + 1) % 4]
                    for kc in range(8):
                        fw.mm(pg[:, :], wgt[:, kc, :], self.hT[:, kc, cs], start=(kc == 0), stop=(kc == 7))
                    for kc in range(8):
                        fw.mm(pu[:, :], wut[:, kc, :], self.hT[:, kc, cs], start=(kc == 0), stop=(kc == 7))
                    sg = sgt[tt % 3]
                    fw.act(sg, pg[:, :], AF.Silu)
                    if combbc is not None:
                        fw.tt("pool", sg, sg, combbc[:, cs], ALU.mult)
                    fw.tt("dve", actT[:, fi, cs], pu[:, :], sg, ALU.mult)
            for fc in range(8):
                wd = wdt[self._wdc % 2]
                self._wdc += 1
                fw.dma("pool", wd, wd_ap.rearrange("(f p) n -> p f n", p=128)[:, half * 11:(half + 1) * 11, fc * 128:(fc + 1) * 128])
                for tt in range(4):
                    cs = slice(tt * 512, (tt + 1) * 512)
                    ps = self.psb[4 + tt % 2]
                    for fi in range(11):
                        fw.mm(ps[:, :], wd[:, fi, :], actT[:, fi, cs], start=(fi == 0), stop=(fi == 10))
                    fw.stt(self.xT[:, fc, cs], ps[:, :], self.modT[:, l, 40 + fc:41 + fc], self.xT[:, fc, cs], ALU.mult, ALU.add)

    def stage_moe(self, l):
        fw, C = self.fw, self.C
        R0 = A_Z2 + 8192
        wr = self.carve(R0, [128, 8, 8], F32)
        wr2 = self.carve(R0 + 256, [128, 8, 8], F32)
        brt = self.carve(R0 + 512, [128, 8], F32)
        crow = self.carve(R0 + 544, [128, 8], F32)
        cb = self.carve(R0 + 576, [128, 8], F32)
        rt = self.carve(R0 + 608, [128, 16], F32)
        lg = self.carve(R0 + 1024, [128, 16, 8], F32)
        m8 = self.carve(R0 + 1536, [128, 16, 8], F32)
        eq = self.carve(R0 + 2048, [128, 16, 8], F32)
        comb = self.carve(R0 + 2560, [128, 16, 8], F32)
        w1 = self.carve(R0 + 3072, [128, 16], F32)
        w2 = self.carve(R0 + 3136, [128, 16], F32)
        e21 = self.carve(R0 + 3200, [128, 16], F32)
        fw.dma("sp", wr, self.inp["w_router"][0].rearrange("(kc p) e -> p kc e", p=128))
        fw.dma("sp", brt[0:1, :], self.inp["b_router"][0:1, :])
        for kc in range(8):
            fw.ts("dve", wr2[:, kc, :], wr[:, kc, :], self.a2[:, l, kc:kc + 1], None, op0=ALU.mult)
        pc = self.psb[2]
        for kc in range(8):
            fw.mm(pc[0:1, 0:8], self.modT[:, l, 24 + kc:25 + kc], wr[:, kc, :], start=(kc == 0), stop=(kc == 7))
        fw.tt("dve", crow[0:1, :], pc[0:1, 0:8], brt[0:1, :], ALU.add)
        pcb = self.psb[3]
        fw.mm(pcb[:, 0:8], C["ones_f"][0:1, :], crow[0:1, :], start=True, stop=True)
        fw.copy("dve", cb, pcb[:, 0:8])
        pl = self.psb[6]
        for t16 in range(16):
            for kc in range(8):
                fw.mm(pl[:, t16 * 8:(t16 + 1) * 8], self.xT[:, kc, t16 * 128:(t16 + 1) * 128], wr2[:, kc, :],
                      start=(kc == 0), stop=(kc == 7))
        fw.copy("dve", rt, self.psb[7][:, 0:16])
        fw.tt("dve", lg, pl[:, 0:128].rearrange("p (a b) -> p a b", b=8), rt.unsqueeze(2).to_broadcast([128, 16, 8]), ALU.mult)
        fw.tt("dve", lg, lg, cb.unsqueeze(1).to_broadcast([128, 16, 8]), ALU.add)
        for t16 in range(16):
            fw.gen("dve", "max", [lg[:, t16, :]], [m8[:, t16, :]], out=m8[:, t16, :], in_=lg[:, t16, :])
        fw.tt("dve", e21, m8[:, :, 1], m8[:, :, 0], ALU.subtract)
        fw.act(e21, e21, AF.Exp)
        fw.ts("dve", w1, e21, 1.0, None, op0=ALU.add)
        fw.gen("dve", "reciprocal", [w1], [w1], out=w1, in_=w1)
        fw.tt("dve", w2, e21, w1, ALU.mult)
        fw.tt("dve", eq, lg, m8[:, :, 0:1].to_broadcast([128, 16, 8]), ALU.is_equal)
        fw.tt("dve", comb, eq, w1.unsqueeze(2).to_broadcast([128, 16, 8]), ALU.mult)
        fw.tt("dve", eq, lg, m8[:, :, 1:2].to_broadcast([128, 16, 8]), ALU.is_equal)
        fw.tt("dve", eq, eq, w2.unsqueeze(2).to_broadcast([128, 16, 8]), ALU.mult)
        fw.tt("dve", comb, comb, eq, ALU.add)
        if "comb" in self.debug:
            o = self.dbg_out("comb", [128, 16, 8], F32)
            fw.dma("sp", o[:, :, :], comb)
        cbc = [self.carve(A_Z2 + i * 4096, [128, T], BF16) for i in range(2)]
        for e in range(NEXP):
            cc = cbc[e % 2]
            for tt in range(4):
                nt = self.ntmp[tt % 3]
                for i in range(4):
                    fw.ts("dve", nt[:, i * 128:(i + 1) * 128], C["ident_f"][:], comb[:, tt * 4 + i, e:e + 1], None, op0=ALU.mult)
                pb = self.psb[6 + tt % 2]
                fw.mm(pb[:, :], C["ones_f"][:], nt[:], start=True, stop=True)
                fw.copy("act", cc[:, tt * 512:(tt + 1) * 512], pb[:, :])
            self.ffn_expert(l, self.inp["w_exp_gate"][0, e], self.inp["w_exp_up"][0, e], self.inp["w_exp_down"][0, e], cc)

    def stage_output(self):
        fw, C = self.fw, self.C
        xo = [self.carve(i * 4096, [128, D], F32) for i in range(2)]
        for t16 in range(16):
            xb = xo[t16 % 2]
            for half in range(2):
                pb = self.psb[(t16 * 2 + half) % 4]
                for i in range(4):
                    dc = half * 4 + i
                    fw.tr(pb[:, i * 128:(i + 1) * 128], self.xT[:, dc, t16 * 128:(t16 + 1) * 128], C["ident_f"][:])
                fw.copy(self.evac_eng(), xb[:, half * 512:(half + 1) * 512], pb[:, :])
            fw.dma("sp", self.out[t16 * 128:(t16 + 1) * 128, :], xb)

    def stage_moe_sparse(self, l):
        fw, C, nc = self.fw, self.C, self.nc
        I32 = mybir.dt.int32
        R0 = A_Z2
        wr = self.carve(R0, [128, 8, 8], F32)
        wr2 = self.carve(R0 + 256, [128, 8, 8], F32)
        brt = self.carve(R0 + 512, [128, 8], F32)
        crow = self.carve(R0 + 544, [128, 8], F32)
        cb = self.carve(R0 + 576, [128, 8], F32)
        rt = self.carve(R0 + 608, [128, 16], F32)
        e21 = self.carve(R0 + 672, [128, 16], F32)
        lg = self.carve(R0 + 1024, [128, 16, 8], F32)
        m8 = self.carve(R0 + 1536, [128, 16, 8], F32)
        eq1 = self.carve(R0 + 2048, [128, 16, 8], F32)
        eq2 = self.carve(R0 + 2560, [128, 16, 8], F32)
        tot = self.carve(R0 + 3072, [128, 16, 8], F32)
        pre = self.carve(R0 + 3584, [128, 16, 8], F32)
        pos = self.carve(R0 + 4096, [128, 16, 8], F32)
        tmp = self.carve(R0 + 4608, [128, 16, 8], F32)
        maskb = self.carve(R0 + 5120, [128, 128], BF16)
        gf = self.carve(R0 + 5376, [128, 16], F32)
        cntf = self.carve(R0 + 5440, [128, 8], F32)
        w1 = self.sb("moe_w1", [128, 16], F32)[:, :]
        w2 = self.sb("moe_w2", [128, 16], F32)[:, :]
        gi = [self.sb("moe_gi%d" % k, [128, 16], I32)[:, :] for k in range(2)]
        cnti = self.sb("moe_cnt", [1, 8], I32)[:, :]
        fw.dma("sp", wr, self.inp["w_router"][0].rearrange("(kc p) e -> p kc e", p=128))
        fw.dma("sp", brt[0:1, :], self.inp["b_router"][0:1, :])
        for kc in range(8):
            fw.ts("dve", wr2[:, kc, :], wr[:, kc, :], self.a2[:, l, kc:kc + 1], None, op0=ALU.mult)
        pc = self.psb[2]
        for kc in range(8):
            fw.mm(pc[0:1, 0:8], self.modT[:, l, 24 + kc:25 + kc], wr[:, kc, :], start=(kc == 0), stop=(kc == 7))
        fw.tt("dve", crow[0:1, :], pc[0:1, 0:8], brt[0:1, :], ALU.add)
        pcb = self.psb[3]
        fw.mm(pcb[:, 0:8], C["ones_f"][0:1, :], crow[0:1, :], start=True, stop=True)
        fw.copy("dve", cb, pcb[:, 0:8])
        pl = self.psb[6]
        for t16 in range(16):
            for kc in range(8):
                fw.mm(pl[:, t16 * 8:(t16 + 1) * 8], self.xT[:, kc, t16 * 128:(t16 + 1) * 128], wr2[:, kc, :],
                      start=(kc == 0), stop=(kc == 7))
        fw.copy("dve", rt, self.psb[7][:, 0:16])
        bc3 = [128, 16, 8]
        fw.tt("dve", lg, pl[:, 0:128].rearrange("p (a b) -> p a b", b=8), rt.unsqueeze(2).to_broadcast(bc3), ALU.mult)
        fw.tt("dve", lg, lg, cb.unsqueeze(1).to_broadcast(bc3), ALU.add)
        for t16 in range(16):
            fw.gen("dve", "max", [lg[:, t16, :]], [m8[:, t16, :]], out=m8[:, t16, :], in_=lg[:, t16, :])
        fw.tt("dve", e21, m8[:, :, 1], m8[:, :, 0], ALU.subtract)
        fw.act(e21, e21, AF.Exp)
        fw.ts("dve", w1, e21, 1.0, None, op0=ALU.add)
        fw.gen("dve", "reciprocal", [w1], [w1], out=w1, in_=w1)
        fw.tt("dve", w2, e21, w1, ALU.mult)
        fw.tt("dve", eq1, lg, m8[:, :, 0:1].to_broadcast(bc3), ALU.is_equal)
        fw.tt("dve", eq2, lg, m8[:, :, 1:2].to_broadcast(bc3), ALU.is_equal)
        fl = lambda a: a.rearrange("p a b -> p (a b)")
        fw.tt("dve", maskb, fl(eq1), fl(eq2), ALU.add)
        ptot, pwi = self.psb[4], self.psb[5]
        fw.mm(ptot[:, 0:128], C["ones_b"][:], maskb, start=True, stop=True)
        for t16 in range(16):
            fw.mm(pwi[:, t16 * 8:(t16 + 1) * 8], C["ustrict"][:], maskb[:, t16 * 8:(t16 + 1) * 8], start=True, stop=True)
        fw.copy("dve", fl(tot), ptot[:, 0:128])
        fw.memset("dve", pre[:, 0, :], 0.0)
        for t16 in range(1, 16):
            fw.tt("dve", pre[:, t16, :], pre[:, t16 - 1, :], tot[:, t16 - 1, :], ALU.add)
        fw.tt("dve", cntf, pre[:, 15, :], tot[:, 15, :], ALU.add)
        fw.copy("dve", cnti[0:1, :], cntf[0:1, :])
        fw.tt("dve", fl(pos), pwi[:, 0:128], fl(pre), ALU.add)
        fw.tt("dve", fl(pos), fl(pos), C["ebase"][:], ALU.add)
        for k, eq in enumerate((eq1, eq2)):
            fw.tt("dve", tmp, pos, eq, ALU.mult)
            fw.gen("dve", "tensor_reduce", [tmp], [gf], out=gf, in_=tmp, axis=AX.X, op=ALU.add)
            fw.copy("dve", gi[k], gf)
        if "moeidx" in self.debug:
            o = self.dbg_out("gi0", [128, 16], I32)
            fw.dma("sp", o[:, :], gi[0][:])
            o = self.dbg_out("gi1", [128, 16], I32)
            fw.dma("sp", o[:, :], gi[1][:])
            o = self.dbg_out("cnt", [1, 8], I32)
            fw.dma("sp", o[:, :], cnti[:])
        rows = [[self.carve(k * 2056 + i * 4112, [128, 1028], BF16) for k in range(2)] for i in range(2)]
        xrow = [self.carve(16448 + i * 4096, [128, D], F32) for i in range(2)]
        wsrc = (w1, w2)
        for t16 in range(16):
            ts_ = slice(t16 * 128, (t16 + 1) * 128)
            ph = self.psb[t16 % 2].bitcast(BF16)
            for kc in range(8):
                fw.tr(ph[:, kc * 128:(kc + 1) * 128], self.hT[:, kc, ts_], C["ident_b"][:])
            for k in range(2):
                Rk = rows[t16 % 2][k]
                fw.copy("act" if k == 0 else "dve", Rk[:, 0:1024], ph[:, :])
                fw.copy("dve", Rk[:, 1024:1026].bitcast(F32), wsrc[k][:, t16:t16 + 1])
                g = nc.gpsimd
                idx = gi[k][:, t16:t16 + 1]
                src = Rk[:, 0:1026]
                fw.op("pool", (lambda src=src, idx=idx: g.indirect_dma_start(
                    out=self.HS[:, :], out_offset=bass.IndirectOffsetOnAxis(ap=idx, axis=0), in_=src, in_offset=None)),
                    reads=[src, idx], writes=[self.HS[:, :]], dma=True)
            xr = xrow[t16 % 2]
            for half in range(2):
                pb = self.psb[2 + half]
                for i in range(4):
                    dc = half * 4 + i
                    fw.tr(pb[:, i * 128:(i + 1) * 128], self.xT[:, dc, ts_], C["ident_f"][:])
                fw.copy("act" if half == 0 else "dve", xr[:, half * 512:(half + 1) * 512], pb[:, :])
            fw.dma("sp", self.XTOK[ts_, :], xr)
        E_WD, E_WGU, E_HTE, E_ACT, E_ACTT, E_G, E_YSL, E_SG, E_WS = 0, 22528, 56320, 89088, 134144, 139776, 143888, 147984, 149392
        wd = self.carve(E_WD, [128, 11, D], BF16)
        wgu = [self.carve(E_WGU + i * 5632, [128, 8, 352], BF16) for i in range(6)]
        hTe = self.carve(E_HTE, [128, 16, 1024], BF16)
        act_sl = self.carve(E_ACT, [128, 16, 1408], BF16)
        actT = [self.carve(E_ACTT + i * 2816, [128, 1408], BF16) for i in range(2)]
        G = [self.carve(E_G + i * 2056, [128, 1028], BF16) for i in range(2)]
        ysl = [self.carve(E_YSL + i * 2048, [128, D], BF16) for i in range(2)]
        sgt = [self.carve(E_SG + i * 704, [128, 352], BF16) for i in range(2)]
        ws = self.carve(E_WS, [128, 16], F32)
        y0 = self.hT.rearrange("p a b -> p (a b)").rearrange("p (c f) -> p c f", c=16)
        self._nw = 0

        def keyof(e):
            return "moe_e%d" % e

        def stage_G(e):
            key = keyof(e)
            for ce in fw.CENGS:
                def ld(key=key, e=e, ce=ce):
                    eo = fw.engobj[ce]
                    reg = eo.alloc_register("r_%s_%s" % (key, ce))
                    ins = eo.reg_load(reg, cnti[0:1, e:e + 1])
                    fw.cond_vals[(key, ce)] = eo.snap(reg)
                    return ins
                fw.op(ce, ld, reads=[cnti[0:1, e:e + 1]], writes=[])
            for c in range(16):
                Gc = G[c % 2]
                fw.dma("sp", Gc[:, 0:1026], self.HS[e * T + c * 128:e * T + (c + 1) * 128, :])
                fw.cur_cond = (key, c * 128)
                fw.copy("dve", ws[:, c:c + 1], Gc[:, 1024:1026].bitcast(F32))
                ph = self.psb[c % 2].bitcast(BF16)
                for kc in range(8):
                    fw.tr(ph[:, kc * 128:(kc + 1) * 128], Gc[:, kc * 128:(kc + 1) * 128], C["ident_b"][:])
                fw.copy("dve", hTe[:, c, :], ph[:, :])
                fw.cur_cond = None
            fw.barrier()

        def stage_U(e, half):
            key = keyof(e)
            Wg, Wu, Wd = self.inp["w_exp_gate"][0, e], self.inp["w_exp_up"][0, e], self.inp["w_exp_down"][0, e]
            tiles = {}

            def issue_cg(cg):
                col0 = half * 1408 + cg * 352
                wgt, wut = wgu[(2 * self._nw) % 6], wgu[(2 * self._nw + 1) % 6]
                self._nw += 1
                fw.dma("pool", wgt, self.wview(Wg, col0, 352))
                fw.dma("pool", wut, self.wview(Wu, col0, 352))
                tiles[cg] = (wgt, wut)
            issue_cg(0)
            issue_cg(1)
            issue_cg(2)
            fw.dma("pool", wd, Wd.rearrange("(f p) n -> p f n", p=128)[:, half * 11:(half + 1) * 11, :])
            for cg in range(4):
                wgt, wut = tiles[cg]
                for c in range(16):
                    pg, pu = self.psb[2 + 2 * (c % 2)], self.psb[3 + 2 * (c % 2)]
                    fw.cur_cond = (key, c * 128)
                    for kc in range(8):
                        fw.mm(pg[:, 0:352], hTe[:, c, kc * 128:(kc + 1) * 128], wgt[:, kc, :], start=(kc == 0), stop=(kc == 7))
                    for kc in range(8):
                        fw.mm(pu[:, 0:352], hTe[:, c, kc * 128:(kc + 1) * 128], wut[:, kc, :], start=(kc == 0), stop=(kc == 7))
                    sg = sgt[c % 2]
                    fw.act(sg, pg[:, 0:352], AF.Silu)
                    fw.stt(act_sl[:, c, cg * 352:(cg + 1) * 352], pu[:, 0:352], ws[:, c:c + 1], sg, ALU.mult, ALU.mult)
                    fw.cur_cond = None
                fw.barrier()
                if cg + 3 < 4:
                    issue_cg(cg + 3)

        def stage_D(e, half):
            key = keyof(e)
            for c in range(16):
                at = actT[c % 2]
                pa, pb2 = self.psb[0].bitcast(BF16), self.psb[1].bitcast(BF16)
                fw.cur_cond = (key, c * 128)
                for f in range(11):
                    dstp = pa[:, f * 128:(f + 1) * 128] if f < 8 else pb2[:, (f - 8) * 128:(f - 7) * 128]
                    fw.tr(dstp, act_sl[:, c, f * 128:(f + 1) * 128], C["ident_b"][:])
                fw.copy("dve", at[:, 0:1024], pa[:, :])
                fw.copy("dve", at[:, 1024:1408], pb2[:, 0:384])
                py = (self.psb[6], self.psb[7])
                for fo in range(2):
                    for f in range(11):
                        fw.mm(py[fo][:, :], at[:, f * 128:(f + 1) * 128], wd[:, f, fo * 512:(fo + 1) * 512],
                              start=(f == 0), stop=(f == 10))
                if half == 0:
                    fw.copy("dve", y0[:, c, 0:512], py[0][:, :])
                    fw.copy("dve", y0[:, c, 512:1024], py[1][:, :])
                else:
                    yb = ysl[c % 2]
                    fw.tt("dve", yb[:, 0:512], py[0][:, :], y0[:, c, 0:512], ALU.add)
                    fw.tt("dve", yb[:, 512:1024], py[1][:, :], y0[:, c, 512:1024], ALU.add)
                fw.cur_cond = None
                if half == 1:
                    fw.dma("sp", self.YS[e * T + c * 128:e * T + (c + 1) * 128, :], ysl[c % 2])
            fw.barrier()

        stage_G(0)
        for e in range(NEXP):
            stage_U(e, 0)
            stage_D(e, 0)
            stage_U(e, 1)
            if e + 1 < NEXP:
                stage_G(e + 1)
            stage_D(e, 1)
        g2bc = self.carve(0, [128, D], F32)
        dg = self.carve(4096, [128, D], F32)
        for dc in range(8):
            fw.ts("dve", dg[:, dc * 128:(dc + 1) * 128], C["ident_f"][:], self.modT[:, l, 40 + dc:41 + dc], None, op0=ALU.mult)
        for half in range(2):
            pb = self.psb[2 + half]
            fw.mm(pb[:, :], C["ones_f"][:], dg[:, half * 512:(half + 1) * 512], start=True, stop=True)
            fw.copy("act", g2bc[:, half * 512:(half + 1) * 512], pb[:, :])
        ya = [[self.carve(8192 + (2 * i + k) * 2048, [128, D], BF16) for k in range(2)] for i in range(2)]
        ysum = [self.carve(16384 + i * 4096, [128, D], F32) for i in range(2)]
        xo = [self.carve(24576 + i * 4096, [128, D], F32) for i in range(2)]
        for t16 in range(16):
            ts_ = slice(t16 * 128, (t16 + 1) * 128)
            g = nc.gpsimd
            for k in range(2):
                dst = ya[t16 % 2][k]
                idx = gi[k][:, t16:t16 + 1]
                fw.op("pool", (lambda dst=dst, idx=idx: g.indirect_dma_start(
                    out=dst, out_offset=None, in_=self.YS[:, :], in_offset=bass.IndirectOffsetOnAxis(ap=idx, axis=0))),
                    reads=[self.YS[:, :], idx], writes=[dst], dma=True)
            xb = xo[t16 % 2]
            fw.dma("sp", xb, self.XTOK[ts_, :])
            y1, y2 = ya[t16 % 2]
            ysm = ysum[t16 % 2]
            fw.tt("dve", ysm, y1, y2, ALU.add)
            fw.tt("pool", ysm, ysm, g2bc, ALU.mult)
            fw.tt("dve", xb, xb, ysm, ALU.add)
            fw.dma("sp", self.out[ts_, :], xb)


_CACHE = {}


def kernel(**inputs):
    inputs = {k: np.asarray(v) for k, v in inputs.items()}
    k = K()
    in_maps = []
    for b in range(8):
        m = host_layout(inputs, b)
        for kk, v in k.hc.items():
            m["c_" + kk] = v
        in_maps.append(m)
    res = run_bass_kernel_spmd(k.nc, in_maps, core_ids=list(range(8)))
    out = np.stack([np.asarray(res.results[b]["out"]) for b in range(8)], axis=0)
    return out.astype(np.float32)
```
